# Optimizing a Trainium2 kernel written in Bass

```python
import math
import jax, jax.numpy as jnp
from jax import lax
import numpy as np

D_MODEL = 1024
BATCH = 8
SEQ = 4096
DEPTH = 1

SSM_WIDTH = D_MODEL // 2
SSM_GROUP = 16
SSM_GROUPS = SSM_WIDTH // SSM_GROUP
SSM_STATE = 64
DT_MIN = 1e-3
DT_MAX = 1e-1
ATTN_HEADS = 8
HEAD_DIM = 64
ATTN_WIDTH = ATTN_HEADS * HEAD_DIM
MOBA_BLOCK = 256
MOBA_TOPK = 3
Q_CHUNK = 16
D_FF = 4 * D_MODEL
RMS_EPS = 1e-6
NEG_INF = -1e30
IN_COLS = SSM_WIDTH + 3 * ATTN_WIDTH + 2 * D_MODEL

kernel_name = "hybrid_s5_moba_gated_block"


def rms_norm(x, g):
    xf = x.astype(jnp.float32)
    var = jnp.mean(xf * xf, axis=-1, keepdims=True)
    return (xf * lax.rsqrt(var + RMS_EPS)).astype(x.dtype) * g


def _ssm_combine(left, right):
    ar1, ai1, br1, bi1 = left
    ar2, ai2, br2, bi2 = right
    ar = ar2 * ar1 - ai2 * ai1
    ai = ar2 * ai1 + ai2 * ar1
    br = ar2 * br1 - ai2 * bi1 + br2
    bi = ar2 * bi1 + ai2 * br1 + bi2
    return ar, ai, br, bi


def s5_mixer(u, lam_re, lam_im, log_dt, b_re, b_im, c_re, c_im, d_skip, w_glu, b_glu):
    f32 = jnp.float32
    bsz, seq, _ = u.shape
    uf = u.astype(f32)
    ug = uf.reshape(bsz, seq, SSM_GROUPS, SSM_GROUP)
    lr = lam_re.astype(f32)
    li = lam_im.astype(f32)
    dt = jnp.exp(log_dt.astype(f32))[:, None]
    mag = jnp.exp(lr * dt)
    ab_re = mag * jnp.cos(li * dt)
    ab_im = mag * jnp.sin(li * dt)
    nr = ab_re - 1.0
    ni = ab_im
    den = lr * lr + li * li
    coef_re = (nr * lr + ni * li) / den
    coef_im = (ni * lr - nr * li) / den
    br = b_re.astype(f32)
    bi = b_im.astype(f32)
    bb_re = coef_re[..., None] * br - coef_im[..., None] * bi
    bb_im = coef_re[..., None] * bi + coef_im[..., None] * br
    bu_re = jnp.einsum('blgh,gph->lbgp', ug, bb_re)
    bu_im = jnp.einsum('blgh,gph->lbgp', ug, bb_im)
    a_re = jnp.broadcast_to(ab_re[None, None], (seq, 1, SSM_GROUPS, SSM_STATE))
    a_im = jnp.broadcast_to(ab_im[None, None], (seq, 1, SSM_GROUPS, SSM_STATE))
    _, _, x_re, x_im = lax.associative_scan(_ssm_combine, (a_re, a_im, bu_re, bu_im), axis=0)
    y = (jnp.einsum('lbgp,ghp->blgh', x_re, c_re.astype(f32))
         - jnp.einsum('lbgp,ghp->blgh', x_im, c_im.astype(f32)))
    y = y.reshape(bsz, seq, SSM_WIDTH) + d_skip.astype(f32) * uf
    v = jax.nn.gelu(y)
    out = v * jax.nn.sigmoid(v @ w_glu.astype(f32) + b_glu.astype(f32))
    return out.astype(u.dtype)


def moba_attention(q, k, v):
    f32 = jnp.float32
    bsz, seq, nh, dh = q.shape
    nb = -(-seq // MOBA_BLOCK)
    lp = nb * MOBA_BLOCK
    pad = lp - seq
    def prep(t):
        t = jnp.transpose(t, (0, 2, 1, 3))
        return jnp.pad(t, ((0, 0), (0, 0), (0, pad), (0, 0)))
    q, k, v = prep(q), prep(k), prep(v)
    kb = k.reshape(bsz, nh, nb, MOBA_BLOCK, dh)
    vb = v.reshape(bsz, nh, nb, MOBA_BLOCK, dh)
    k_mean = jnp.mean(kb.astype(f32), axis=3)
    gate = jnp.einsum('bhtd,bhnd->bhtn', q.astype(f32), k_mean)
    pos = jnp.arange(lp, dtype=jnp.int32)
    qblk = pos // MOBA_BLOCK
    past = jnp.arange(nb, dtype=jnp.int32)[None, :] < qblk[:, None]
    gate = jnp.where(past, gate, NEG_INF)
    n_sel = min(MOBA_TOPK, max(nb - 1, 0))
    if n_sel > 0:
        _, sel = lax.top_k(gate, n_sel)
        sel = sel.astype(jnp.int32)
    else:
        sel = jnp.zeros(gate.shape[:-1] + (0,), jnp.int32)
    sel_valid = sel < qblk[:, None]
    own = jnp.broadcast_to(qblk[:, None], sel.shape[:-1] + (1,))
    idx = jnp.concatenate([sel, own], axis=-1)
    valid = jnp.concatenate([sel_valid, jnp.ones(own.shape, bool)], axis=-1)
    n_slots = idx.shape[-1]
    bh = bsz * nh
    nc = lp // Q_CHUNK
    def to_chunks(t):
        t = t.reshape((bh, nc, Q_CHUNK) + t.shape[3:])
        return jnp.moveaxis(t, 1, 0)
    q_ch = to_chunks(q)
    idx_ch = to_chunks(idx)
    val_ch = to_chunks(valid)
    pos_ch = pos.reshape(nc, Q_CHUNK)
    kb_f = kb.reshape(bh, nb, MOBA_BLOCK, dh)
    vb_f = vb.reshape(bh, nb, MOBA_BLOCK, dh)
    scale = 1.0 / math.sqrt(dh)
    offs = jnp.arange(MOBA_BLOCK, dtype=jnp.int32)
    gather = jax.vmap(lambda blocks, ii: blocks[ii])

    def chunk_fn(args):
        qc, ic, vc, pc = args
        kg = gather(kb_f, ic)
        vg = gather(vb_f, ic)
        s = jnp.einsum('nqd,nqjsd->nqjs', qc, kg).astype(f32) * scale
        key_pos = ic[..., None] * MOBA_BLOCK + offs
        mask = vc[..., None] & (key_pos <= pc[None, :, None, None])
        s = jnp.where(mask, s, NEG_INF)
        p = jax.nn.softmax(s.reshape(bh, Q_CHUNK, n_slots * MOBA_BLOCK), axis=-1)
        p = p.reshape(s.shape).astype(vg.dtype)
        return jnp.einsum('nqjs,nqjsd->nqd', p, vg)

    out = lax.map(chunk_fn, (q_ch, idx_ch, val_ch, pos_ch))
    out = jnp.moveaxis(out, 0, 1).reshape(bsz, nh, lp, dh)[:, :, :seq]
    return jnp.transpose(out, (0, 2, 1, 3)).reshape(bsz, seq, nh * dh)


def setup_inputs(seed: int = 0) -> dict:
    key = jax.random.key(seed)
    ks = jax.random.split(key, 24)
    f32 = jnp.float32
    def nrm(k, shape, scale):
        return jax.random.normal(k, shape, f32) * scale
    def gain(k):
        return 1.0 + 0.01 * jax.random.normal(k, (DEPTH, D_MODEL), f32)
    x = jax.random.normal(ks[0], (BATCH, SEQ, D_MODEL), f32)
    lam_re = -0.5 + 0.01 * jax.random.normal(ks[1], (DEPTH, SSM_GROUPS, SSM_STATE), f32)
    lam_im = (math.pi * jnp.arange(SSM_STATE, dtype=f32))[None, None, :] \
        + 0.01 * jax.random.normal(ks[2], (DEPTH, SSM_GROUPS, SSM_STATE), f32)
    log_dt = jax.random.uniform(ks[3], (DEPTH, SSM_GROUPS), f32,
                                minval=math.log(DT_MIN), maxval=math.log(DT_MAX))
    return {
        "x": x,
        "g_pre_mix": gain(ks[4]),
        "w_in": nrm(ks[5], (DEPTH, D_MODEL, IN_COLS), D_MODEL ** -0.5),
        "lam_re": lam_re,
        "lam_im": lam_im,
        "log_dt": log_dt,
        "b_re": nrm(ks[6], (DEPTH, SSM_GROUPS, SSM_STATE, SSM_GROUP), (2 * SSM_GROUP) ** -0.5),
        "b_im": nrm(ks[7], (DEPTH, SSM_GROUPS, SSM_STATE, SSM_GROUP), (2 * SSM_GROUP) ** -0.5),
        "c_re": nrm(ks[8], (DEPTH, SSM_GROUPS, SSM_GROUP, SSM_STATE), (2 * SSM_STATE) ** -0.5),
        "c_im": nrm(ks[9], (DEPTH, SSM_GROUPS, SSM_GROUP, SSM_STATE), (2 * SSM_STATE) ** -0.5),
        "d_skip": nrm(ks[10], (DEPTH, SSM_WIDTH), 1.0),
        "w_glu": nrm(ks[11], (DEPTH, SSM_WIDTH, SSM_WIDTH), SSM_WIDTH ** -0.5),
        "b_glu": nrm(ks[12], (DEPTH, SSM_WIDTH), 0.01),
        "w_branch_a": nrm(ks[13], (DEPTH, SSM_WIDTH, D_MODEL), SSM_WIDTH ** -0.5),
        "w_branch_b": nrm(ks[14], (DEPTH, ATTN_WIDTH, D_MODEL), ATTN_WIDTH ** -0.5),
        "w_out": nrm(ks[15], (DEPTH, D_MODEL, D_MODEL), D_MODEL ** -0.5),
        "g_post_mix": gain(ks[16]),
        "g_pre_ffn": gain(ks[17]),
        "w_ff1": nrm(ks[18], (DEPTH, D_MODEL, D_FF), D_MODEL ** -0.5),
        "w_ff2": nrm(ks[19], (DEPTH, D_FF, D_MODEL), D_FF ** -0.5),
        "g_post_ffn": gain(ks[20]),
    }


def reference(x, g_pre_mix, w_in, lam_re, lam_im, log_dt, b_re, b_im, c_re, c_im, d_skip,
              w_glu, b_glu, w_branch_a, w_branch_b, w_out, g_post_mix, g_pre_ffn,
              w_ff1, w_ff2, g_post_ffn):
    bsz, seq, _ = x.shape
    cuts = [SSM_WIDTH,
            SSM_WIDTH + ATTN_WIDTH,
            SSM_WIDTH + 2 * ATTN_WIDTH,
            SSM_WIDTH + 3 * ATTN_WIDTH,
            SSM_WIDTH + 3 * ATTN_WIDTH + D_MODEL]
    h = x
    for l in range(DEPTH):
        u = rms_norm(h, g_pre_mix[l])
        z = u @ w_in[l]
        z_ssm, z_q, z_k, z_v, z_ga, z_gb = jnp.split(z, cuts, axis=-1)
        y_a = s5_mixer(z_ssm, lam_re[l], lam_im[l], log_dt[l], b_re[l], b_im[l],
                       c_re[l], c_im[l], d_skip[l], w_glu[l], b_glu[l])
        heads = (bsz, seq, ATTN_HEADS, HEAD_DIM)
        y_b = moba_attention(z_q.reshape(heads), z_k.reshape(heads), z_v.reshape(heads))
        merged = (jax.nn.sigmoid(z_ga) * (y_a @ w_branch_a[l])
                  + jax.nn.sigmoid(z_gb) * (y_b @ w_branch_b[l]))
        mix = merged @ w_out[l]
        h = h + rms_norm(mix, g_post_mix[l])
        f = rms_norm(h, g_pre_ffn[l])
        f = jnp.square(jax.nn.relu(f @ w_ff1[l])) @ w_ff2[l]
        h = h + rms_norm(f, g_post_ffn[l])
    return h
```

```python
import contextlib
import math
import numpy as np
import concourse.bass as bass
import concourse.mybir as mybir
from concourse.bass_utils import run_bass_kernel_spmd

F32 = mybir.dt.float32
BF16 = mybir.dt.bfloat16
I32 = mybir.dt.int32
AF = mybir.ActivationFunctionType
ALU = mybir.AluOpType
AX = mybir.AxisListType

L = 4096
D = 1024
NEG = -1.0e30
EPS = 1e-6
ENGS = ("pe", "act", "dve", "pool", "sp")
STOP_AFTER = None
DBG_NOKS = False
DBG_NBLK = 16
DBG_QV = ""
DBG_ONLYQ = False
POOL_DMA_DEPTH = 4
DBG_NOYA = False
STRICT = False


class Buf:
    __slots__ = ("name", "last_w", "readers")

    def __init__(self, name):
        self.name = name
        self.last_w = None
        self.readers = []


class Op:
    __slots__ = ("eng", "fn", "deps", "idx", "is_dma", "dsem", "dval", "signal", "know")


class Prog:
    def __init__(self, nc):
        self.nc = nc
        self.ops = {e: [] for e in ENGS}
        self.all = []
        self.dma_sems = {}
        self.know = {e: {} for e in ENGS}

    def op(self, eng, fn, reads=(), writes=(), dma_key=None, extra_deps=()):
        o = Op()
        o.eng = eng
        o.fn = fn
        o.is_dma = dma_key is not None
        o.signal = False
        o.idx = len(self.ops[eng])
        cands = []
        for b in reads:
            if b.last_w is not None:
                cands.append((b.last_w, True))
        for b in writes:
            if b.last_w is not None:
                cands.append((b.last_w, False))
            for r in b.readers:
                cands.append((r, False))
        for p in extra_deps:
            cands.append((p, True))
        need = {}
        kn = self.know[eng]
        for (p, raw) in cands:
            if p.is_dma:
                if (not raw) and dma_key is not None and p.dsem == dma_key:
                    continue
                key = ("dma", p.dsem)
                val = p.dval
            else:
                if p.eng == eng and not raw and not STRICT:
                    continue
                key = ("eng", p.eng)
                val = p.idx
            if kn.get(key, -1) >= val:
                continue
            if key not in need or need[key][0] < val:
                need[key] = (val, p)
        o.deps = []
        for key, (val, p) in need.items():
            o.deps.append(p)
            if kn.get(key, -1) < val:
                kn[key] = val
            for k2, v2 in p.know.items():
                if kn.get(k2, -1) < v2:
                    kn[k2] = v2
        if o.is_dma:
            cnt = self.dma_sems.setdefault(dma_key, [0])
            cnt[0] += 1
            o.dsem = dma_key
            o.dval = cnt[0]
        o.know = dict(kn)
        if o.is_dma:
            o.know[("dma", o.dsem)] = o.dval
        else:
            o.know[("eng", eng)] = o.idx
        for b in reads:
            b.readers.append(o)
        for b in writes:
            b.last_w = o
            b.readers = []
        self.ops[eng].append(o)
        self.all.append(o)
        return o

    def barrier(self, dma_ops=()):
        lasts = [self.ops[e][-1] for e in ENGS if self.ops[e] and not self.ops[e][-1].is_dma]
        lasts = []
        for e in ENGS:
            for o in reversed(self.ops[e]):
                if not o.is_dma:
                    lasts.append(o)
                    break
        deps = list(lasts) + list(dma_ops)
        for e in ENGS:
            self.op(e, lambda eng: eng.nop(), extra_deps=deps)

    def emit(self, final_wait_ops=()):
        nc = self.nc
        for o in self.all:
            for p in o.deps:
                if not p.is_dma:
                    p.signal = True
        sigval = {}
        for e in ENGS:
            c = 0
            for o in self.ops[e]:
                if o.signal:
                    c += 1
                sigval[(e, o.idx)] = c
        with contextlib.ExitStack() as st:
            esem = {e: st.enter_context(nc.semaphore("sem_" + e)) for e in ENGS}
            dsem = {k: st.enter_context(nc.semaphore("dsem%d" % i)) for i, k in enumerate(self.dma_sems)}
            block = st.enter_context(nc.Block())
            engobj = {"pe": block.tensor, "act": block.scalar, "dve": block.vector,
                      "pool": block.gpsimd, "sp": block.sync}

            def run(ename, eng):
                for o in self.ops[ename]:
                    for p in o.deps:
                        if p.is_dma:
                            eng.wait_ge(dsem[p.dsem], 16 * p.dval)
                        else:
                            eng.wait_ge(esem[p.eng], sigval[(p.eng, p.idx)])
                    ins = o.fn(eng)
                    if o.is_dma:
                        ins.then_inc(dsem[o.dsem], 16)
                    elif o.signal:
                        ins.then_inc(esem[ename], 1)
                if ename == "sp":
                    fw = {}
                    for p in final_wait_ops:
                        fw[p.dsem] = max(fw.get(p.dsem, 0), p.dval)
                    for k, v in fw.items():
                        eng.wait_ge(dsem[k], 16 * v)

            for ename in ENGS:
                engobj[ename](lambda eng, ename=ename: run(ename, eng))


def build_nc(dbg=False):
    nc = bass.Bass("TRN2", target_bir_lowering=False)

    def din(name, shape, dt=F32):
        return nc.dram_tensor(name, list(shape), dt, kind="ExternalInput").ap()

    x_d = din("x", [L, D])
    w_in_d = din("w_in", [D, 4096])
    w_glu_d = din("w_glu", [512, 512])
    w_a_d = din("w_a", [512, D])
    w_b_d = din("w_b", [512, D])
    w_out_d = din("w_out", [D, D])
    w_ff1_d = din("w_ff1", [D, 4096])
    w_ff2_d = din("w_ff2", [4096, D])
    g1_d = din("g1", [128, 8])
    g3_d = din("g3", [128, 8])
    g2_d = din("g2", [1, D])
    g4_d = din("g4", [1, D])
    bglu_d = din("bglu", [128, 4])
    LR_d = din("LR", [128, 16])
    LI_d = din("LI", [128, 16])
    LDT_d = din("LDT", [128, 16])
    BR_d = din("BR", [128, 256])
    BI_d = din("BI", [128, 256])
    CR_d = din("CR", [128, 256])
    CI_d = din("CI", [128, 256])
    DS_d = din("DS", [128, 32])
    out_d = nc.dram_tensor("out", [L, D], F32, kind="ExternalOutput").ap()
    ya_d = nc.dram_tensor("ya_scr", [512, L], BF16, kind="ExternalOutput" if dbg else "Internal").ap()

    if dbg:
        dbgb_d = nc.dram_tensor("dbgb", [128, 8192], BF16, kind="ExternalOutput").ap()
        dbgf_d = nc.dram_tensor("dbgf", [128, 4096], F32, kind="ExternalOutput").ap()
    pg = Prog(nc)
    O = pg.op
    bufs = {}

    def B(name):
        if name not in bufs:
            bufs[name] = Buf(name)
        return bufs[name]

    final_stores = []
    TWO_PI = 2.0 * math.pi

    with contextlib.ExitStack() as top:
        def T(st, name, shape, dt):
            return st.enter_context(nc.sbuf_tensor("s_" + name, list(shape), dt))

        def PS(st, name, shape, dt):
            return st.enter_context(nc.psum_tensor("p_" + name, list(shape), dt))

        identf = T(top, "identf", [128, 128], F32)
        identb = T(top, "identb", [128, 128], BF16)
        O("pool", lambda e: e.memset(identf[:], 1.0), writes=[B("identf")])
        O("pool", lambda e: e.affine_select(out=identf[:], in_=identf[:], pattern=[[1, 128]], compare_op=ALU.is_equal,
                                            fill=0.0, base=0, channel_multiplier=-1), reads=[B("identf")], writes=[B("identf")])
        O("dve", lambda e: e.tensor_copy(out=identb[:], in_=identf[:]), reads=[B("identf")], writes=[B("identb")])

        pool_dmas = []

        def load(eng, dst, src, bname, key=None):
            ex = ()
            if eng == "pool" and len(pool_dmas) >= POOL_DMA_DEPTH:
                ex = (pool_dmas[-POOL_DMA_DEPTH],)
            o = O(eng, lambda e: e.dma_start(out=dst, in_=src), writes=[B(bname)], dma_key=key or bname, extra_deps=ex)
            if eng == "pool":
                pool_dmas.append(o)
            return o

        def rmsnorm_T(st_name, xt, xbuf, gsb, uT_dst_fn, ubuf, ps_list, psbufs, scr, pfx, gbuf="gsb"):
            sq, ss, rs, xn = scr["sq"], scr["ss"], scr["rs"], scr["xn"]
            O("act", lambda e: e.activation(out=sq, in_=xt, func=AF.Square, accum_out=ss), reads=[xbuf], writes=[B(pfx + "sq"), B(pfx + "ss")])
            O("act", lambda e: e.activation(out=rs, in_=ss, func=AF.Sqrt, scale=1.0 / D, bias=scr["eps"]), reads=[B(pfx + "ss"), B("epsb")], writes=[B(pfx + "rs")])
            O("dve", lambda e: e.reciprocal(out=rs, in_=rs), reads=[B(pfx + "rs")], writes=[B(pfx + "rs")])
            O("dve", lambda e: e.tensor_scalar(out=xn, in0=xt, scalar1=rs, scalar2=None, op0=ALU.mult), reads=[xbuf, B(pfx + "rs")], writes=[B(pfx + "xn")])
            for half in range(2):
                ps = ps_list[half]
                pb = psbufs[half]
                for k in range(4):
                    dc = half * 4 + k
                    O("pe", lambda e, dc=dc, k=k, ps=ps: e.transpose(out=ps[:, k * 128:(k + 1) * 128], in_=xn[:, dc * 128:(dc + 1) * 128], identity=identf[:]),
                      reads=[B(pfx + "xn"), B("identf")], writes=[pb])
                for k in range(4):
                    dc = half * 4 + k
                    eng = "act" if k % 2 == 0 else "dve"
                    if eng == "act":
                        O("act", lambda e, dc=dc, k=k, ps=ps: e.activation(out=uT_dst_fn(dc), in_=ps[:, k * 128:(k + 1) * 128], func=AF.Copy, scale=gsb[:, dc:dc + 1]),
                          reads=[pb, B(gbuf)], writes=[ubuf])
                    else:
                        O("dve", lambda e, dc=dc, k=k, ps=ps: e.tensor_scalar(out=uT_dst_fn(dc), in0=ps[:, k * 128:(k + 1) * 128], scalar1=gsb[:, dc:dc + 1], scalar2=None, op0=ALU.mult),
                          reads=[pb, B(gbuf)], writes=[ubuf])

        epsb = T(top, "epsb", [128, 1], F32)
        O("dve", lambda e: e.memset(epsb[:], EPS), writes=[B("epsb")])
        g1sb = T(top, "g1sb", [128, 8], F32)
        g3sb = T(top, "g3sb", [128, 8], F32)
        bglu = T(top, "bglu", [128, 4], F32)
        load("sp", g1sb[:], g1_d, "gsb", "c0")
        load("sp", g3sb[:], g3_d, "gsb3", "c1")
        load("sp", bglu[:], bglu_d, "bglu", "c2")

        sKV = top.enter_context(contextlib.ExitStack())
        KT = T(sKV, "KT", [128, 4, L], BF16)
        VA = T(sKV, "VA", [128, 32, 8, 66], BF16)
        KS = T(sKV, "KS", [128, 4, 16], F32)
        O("pool", lambda e: e.memset(VA[:, :, :, 64:65], 1.0), writes=[B("VAones")])
        O("pool", lambda e: e.memset(VA[:, :, :, 65:66], 0.0), writes=[B("VAzeros")])

        with contextlib.ExitStack() as sS:
            MI = T(sS, "MI", [128, 32, 128], BF16)
            MinR = T(sS, "MinR", [128, 32, 64], BF16)
            MinI = T(sS, "MinI", [128, 32, 64], BF16)
            MoRp = T(sS, "MoRp", [128, 16, 2, 128], BF16)
            MoIp = T(sS, "MoIp", [128, 16, 2, 128], BF16)
            RHO8 = T(sS, "RHO8", [128, 16], F32)
            TH8 = T(sS, "TH8", [128, 16], F32)

            with contextlib.ExitStack() as s0:
                LR = T(s0, "LR", [128, 16], F32)
                LI = T(s0, "LI", [128, 16], F32)
                DT = T(s0, "DT", [128, 16], F32)
                BR = T(s0, "BR", [128, 16, 16], F32)
                BI = T(s0, "BI", [128, 16, 16], F32)
                CR = T(s0, "CR", [128, 16, 16], F32)
                CI = T(s0, "CI", [128, 16, 16], F32)
                DS = T(s0, "DS", [128, 32], F32)
                NVi = T(s0, "NVi", [128, 17], I32)
                NV = T(s0, "NV", [128, 17], F32)
                LRDT = T(s0, "LRDT", [128, 16], F32)
                TH = T(s0, "TH", [128, 16], F32)
                ARG = T(s0, "ARG", [128, 16, 17], F32)
                ARK = T(s0, "ARK", [128, 16, 17], F32)
                ARKi = T(s0, "ARKi", [128, 16, 17], I32)
                MAG = T(s0, "MAG", [128, 16, 17], F32)
                PWR = T(s0, "PWR", [128, 16, 17], F32)
                PWI = T(s0, "PWI", [128, 16, 17], F32)
                zt1 = T(s0, "zt1", [128, 16], F32)
                zt2 = T(s0, "zt2", [128, 16], F32)
                zt3 = T(s0, "zt3", [128, 16], F32)
                cfr = T(s0, "cfr", [128, 16], F32)
                cfi = T(s0, "cfi", [128, 16], F32)
                BBR = T(s0, "BBR", [128, 16, 16], F32)
                BBI = T(s0, "BBI", [128, 16, 16], F32)
                tb = T(s0, "tb", [128, 16, 16], F32)
                HR = T(s0, "HR", [128, 16, 8, 16], F32)
                HI = T(s0, "HI", [128, 16, 8, 16], F32)
                GR = T(s0, "GR", [128, 16, 8, 16], F32)
                GI = T(s0, "GI", [128, 16, 8, 16], F32)
                MOR = T(s0, "MOR", [128, 16, 8, 16], F32)
                MOI = T(s0, "MOI", [128, 16, 8, 16], F32)
                tq = T(s0, "tq", [128, 16, 8, 16], F32)
                msk = T(s0, "msk", [128, 8, 16], F32)
                tmi = T(s0, "tmi", [128, 128], F32)
                ps_s0 = [PS(s0, "ps_s0_%d" % k, [128, 512], F32) for k in range(4)]

                load("sp", LR[:], LR_d, "LR", "c3")
                load("sp", LI[:], LI_d, "LI", "c4")
                load("sp", DT[:], LDT_d, "DT", "c5")
                load("sp", BR[:].rearrange("p a b -> p (a b)"), BR_d, "BR", "c6")
                load("sp", BI[:].rearrange("p a b -> p (a b)"), BI_d, "BI", "c7")
                load("sp", CR[:].rearrange("p a b -> p (a b)"), CR_d, "CR", "c8")
                load("sp", CI[:].rearrange("p a b -> p (a b)"), CI_d, "CI", "c9")
                load("sp", DS[:], DS_d, "DS", "c10")

                def V_(fn, r, w, eng="dve"):
                    return O(eng, fn, reads=[B(n) for n in r], writes=[B(n) for n in w])

                V_(lambda e: e.iota(NVi[:, 0:8], pattern=[[-1, 8]], base=-1, channel_multiplier=0), [], ["NVi"], "pool")
                V_(lambda e: e.iota(NVi[:, 8:17], pattern=[[1, 9]], base=0, channel_multiplier=0), [], ["NVi"], "pool")
                V_(lambda e: e.tensor_copy(out=NV[:], in_=NVi[:]), ["NVi"], ["NV"])
                V_(lambda e: e.activation(out=DT[:], in_=DT[:], func=AF.Exp), ["DT"], ["DT"], "act")
                V_(lambda e: e.tensor_tensor(out=LRDT[:], in0=LR[:], in1=DT[:], op=ALU.mult), ["LR", "DT"], ["LRDT"])
                V_(lambda e: e.tensor_tensor(out=TH[:], in0=LI[:], in1=DT[:], op=ALU.mult), ["LI", "DT"], ["TH"])
                bc3 = lambda a: a.unsqueeze(2).to_broadcast([128, 16, 17])
                nvb = NV[:].unsqueeze(1).to_broadcast([128, 16, 17])
                V_(lambda e: e.tensor_tensor(out=ARG[:], in0=bc3(LRDT[:]), in1=nvb, op=ALU.mult), ["LRDT", "NV"], ["ARG"])
                V_(lambda e: e.activation(out=MAG[:], in_=ARG[:], func=AF.Exp), ["ARG"], ["MAG"], "act")

                def sin_of(dst, src_fn, srcbufs, dstbuf, shift, shape3):
                    scrF, scrI = shape3
                    V_(lambda e: e.tensor_scalar(out=scrF, in0=src_fn(), scalar1=shift, scalar2=1.0 / TWO_PI, op0=ALU.add, op1=ALU.mult), srcbufs, ["scrF"])
                    V_(lambda e: e.tensor_copy(out=scrI, in_=scrF), ["scrF"], ["scrI"])
                    V_(lambda e: e.tensor_copy(out=scrF, in_=scrI), ["scrI"], ["scrF"])
                    V_(lambda e: e.tensor_scalar(out=scrF, in0=scrF, scalar1=-TWO_PI, scalar2=shift, op0=ALU.mult, op1=ALU.add), ["scrF"], ["scrF"])
                    V_(lambda e: e.tensor_tensor(out=scrF, in0=scrF, in1=src_fn(), op=ALU.add), ["scrF"] + srcbufs, ["scrF"])
                    V_(lambda e: e.tensor_scalar(out=scrF, in0=scrF, scalar1=3.14159, scalar2=-3.14159, op0=ALU.min, op1=ALU.max), ["scrF"], ["scrF"])
                    V_(lambda e: e.activation(out=dst, in_=scrF, func=AF.Sin), ["scrF"], [dstbuf], "act")

                V_(lambda e: e.tensor_tensor(out=ARG[:], in0=bc3(TH[:]), in1=nvb, op=ALU.mult), ["TH", "NV", "MAG"], ["ARG"])
                sin_of(PWI[:], lambda: ARG[:], ["ARG"], "PWI", 0.0, (ARK[:], ARKi[:]))
                sin_of(PWR[:], lambda: ARG[:], ["ARG"], "PWR", math.pi / 2, (ARK[:], ARKi[:]))
                V_(lambda e: e.tensor_tensor(out=PWR[:], in0=PWR[:], in1=MAG[:], op=ALU.mult), ["PWR", "MAG"], ["PWR"])
                V_(lambda e: e.tensor_tensor(out=PWI[:], in0=PWI[:], in1=MAG[:], op=ALU.mult), ["PWI", "MAG"], ["PWI"])
                V_(lambda e: e.tensor_copy(out=RHO8[:], in_=MAG[:, :, 16]), ["MAG"], ["RHO8"])
                V_(lambda e: e.tensor_scalar(out=TH8[:], in0=TH[:], scalar1=8.0, scalar2=None, op0=ALU.mult), ["TH"], ["TH8"])
                abr = PWR[:, :, 9]
                abi = PWI[:, :, 9]
                V_(lambda e: e.tensor_scalar(out=zt1[:], in0=abr, scalar1=-1.0, scalar2=None, op0=ALU.add), ["PWR"], ["zt1"])
                V_(lambda e: e.tensor_tensor(out=zt2[:], in0=LR[:], in1=LR[:], op=ALU.mult), ["LR"], ["zt2"])
                V_(lambda e: e.tensor_tensor(out=zt3[:], in0=LI[:], in1=LI[:], op=ALU.mult), ["LI"], ["zt3"])
                V_(lambda e: e.tensor_tensor(out=zt2[:], in0=zt2[:], in1=zt3[:], op=ALU.add), ["zt2", "zt3"], ["zt2"])
                V_(lambda e: e.reciprocal(out=zt2[:], in_=zt2[:]), ["zt2"], ["zt2"])
                V_(lambda e: e.tensor_tensor(out=cfr[:], in0=zt1[:], in1=LR[:], op=ALU.mult), ["zt1", "LR"], ["cfr"])
                V_(lambda e: e.tensor_tensor(out=zt3[:], in0=abi, in1=LI[:], op=ALU.mult), ["PWI", "LI"], ["zt3"])
                V_(lambda e: e.tensor_tensor(out=cfr[:], in0=cfr[:], in1=zt3[:], op=ALU.add), ["cfr", "zt3"], ["cfr"])
                V_(lambda e: e.tensor_tensor(out=cfr[:], in0=cfr[:], in1=zt2[:], op=ALU.mult), ["cfr", "zt2"], ["cfr"])
                V_(lambda e: e.tensor_tensor(out=cfi[:], in0=abi, in1=LR[:], op=ALU.mult), ["PWI", "LR"], ["cfi"])
                V_(lambda e: e.tensor_tensor(out=zt3[:], in0=zt1[:], in1=LI[:], op=ALU.mult), ["zt1", "LI"], ["zt3"])
                V_(lambda e: e.tensor_tensor(out=cfi[:], in0=cfi[:], in1=zt3[:], op=ALU.subtract), ["cfi", "zt3"], ["cfi"])
                V_(lambda e: e.tensor_tensor(out=cfi[:], in0=cfi[:], in1=zt2[:], op=ALU.mult), ["cfi", "zt2"], ["cfi"])
                bh = lambda a: a.unsqueeze(2).to_broadcast([128, 16, 16])
                V_(lambda e: e.tensor_tensor(out=BBR[:], in0=bh(cfr[:]), in1=BR[:], op=ALU.mult), ["cfr", "BR"], ["BBR"])
                V_(lambda e: e.tensor_tensor(out=tb[:], in0=bh(cfi[:]), in1=BI[:], op=ALU.mult), ["cfi", "BI"], ["tb"])
                V_(lambda e: e.tensor_tensor(out=BBR[:], in0=BBR[:], in1=tb[:], op=ALU.subtract), ["BBR", "tb"], ["BBR"])
                V_(lambda e: e.tensor_tensor(out=BBI[:], in0=bh(cfr[:]), in1=BI[:], op=ALU.mult), ["cfr", "BI"], ["BBI"])
                V_(lambda e: e.tensor_tensor(out=tb[:], in0=bh(cfi[:]), in1=BR[:], op=ALU.mult), ["cfi", "BR"], ["tb"])
                V_(lambda e: e.tensor_tensor(out=BBI[:], in0=BBI[:], in1=tb[:], op=ALU.add), ["BBI", "tb"], ["BBI"])
                pwb = lambda a, lo: a[:, :, lo:lo + 8].unsqueeze(3).to_broadcast([128, 16, 8, 16])
                b4 = lambda a: a.unsqueeze(2).to_broadcast([128, 16, 8, 16])
                V_(lambda e: e.tensor_tensor(out=HR[:], in0=pwb(PWR, 0), in1=b4(BBR[:]), op=ALU.mult), ["PWR", "BBR"], ["HR"])
                V_(lambda e: e.tensor_tensor(out=tq[:], in0=pwb(PWI, 0), in1=b4(BBI[:]), op=ALU.mult), ["PWI", "BBI"], ["tq"])
                V_(lambda e: e.tensor_tensor(out=HR[:], in0=HR[:], in1=tq[:], op=ALU.subtract), ["HR", "tq"], ["HR"])
                V_(lambda e: e.tensor_tensor(out=HI[:], in0=pwb(PWR, 0), in1=b4(BBI[:]), op=ALU.mult), ["PWR", "BBI"], ["HI"])
                V_(lambda e: e.tensor_tensor(out=tq[:], in0=pwb(PWI, 0), in1=b4(BBR[:]), op=ALU.mult), ["PWI", "BBR"], ["tq"])
                V_(lambda e: e.tensor_tensor(out=HI[:], in0=HI[:], in1=tq[:], op=ALU.add), ["HI", "tq"], ["HI"])
                f3 = lambda a: a.rearrange("p q i h -> p q (i h)")
                a8 = lambda a: a[:, :, 16:17].to_broadcast([128, 16, 128])
                V_(lambda e: e.tensor_tensor(out=f3(GR[:]), in0=a8(PWR), in1=f3(HR[:]), op=ALU.mult), ["PWR", "HR"], ["GR"])
                V_(lambda e: e.tensor_tensor(out=f3(tq[:]), in0=a8(PWI), in1=f3(HI[:]), op=ALU.mult), ["PWI", "HI"], ["tq"])
                V_(lambda e: e.tensor_tensor(out=GR[:], in0=GR[:], in1=tq[:], op=ALU.subtract), ["GR", "tq"], ["GR"])
                V_(lambda e: e.tensor_tensor(out=f3(GI[:]), in0=a8(PWR), in1=f3(HI[:]), op=ALU.mult), ["PWR", "HI"], ["GI"])
                V_(lambda e: e.tensor_tensor(out=f3(tq[:]), in0=a8(PWI), in1=f3(HR[:]), op=ALU.mult), ["PWI", "HR"], ["tq"])
                V_(lambda e: e.tensor_tensor(out=GI[:], in0=GI[:], in1=tq[:], op=ALU.add), ["GI", "tq"], ["GI"])
                V_(lambda e: e.tensor_tensor(out=MOR[:], in0=pwb(PWR, 9), in1=b4(CR[:]), op=ALU.mult), ["PWR", "CR"], ["MOR"])
                V_(lambda e: e.tensor_tensor(out=tq[:], in0=pwb(PWI, 9), in1=b4(CI[:]), op=ALU.mult), ["PWI", "CI"], ["tq"])
                V_(lambda e: e.tensor_tensor(out=MOR[:], in0=MOR[:], in1=tq[:], op=ALU.subtract), ["MOR", "tq"], ["MOR"])
                V_(lambda e: e.tensor_tensor(out=MOI[:], in0=pwb(PWI, 9), in1=b4(CR[:]), op=ALU.mult), ["PWI", "CR"], ["MOI"])
                V_(lambda e: e.tensor_tensor(out=tq[:], in0=pwb(PWR, 9), in1=b4(CI[:]), op=ALU.mult), ["PWR", "CI"], ["tq"])
                V_(lambda e: e.tensor_tensor(out=MOI[:], in0=MOI[:], in1=tq[:], op=ALU.add), ["MOI", "tq"], ["MOI"])
                V_(lambda e: e.tensor_scalar(out=MOI[:], in0=MOI[:], scalar1=-1.0, scalar2=None, op0=ALU.mult), ["MOI"], ["MOI"])
                V_(lambda e: e.memset(MoRp[:], 0.0), [], ["MoRp"], "pool")
                V_(lambda e: e.memset(MoIp[:], 0.0), [], ["MoIp"], "pool")
                for m in range(2):
                    rows = slice(64 * m, 64 * m + 64)
                    V_(lambda e, rows=rows, m=m: e.tensor_copy(out=MoRp[rows, :, m, :], in_=f3(MOR[:])[rows, :, :]), ["MOR", "MoRp"], ["MoRp"])
                    V_(lambda e, rows=rows, m=m: e.tensor_copy(out=MoIp[rows, :, m, :], in_=f3(MOI[:])[rows, :, :]), ["MOI", "MoIp"], ["MoIp"])
                V_(lambda e: e.memset(msk[:], 1.0), [], ["msk"], "pool")
                V_(lambda e: e.affine_select(out=msk[:], in_=msk[:], pattern=[[16, 8], [0, 16]], compare_op=ALU.is_ge,
                                             fill=0.0, base=15, channel_multiplier=-1), ["msk"], ["msk"], "pool")
                for g in range(32):
                    q, m = g // 2, g % 2
                    rows = slice(64 * m, 64 * m + 64)
                    ps = ps_s0[g % 4]
                    pb = B("ps_s0_%d" % (g % 4))
                    O("pe", lambda e, ps=ps, rows=rows, q=q: e.transpose(out=ps[:, 0:64], in_=f3(GR[:])[rows, q, :], identity=identf[rows, rows]),
                      reads=[B("GR"), B("identf")], writes=[pb])
                    O("pe", lambda e, ps=ps, rows=rows, q=q: e.transpose(out=ps[:, 64:128], in_=f3(GI[:])[rows, q, :], identity=identf[rows, rows]),
                      reads=[B("GI"), B("identf")], writes=[pb])
                    O("pe", lambda e, ps=ps, rows=rows, q=q: e.matmul(ps[:, 128:256], lhsT=f3(HR[:])[rows, q, :], rhs=f3(MOR[:])[rows, q, :], start=True, stop=False),
                      reads=[B("HR"), B("MOR")], writes=[pb])
                    O("pe", lambda e, ps=ps, rows=rows, q=q: e.matmul(ps[:, 128:256], lhsT=f3(HI[:])[rows, q, :], rhs=f3(MOI[:])[rows, q, :], start=False, stop=True),
                      reads=[B("HI"), B("MOI")], writes=[pb])
                    O("act", lambda e, ps=ps, g=g: e.activation(func=AF.Identity, out=MinR[:, g, :], in_=ps[:, 0:64]), reads=[pb], writes=[B("MinR")])
                    O("act", lambda e, ps=ps, g=g: e.activation(func=AF.Identity, out=MinI[:, g, :], in_=ps[:, 64:128]), reads=[pb], writes=[B("MinI")])
                    O("dve", lambda e, ps=ps: e.tensor_tensor(out=tmi[:], in0=ps[:, 128:256], in1=msk[:].rearrange("p j h -> p (j h)"), op=ALU.mult),
                      reads=[pb, B("msk")], writes=[B("tmi")])
                    O("dve", lambda e, g=g: e.scalar_tensor_tensor(out=MI[:, g, :], in0=identf[:], scalar=DS[:, g:g + 1], in1=tmi[:], op0=ALU.mult, op1=ALU.add),
                      reads=[B("identf"), B("DS"), B("tmi")], writes=[B("MI")])
                if STOP_AFTER == "S0":
                    d1 = O("sp", lambda e: e.dma_start(out=dbgb_d[:, 0:4096], in_=MI[:].rearrange("p g f -> p (g f)")), reads=[B("MI")], dma_key="dbg1")
                    d2 = O("sp", lambda e: e.dma_start(out=dbgb_d[:, 4096:6144], in_=MinR[:].rearrange("p g f -> p (g f)")), reads=[B("MinR")], dma_key="dbg2")
                    d3 = O("sp", lambda e: e.dma_start(out=dbgb_d[:, 6144:8192], in_=MoRp[:, 0:8, :, :].rearrange("p q m f -> p (q m f)")), reads=[B("MoRp")], dma_key="dbg3")
                    d4 = O("sp", lambda e: e.dma_start(out=dbgf_d[:, 0:272], in_=PWR[:].rearrange("p q n -> p (q n)")), reads=[B("PWR")], dma_key="dbg4")
                    d5 = O("sp", lambda e: e.dma_start(out=dbgf_d[:, 272:544], in_=PWI[:].rearrange("p q n -> p (q n)")), reads=[B("PWI")], dma_key="dbg5")
                    d6 = O("sp", lambda e: e.dma_start(out=dbgf_d[:, 544:2592], in_=HR[:].rearrange("p q i h -> p (q i h)")), reads=[B("HR")], dma_key="dbg6")
                    pg.emit(final_wait_ops=[d1, d2, d3, d4, d5, d6])
                    return nc
            pg.barrier()

            with contextlib.ExitStack() as s1:
                Zt = T(s1, "Zt", [128, 4, 32, 8, 16], BF16)
                wS = T(s1, "wS", [128, 8, 512], BF16)
                wK = T(s1, "wK", [128, 8, 512], BF16)
                wV = T(s1, "wV", [128, 8, 512], BF16)
                wG = T(s1, "wG", [128, 4, 512], BF16)
                for dc in range(8):
                    rows = slice(dc * 128, (dc + 1) * 128)
                    load("pool", wS[:, dc, :], w_in_d[rows, 0:512], "wS", "wS")
                    load("pool", wK[:, dc, :], w_in_d[rows, 1024:1536], "wK", "wK")
                    load("pool", wV[:, dc, :], w_in_d[rows, 1536:2048], "wV", "wV")
                for fc in range(4):
                    load("pool", wG[:, fc, :], w_glu_d[fc * 128:(fc + 1) * 128, :], "wG", "wG")

                if STOP_AFTER == "S1a0":
                    d1 = O("sp", lambda e: e.dma_start(out=dbgb_d[:, 0:4096], in_=wS[:].rearrange("p a b -> p (a b)")), reads=[B("wS")], dma_key="dbg1")
                    d2 = O("sp", lambda e: e.dma_start(out=dbgb_d[:, 4096:6144], in_=wG[:].rearrange("p a b -> p (a b)")), reads=[B("wG")], dma_key="dbg2")
                    pg.emit(final_wait_ops=[d1, d2])
                    return nc
                with contextlib.ExitStack() as s1a:
                    uT = T(s1a, "uT", [128, 8, 1024], BF16)
                    xt = [T(s1a, "xt%d" % k, [128, D], F32) for k in range(2)]
                    scr = {"sq": T(s1a, "sq", [128, D], BF16)[:], "ss": T(s1a, "ss", [128, 1], F32)[:], "rs": T(s1a, "rs", [128, 1], F32)[:],
                           "xn": T(s1a, "xn", [128, D], F32)[:], "eps": epsb[:]}
                    psT = [PS(s1a, "psT%d" % k, [128, 512], F32) for k in range(2)]
                    psM = [PS(s1a, "psM%d" % k, [128, 512], F32) for k in range(4)]
                    psTb = [B("psT0"), B("psT1")]
                    nmm = [0]

                    def next_psM():
                        k = nmm[0] % 4
                        nmm[0] += 1
                        return psM[k], B("psM%d" % k)

                    x_loads = {}

                    def issue_xload(tt):
                        slot = tt % 2
                        x_loads[tt] = load("sp", xt[slot][:], x_d[tt * 128:(tt + 1) * 128, :], "xt%d" % slot, "xt%d" % slot)

                    issue_xload(0)
                    for s in range(4):
                        for t8 in range(8):
                            tt = s * 8 + t8
                            if tt + 1 < 32:
                                issue_xload(tt + 1)
                            slot = tt % 2
                            rmsnorm_T("S", xt[slot][:], B("xt%d" % slot), g1sb,
                                      lambda dc, t8=t8: uT[:, dc, t8 * 128:(t8 + 1) * 128], B("uT"), psT, psTb, scr, "S")
                        if STOP_AFTER == "S1a1":
                            d1 = O("sp", lambda e: e.dma_start(out=dbgb_d[:, 0:8192], in_=uT[:].rearrange("p a b -> p (a b)")), reads=[B("uT"), B("Zt0"), B("KT"), B("VA"), B("KS")], dma_key="dbg1")
                            pg.emit(final_wait_ops=[d1])
                            return nc
                        for i in range(8):
                            ps, pb = next_psM()
                            for dc in range(8):
                                O("pe", lambda e, ps=ps, dc=dc, i=i: e.matmul(ps[:, :], lhsT=uT[:, dc, :].rearrange("p (c i) -> p i c", i=8)[:, i, :],
                                                                               rhs=wS[:, dc, :], start=(dc == 0), stop=(dc == 7)),
                                  reads=[B("uT"), B("wS")], writes=[pb])
                            eng = "act" if i % 2 == 0 else "dve"
                            if eng == "act":
                                O("act", lambda e, ps=ps, s=s, i=i: e.activation(func=AF.Identity, out=Zt[:, s, :, i, :], in_=ps[:, :].rearrange("p (g h) -> p g h", g=32)), reads=[pb], writes=[B("Zt%d" % q_) for q_ in range(16)])
                            else:
                                O("dve", lambda e, ps=ps, s=s, i=i: e.tensor_copy(out=Zt[:, s, :, i, :], in_=ps[:, :].rearrange("p (g h) -> p g h", g=32)), reads=[pb], writes=[B("Zt%d" % q_) for q_ in range(16)])
                        if STOP_AFTER == "S1a2":
                            d1 = O("sp", lambda e: e.dma_start(out=dbgb_d[:, 0:8192], in_=uT[:].rearrange("p a b -> p (a b)")), reads=[B("uT"), B("Zt0"), B("KT"), B("VA"), B("KS")], dma_key="dbg1")
                            pg.emit(final_wait_ops=[d1])
                            return nc
                        for hp in range(4):
                            for th in range(2):
                                ps, pb = next_psM()
                                for dc in range(8):
                                    O("pe", lambda e, ps=ps, dc=dc, hp=hp, th=th: e.matmul(ps[:, :], lhsT=wK[:, dc, hp * 128:(hp + 1) * 128],
                                                                                           rhs=uT[:, dc, th * 512:(th + 1) * 512], start=(dc == 0), stop=(dc == 7)),
                                      reads=[B("uT"), B("wK")], writes=[pb])
                                tok0 = s * 1024 + th * 512
                                for bb in range(2):
                                    blk = tok0 // 256 + bb
                                    O("act", lambda e, ps=ps, hp=hp, tok0=tok0, bb=bb, blk=blk: e.activation(
                                        out=KT[:, hp, tok0 + bb * 256:tok0 + (bb + 1) * 256], in_=ps[:, bb * 256:(bb + 1) * 256], func=AF.Copy,
                                        accum_out=KS[:, hp, blk:blk + 1]), reads=[pb], writes=[B("KT"), B("KS")])
                        if STOP_AFTER == "S1a3":
                            d1 = O("sp", lambda e: e.dma_start(out=dbgb_d[:, 0:8192], in_=uT[:].rearrange("p a b -> p (a b)")), reads=[B("uT"), B("Zt0"), B("KT"), B("VA"), B("KS")], dma_key="dbg1")
                            pg.emit(final_wait_ops=[d1])
                            return nc
                        for t8 in range(8):
                            tt = s * 8 + t8
                            ps, pb = next_psM()
                            for dc in range(8):
                                O("pe", lambda e, ps=ps, dc=dc, t8=t8: e.matmul(ps[:, :], lhsT=uT[:, dc, t8 * 128:(t8 + 1) * 128], rhs=wV[:, dc, :],
                                                                                start=(dc == 0), stop=(dc == 7)), reads=[B("uT"), B("wV")], writes=[pb])
                            if t8 % 2 == 0:
                                O("act", lambda e, ps=ps, tt=tt: e.activation(func=AF.Identity, out=VA[:, tt, :, 0:64], in_=ps[:, :].rearrange("p (h d) -> p h d", h=8)), reads=[pb], writes=[B("VA")])
                            else:
                                O("dve", lambda e, ps=ps, tt=tt: e.tensor_copy(out=VA[:, tt, :, 0:64], in_=ps[:, :].rearrange("p (h d) -> p h d", h=8)), reads=[pb], writes=[B("VA")])
                pg.barrier()
                if STOP_AFTER == "S1a":
                    d1 = O("sp", lambda e: e.dma_start(out=dbgb_d[:, 0:4096], in_=KT[:, 1, :]), reads=[B("KT")], dma_key="dbg1")
                    d2 = O("sp", lambda e: e.dma_start(out=dbgb_d[:, 4096:8192], in_=Zt[:, 1, :, :, :].rearrange("p g i h -> p (g i h)")), reads=[B("Zt0")], dma_key="dbg2")
                    d3 = O("sp", lambda e: e.dma_start(out=dbgf_d[:, 0:64], in_=KS[:].rearrange("p a b -> p (a b)")), reads=[B("KS")], dma_key="dbg3")
                    pg.emit(final_wait_ops=[d1, d2, d3])
                    return nc

                with contextlib.ExitStack() as s1b:
                    Ug = [T(s1b, "Ug%d" % m, [128, 512], BF16) for m in range(2)]
                    CI_i = T(s1b, "CIi", [128, 512], I32)
                    CIf = T(s1b, "CIf", [128, 512], F32)
                    ANG = T(s1b, "ANG", [128, 512], F32)
                    sF = T(s1b, "sF", [128, 512], F32)
                    sI = T(s1b, "sI", [128, 512], I32)
                    COSC = T(s1b, "COSC", [128, 512], F32)
                    SINC = T(s1b, "SINC", [128, 512], F32)
                    WR = T(s1b, "WR", [128, 512], F32)
                    WI = T(s1b, "WI", [128, 512], F32)
                    ta = T(s1b, "ta", [128, 512], F32)
                    tbb = T(s1b, "tbb", [128, 512], F32)
                    XPR = T(s1b, "XPR", [128, 516], BF16)
                    XPI = T(s1b, "XPI", [128, 516], BF16)
                    gs = T(s1b, "gs", [128, 512], F32)
                    gu = T(s1b, "gu", [128, 512], F32)
                    psU = PS(s1b, "psU", [128, 1024], BF16)
                    psXr = PS(s1b, "psXr", [128, 512], F32)
                    psXi = PS(s1b, "psXi", [128, 512], F32)
                    psY = [PS(s1b, "psY%d" % m, [128, 512], F32) for m in range(2)]

                    O("pool", lambda e: e.iota(CI_i[:], pattern=[[1, 512]], base=0, channel_multiplier=0), writes=[B("CIi")])
                    O("dve", lambda e: e.tensor_copy(out=CIf[:], in_=CI_i[:]), reads=[B("CIi")], writes=[B("CIf")])
                    O("dve", lambda e: e.memset(XPR[:, 0:1], 0.0), writes=[B("XPR")])
                    O("dve", lambda e: e.memset(XPI[:, 0:1], 0.0), writes=[B("XPI")])

                    def V2(fn, r, w, eng="dve"):
                        return O(eng, fn, reads=[B(n) for n in r], writes=[B(n) for n in w])

                    def sin2(dst, dstbuf, shift):
                        V2(lambda e: e.tensor_scalar(out=sF[:], in0=ANG[:], scalar1=shift, scalar2=1.0 / TWO_PI, op0=ALU.add, op1=ALU.mult), ["ANG"], ["sF"])
                        V2(lambda e: e.tensor_copy(out=sI[:], in_=sF[:]), ["sF"], ["sI"])
                        V2(lambda e: e.tensor_copy(out=sF[:], in_=sI[:]), ["sI"], ["sF"])
                        V2(lambda e: e.tensor_scalar(out=sF[:], in0=sF[:], scalar1=-TWO_PI, scalar2=shift, op0=ALU.mult, op1=ALU.add), ["sF"], ["sF"])
                        V2(lambda e: e.tensor_tensor(out=sF[:], in0=sF[:], in1=ANG[:], op=ALU.add), ["sF", "ANG"], ["sF"])
                        V2(lambda e: e.tensor_scalar(out=sF[:], in0=sF[:], scalar1=3.14159, scalar2=-3.14159, op0=ALU.min, op1=ALU.max), ["sF"], ["sF"])
                        V2(lambda e: e.activation(out=dst, in_=sF[:], func=AF.Sin), ["sF"], [dstbuf], "act")

                    for q in range(16):
                        zb = B("Zt%d" % q)
                        for m in range(2):
                            g = 2 * q + m
                            for s in range(4):
                                O("pe", lambda e, s=s, g=g, m=m: e.transpose(out=psU[:, m * 512 + s * 128: m * 512 + (s + 1) * 128],
                                                                             in_=Zt[:, s, g, :, :].rearrange("p i h -> p (i h)"), identity=identb[:]),
                                  reads=[zb, B("identb")], writes=[B("psU")])
                            if m == 0:
                                O("act", lambda e, m=m: e.activation(func=AF.Identity, out=Ug[m][:], in_=psU[:, m * 512:(m + 1) * 512]), reads=[B("psU")], writes=[B("Ug%d" % m)])
                            else:
                                O("dve", lambda e, m=m: e.tensor_copy(out=Ug[m][:], in_=psU[:, m * 512:(m + 1) * 512]), reads=[B("psU")], writes=[B("Ug%d" % m)])
                        for m in range(2):
                            g = 2 * q + m
                            O("pe", lambda e, m=m, g=g: e.matmul(psXr[64 * m:64 * m + 64, :], lhsT=MinR[:, g, :], rhs=Ug[m][:], start=True, stop=True),
                              reads=[B("MinR"), B("Ug%d" % m)], writes=[B("psXr")])
                            O("pe", lambda e, m=m, g=g: e.matmul(psXi[64 * m:64 * m + 64, :], lhsT=MinI[:, g, :], rhs=Ug[m][:], start=True, stop=True),
                              reads=[B("MinI"), B("Ug%d" % m)], writes=[B("psXi")])
                        V2(lambda e, q=q: e.tensor_scalar(out=ANG[:], in0=CIf[:], scalar1=TH8[:, q:q + 1], scalar2=None, op0=ALU.mult), ["CIf", "TH8"], ["ANG"])
                        sin2(SINC[:], "SINC", 0.0)
                        sin2(COSC[:], "COSC", math.pi / 2)
                        V2(lambda e: e.tensor_tensor(out=ta[:], in0=psXr[:], in1=COSC[:], op=ALU.mult), ["psXr", "COSC"], ["ta"])
                        V2(lambda e: e.tensor_tensor(out=tbb[:], in0=psXi[:], in1=SINC[:], op=ALU.mult), ["psXi", "SINC"], ["tbb"])
                        V2(lambda e: e.tensor_tensor(out=ta[:], in0=ta[:], in1=tbb[:], op=ALU.add), ["ta", "tbb"], ["ta"])
                        V2(lambda e, q=q: e.tensor_tensor_scan(out=WR[:], data0=RHO8[:, q:q + 1].to_broadcast([128, 512]), data1=ta[:], initial=0.0,
                                                                op0=ALU.mult, op1=ALU.add), ["ta", "RHO8"], ["WR"])
                        V2(lambda e: e.tensor_tensor(out=ta[:], in0=psXi[:], in1=COSC[:], op=ALU.mult), ["psXi", "COSC", "WR"], ["ta"])
                        V2(lambda e: e.tensor_tensor(out=tbb[:], in0=psXr[:], in1=SINC[:], op=ALU.mult), ["psXr", "SINC"], ["tbb"])
                        V2(lambda e: e.tensor_tensor(out=ta[:], in0=ta[:], in1=tbb[:], op=ALU.subtract), ["ta", "tbb"], ["ta"])
                        V2(lambda e, q=q: e.tensor_tensor_scan(out=WI[:], data0=RHO8[:, q:q + 1].to_broadcast([128, 512]), data1=ta[:], initial=0.0,
                                                                op0=ALU.mult, op1=ALU.add), ["ta", "RHO8"], ["WI"])
                        V2(lambda e: e.tensor_tensor(out=ta[:], in0=WR[:], in1=COSC[:], op=ALU.mult), ["WR", "COSC", "WI"], ["ta"])
                        V2(lambda e: e.tensor_tensor(out=tbb[:], in0=WI[:], in1=SINC[:], op=ALU.mult), ["WI", "SINC"], ["tbb"])
                        V2(lambda e: e.tensor_tensor(out=XPR[:, 1:513], in0=ta[:], in1=tbb[:], op=ALU.subtract), ["ta", "tbb"], ["XPR"])
                        V2(lambda e: e.tensor_tensor(out=ta[:], in0=WI[:], in1=COSC[:], op=ALU.mult), ["WI", "COSC", "XPR"], ["ta"])
                        V2(lambda e: e.tensor_tensor(out=tbb[:], in0=WR[:], in1=SINC[:], op=ALU.mult), ["WR", "SINC"], ["tbb"])
                        V2(lambda e: e.tensor_tensor(out=XPI[:, 1:513], in0=ta[:], in1=tbb[:], op=ALU.add), ["ta", "tbb"], ["XPI"])
                        for m in range(2):
                            g = 2 * q + m
                            pbY = B("psY%d" % m)
                            for s in range(4):
                                cs = slice(s * 128, (s + 1) * 128)
                                outp = psY[m][:, s * 128:(s + 1) * 128]
                                O("pe", lambda e, outp=outp, cs=cs, g=g, m=m: e.matmul(outp, lhsT=Ug[m][:, cs], rhs=MI[:, g, :], start=True, stop=False),
                                  reads=[B("Ug%d" % m), B("MI")], writes=[pbY])
                                O("pe", lambda e, outp=outp, cs=cs, q=q, m=m: e.matmul(outp, lhsT=XPR[:, cs], rhs=MoRp[:, q, m, :], start=False, stop=False),
                                  reads=[B("XPR"), B("MoRp")], writes=[pbY])
                                O("pe", lambda e, outp=outp, cs=cs, q=q, m=m: e.matmul(outp, lhsT=XPI[:, cs], rhs=MoIp[:, q, m, :], start=False, stop=True),
                                  reads=[B("XPI"), B("MoIp")], writes=[pbY])
                            yv = psY[m][:, :]
                            V2(lambda e, yv=yv: e.activation(out=gs[:], in_=yv, func=AF.Square), ["psY%d" % m], ["gs"], "act")
                            V2(lambda e: e.tensor_scalar(out=gs[:], in0=gs[:], scalar1=0.044715, scalar2=1.0, op0=ALU.mult, op1=ALU.add), ["gs"], ["gs"])
                            V2(lambda e, yv=yv: e.tensor_tensor(out=gu[:], in0=yv, in1=gs[:], op=ALU.mult), ["psY%d" % m, "gs"], ["gu"])
                            V2(lambda e: e.activation(out=gu[:], in_=gu[:], func=AF.Sigmoid, scale=1.5957691216), ["gu"], ["gu"], "act")
                            O("dve", lambda e, yv=yv, g=g: e.tensor_tensor(out=Zt[:, :, g, :, :],
                                                                            in0=yv.rearrange("p (s j h) -> p s j h", s=4, j=8),
                                                                            in1=gu[:].rearrange("p (s j h) -> p s j h", s=4, j=8), op=ALU.mult),
                              reads=[pbY, B("gu")], writes=[zb])
                pg.barrier()

                with contextlib.ExitStack() as s1c:
                    vT = [T(s1c, "vT%d" % k, [128, 4, 1024], BF16) for k in range(2)]
                    yaT = [T(s1c, "yaT%d" % k, [128, 4, 1024], BF16) for k in range(2)]
                    sg = T(s1c, "sg", [128, 512], F32)
                    tmpV = T(s1c, "tmpV", [128, 8, 512], BF16)
                    psV = [PS(s1c, "psV%d" % k, [128, 1024], BF16) for k in range(2)]
                    psG = [PS(s1c, "psG%d" % k, [128, 512], F32) for k in range(2)]
                    zall = [B("Zt%d" % q_) for q_ in range(16)]
                    cnt = 0
                    for s in range(4):
                        sl = s % 2
                        O("pool", lambda e, s=s: e.tensor_copy(out=tmpV[:].rearrange("p j (g h) -> p j g h", g=32), in_=Zt[:, s, :, :, :].rearrange("p g j h -> p j g h")),
                          reads=zall, writes=[B("tmpV")])
                        for fc in range(4):
                            pv = psV[fc % 2]
                            pvb = B("psV%d" % (fc % 2))
                            for j in range(8):
                                O("pe", lambda e, pv=pv, s=s, j=j, fc=fc: e.transpose(out=pv[:, j * 128:(j + 1) * 128], in_=tmpV[:, j, fc * 128:(fc + 1) * 128], identity=identb[:]),
                                  reads=[B("tmpV"), B("identb")], writes=[pvb])
                            dst = vT[sl][:, fc, :].rearrange("p (c j) -> p j c", j=8)
                            srcv = pv[:, :].rearrange("p (j c) -> p j c", j=8)
                            if fc % 2 == 0:
                                O("act", lambda e, dst=dst, srcv=srcv: e.activation(func=AF.Identity, out=dst, in_=srcv), reads=[pvb], writes=[B("vT%d" % sl)])
                            else:
                                O("dve", lambda e, dst=dst, srcv=srcv: e.tensor_copy(out=dst, in_=srcv), reads=[pvb], writes=[B("vT%d" % sl)])
                        for oc in range(4):
                            for th in range(2):
                                pgk = cnt % 2
                                cnt += 1
                                pG = psG[pgk]
                                pGb = B("psG%d" % pgk)
                                ts_ = slice(th * 512, (th + 1) * 512)
                                for fc in range(4):
                                    O("pe", lambda e, pG=pG, fc=fc, oc=oc, ts_=ts_, sl=sl: e.matmul(pG[:, :], lhsT=wG[:, fc, oc * 128:(oc + 1) * 128], rhs=vT[sl][:, fc, ts_],
                                                                                                   start=(fc == 0), stop=(fc == 3)), reads=[B("wG"), B("vT%d" % sl)], writes=[pGb])
                                O("act", lambda e, pG=pG, oc=oc: e.activation(out=sg[:], in_=pG[:, :], func=AF.Sigmoid, bias=bglu[:, oc:oc + 1]),
                                  reads=[pGb, B("bglu")], writes=[B("sg")])
                                O("dve", lambda e, oc=oc, ts_=ts_, sl=sl: e.tensor_tensor(out=yaT[sl][:, oc, ts_], in0=vT[sl][:, oc, ts_], in1=sg[:], op=ALU.mult),
                                  reads=[B("vT%d" % sl), B("sg")], writes=[B("yaT%d" % sl)])
                        for oc in range(4):
                            stp = O("sp", lambda e, oc=oc, s=s, sl=sl: e.dma_start(out=ya_d[oc * 128:(oc + 1) * 128, s * 1024:(s + 1) * 1024], in_=yaT[sl][:, oc, :]),
                                    reads=[B("yaT%d" % sl)], writes=[B("ya_d_%d_%d" % (s, oc))], dma_key="yast%d" % sl)
                    ya_last = [pg.ops["sp"][-1]]
                    ya_stores = [o for o in pg.ops["sp"] if o.is_dma and isinstance(o.dsem, str) and o.dsem.startswith("yast")]
            pg.barrier(dma_ops=ya_stores[-8:])

        if STOP_AFTER == "S":
            pg.emit(final_wait_ops=ya_stores[-8:])
            return nc

        with contextlib.ExitStack() as sA:
            wQ = T(sA, "wQ", [128, 8, 512], BF16)
            wGA = T(sA, "wGA", [128, 8, 1024], BF16)
            wGB = T(sA, "wGB", [128, 8, 1024], BF16)
            wA = T(sA, "wA", [128, 4, 1024], BF16)
            wB = T(sA, "wB", [128, 4, 1024], BF16)
            wO = T(sA, "wO", [128, 8, 1024], BF16)
            g2b = T(sA, "g2b", [128, D], F32)
            load("sp", g2b[:], g2_d.partition_broadcast(128), "g2b", "g2b")
            for dc in range(8):
                rows = slice(dc * 128, (dc + 1) * 128)
                load("pool", wQ[:, dc, :], w_in_d[rows, 512:1024], "wQ", "wQ")
            for dc in range(8 if not DBG_ONLYQ else 0):
                rows = slice(dc * 128, (dc + 1) * 128)
                load("pool", wGA[:, dc, :], w_in_d[rows, 2048:3072], "wGA", "wGA")
                load("pool", wGB[:, dc, :], w_in_d[rows, 3072:4096], "wGB", "wGB")
            for fc in range(4 if not DBG_ONLYQ else 0):
                rows = slice(fc * 128, (fc + 1) * 128)
                load("pool", wA[:, fc, :], w_a_d[rows, :], "wA", "wA")
                load("pool", wB[:, fc, :], w_b_d[rows, :], "wB", "wB")
            for dc in range(8 if not DBG_ONLYQ else 0):
                rows = slice(dc * 128, (dc + 1) * 128)
                load("pool", wO[:, dc, :], w_out_d[rows, :], "wO", "wO")

            xq = [T(sA, "xq%d" % k, [128, 2, D], F32) for k in range(2)]
            yaq = [T(sA, "yaq%d" % k, [128, 4, 256], BF16) for k in range(2)]
            uTq = T(sA, "uTq", [128, 8, 256], BF16)
            scrA = {"sq": T(sA, "sqA", [128, D], BF16)[:], "ss": T(sA, "ssA", [128, 1], F32)[:], "rs": T(sA, "rsA", [128, 1], F32)[:],
                    "xn": T(sA, "xnA", [128, D], F32)[:], "eps": epsb[:]}
            QT = T(sA, "QT", [128, 4, 256], BF16)
            QTf = T(sA, "QTf", [128, 4, 256], F32)
            gate = T(sA, "gate", [128, 2, 8, 16], F32)
            top8 = T(sA, "top8", [128, 2, 8, 8], F32)
            sel = T(sA, "sel", [128, 2, 8, 16], F32)
            trim = T(sA, "trim", [128, 2, 256], BF16)
            PT = [T(sA, "PT%d" % k, [128, 2, 256], BF16) for k in range(3)]
            acc = T(sA, "acc", [128, 2, 8, 65], F32)
            rden = T(sA, "rden", [128, 2, 8], F32)
            ybt = T(sA, "ybt", [128, 2, 512], BF16)
            ybT = T(sA, "ybT", [128, 4, 256], BF16)
            sga = T(sA, "sga", [128, 256], F32)
            sgb = T(sA, "sgb", [128, 256], F32)
            tma = T(sA, "tma", [128, 256], F32)
            mT = T(sA, "mT", [128, 8, 256], BF16)
            mg = scrA["xn"]
            ssM = T(sA, "ssM", [128, 2], F32)
            rsM = T(sA, "rsM", [128, 1], F32)
            psT = [PS(sA, "psTA%d" % k, [128, 512], F32) for k in range(2)]
            psS = [PS(sA, "psS%d" % k, [128, 512], F32) for k in range(2)]
            psO = [PS(sA, "psO%d" % k, [128, 512], F32) for k in range(2)]
            psW = [PS(sA, "psW%d" % k, [128, 512], F32) for k in range(2)]
            psTb = [B("psTA0"), B("psTA1")]

            O("pool", lambda e: e.memset(trim[:], 1.0), writes=[B("trim")])
            for kh in range(2):
                O("pool", lambda e, kh=kh: e.affine_select(out=trim[:, kh, :], in_=trim[:, kh, :], pattern=[[1, 256]], compare_op=ALU.is_ge,
                                                           fill=0.0, base=-128 * kh, channel_multiplier=-1), reads=[B("trim")], writes=[B("trim")])

            def issue_A_loads(i):
                sl = i % 2
                for qh in range(2):
                    tt = 2 * i + qh
                    load("sp", xq[sl][:, qh, :], x_d[tt * 128:(tt + 1) * 128, :], "xq%d" % sl, "xq%d" % sl)
                if not DBG_NOYA:
                    for fc in range(4):
                        load("sp", yaq[sl][:, fc, :], ya_d[fc * 128:(fc + 1) * 128, i * 256:(i + 1) * 256], "yaq%d" % sl, "yaq%d" % sl)

            if STOP_AFTER == "A0":
                pg.emit(final_wait_ops=[])
                return nc
            issue_A_loads(0)
            ptc = [0]
            h1_stores = []
            for i in range(DBG_NBLK):
                sl = i % 2
                if i + 1 < DBG_NBLK:
                    issue_A_loads(i + 1)
                for qh in range(2):
                    rmsnorm_T("A", xq[sl][:, qh, :], B("xq%d" % sl), g1sb,
                              lambda dc, qh=qh: uTq[:, dc, qh * 128:(qh + 1) * 128], B("uTq"), psT, psTb, scrA, "A")
                if STOP_AFTER == "A1":
                    pg.emit(final_wait_ops=[])
                    return nc
                for hp in range(4):
                    ps = psS[hp % 2]
                    pb = B("psS%d" % (hp % 2))
                    for dc in range(8):
                        O("pe", lambda e, ps=ps, dc=dc, hp=hp: e.matmul(ps[:, 0:256], lhsT=wQ[:, dc, hp * 128:(hp + 1) * 128], rhs=uTq[:, dc, :],
                                                                        start=(dc == 0), stop=(dc == 7)), reads=[B("wQ"), B("uTq")], writes=[pb])
                    O("dve", lambda e, ps=ps, hp=hp: e.tensor_copy(out=QTf[:, hp, :], in_=ps[:, 0:256]), reads=[pb], writes=[B("QTf")])
                    O("act", lambda e, hp=hp: e.activation(func=AF.Identity, out=QT[:, hp, :], in_=QTf[:, hp, :]), reads=[B("QTf")], writes=[B("QT")])
                if STOP_AFTER == "A2":
                    pg.emit(final_wait_ops=[])
                    return nc
                use_sel = i >= 4
                if use_sel:
                    for qh in range(2):
                        pgt = psO[qh]
                        pgb = B("psO%d" % qh)
                        for h in range(8):
                            hp, par = h // 2, h % 2
                            rows = slice(64 * par, 64 * par + 64)
                            O("pe", lambda e, pgt=pgt, h=h, hp=hp, rows=rows, qh=qh: e.matmul(pgt[:, h * 16:(h + 1) * 16], lhsT=QTf[rows, hp, qh * 128:(qh + 1) * 128],
                                                                                            rhs=KS[rows, hp, :], start=True, stop=True),
                              reads=[B("QTf"), B("KS")], writes=[pgb])
                        O("dve", lambda e, qh=qh: e.memset(gate[:, qh, :, :], NEG), writes=[B("gate")])
                        O("dve", lambda e, qh=qh, pgt=pgt, i=i: e.tensor_copy(out=gate[:, qh, :, 0:i], in_=pgt[:, 0:128].rearrange("p (h j) -> p h j", h=8)[:, :, 0:i]),
                          reads=[pgb], writes=[B("gate")])
                        for h in range(8):
                            O("dve", lambda e, qh=qh, h=h: e.max(out=top8[:, qh, h, :], in_=gate[:, qh, h, :]), reads=[B("gate")], writes=[B("top8")])
                        O("dve", lambda e, qh=qh: e.tensor_tensor(out=sel[:, qh, :, :], in0=gate[:, qh, :, :], in1=top8[:, qh, :, 2:3].to_broadcast([128, 8, 16]), op=ALU.is_ge),
                          reads=[B("gate"), B("top8")], writes=[B("sel")])
                for h in range(8):
                    hp, par = h // 2, h % 2
                    rows = slice(64 * par, 64 * par + 64)
                    for j in range(i + 1):
                        k3 = ptc[0] % 3
                        k2 = ptc[0] % 2
                        ptc[0] += 1
                        pS = psS[k2]
                        pSb = B("psS%d" % k2)
                        pt = PT[k3]
                        ptb = B("PT%d" % k3)
                        for kh in range(2):
                            key0 = j * 256 + kh * 128
                            O("pe", lambda e, pS=pS, kh=kh, rows=rows, hp=hp, key0=key0: e.matmul(pS[:, kh * 256:(kh + 1) * 256], lhsT=KT[rows, hp, key0:key0 + 128],
                                                                                                 rhs=QT[rows, hp, :], start=True, stop=True),
                              reads=[B("KT"), B("QT")], writes=[pSb])
                        if "noexp" in DBG_QV:
                            continue
                        O("act", lambda e, pS=pS, pt=pt: e.activation(out=pt[:].rearrange("p a b -> p (a b)"), in_=pS[:, :], func=AF.Exp, scale=0.125),
                          reads=[pSb], writes=[ptb])
                        if j == i and "nomask" not in DBG_QV:
                            O("pool", lambda e, pt=pt: e.tensor_tensor(out=pt[:], in0=pt[:], in1=trim[:], op=ALU.mult), reads=[ptb, B("trim")], writes=[ptb])
                        if "nopv" in DBG_QV:
                            continue
                        pO = psO[k2]
                        pOb = B("psO%d" % k2)
                        for qh in range(2):
                            for kh in range(2):
                                if j == i and kh == 1 and qh == 0:
                                    continue
                                first = (kh == 0)
                                last = (kh == 1) or (j == i and qh == 0)
                                O("pe", lambda e, pO=pO, qh=qh, kh=kh, pt=pt, j=j, h=h, first=first, last=last: e.matmul(
                                    pO[:, qh * 128:qh * 128 + 66], lhsT=pt[:, kh, qh * 128:(qh + 1) * 128], rhs=VA[:, 2 * j + kh, h, :], start=first, stop=last),
                                  reads=[ptb, B("VA"), B("VAones"), B("VAzeros")], writes=[pOb])
                        for qh in range(2):
                            src = pO[:, qh * 128:qh * 128 + 65]
                            dsta = acc[:, qh, h, :]
                            if j == 0:
                                if use_sel and j < i:
                                    O("dve", lambda e, src=src, dsta=dsta, qh=qh, h=h, j=j: e.tensor_scalar(out=dsta, in0=src, scalar1=sel[:, qh, h, j:j + 1], scalar2=None, op0=ALU.mult),
                                      reads=[pOb, B("sel")], writes=[B("acc")])
                                else:
                                    O("dve", lambda e, src=src, dsta=dsta: e.tensor_copy(out=dsta, in_=src), reads=[pOb], writes=[B("acc")])
                            else:
                                if use_sel and j < i:
                                    O("dve", lambda e, src=src, dsta=dsta, qh=qh, h=h, j=j: e.scalar_tensor_tensor(out=dsta, in0=src, scalar=sel[:, qh, h, j:j + 1], in1=dsta,
                                                                                                                      op0=ALU.mult, op1=ALU.add),
                                      reads=[pOb, B("sel"), B("acc")], writes=[B("acc")])
                                else:
                                    O("dve", lambda e, src=src, dsta=dsta: e.tensor_tensor(out=dsta, in0=src, in1=dsta, op=ALU.add), reads=[pOb, B("acc")], writes=[B("acc")])
                if STOP_AFTER == "A3":
                    pg.emit(final_wait_ops=[])
                    return nc
                for qh in range(2):
                    O("dve", lambda e, qh=qh: e.reciprocal(out=rden[:, qh, :], in_=acc[:, qh, :, 64]), reads=[B("acc")], writes=[B("rden")])
                    O("dve", lambda e, qh=qh: e.tensor_tensor(out=ybt[:, qh, :].rearrange("p (h d) -> p h d", h=8), in0=acc[:, qh, :, 0:64],
                                                              in1=rden[:, qh, :].unsqueeze(2).to_broadcast([128, 8, 64]), op=ALU.mult),
                      reads=[B("acc"), B("rden")], writes=[B("ybt")])
                pvT = psT[0]
                for qh in range(2):
                    for fc in range(4):
                        O("pe", lambda e, qh=qh, fc=fc: e.transpose(out=pvT[:].bitcast(BF16)[:, (qh * 4 + fc) * 128:(qh * 4 + fc + 1) * 128],
                                                                     in_=ybt[:, qh, fc * 128:(fc + 1) * 128], identity=identb[:]),
                          reads=[B("ybt"), B("identb")], writes=[psTb[0]])
                for qh in range(2):
                    O("act", lambda e, qh=qh: e.activation(func=AF.Identity, out=ybT[:, :, qh * 128:(qh + 1) * 128], in_=pvT[:].bitcast(BF16)[:, qh * 512:(qh + 1) * 512].rearrange("p (f t) -> p f t", f=4)),
                      reads=[psTb[0]], writes=[B("ybT")])
                if STOP_AFTER == "A4":
                    pg.emit(final_wait_ops=[])
                    return nc
                for oc in range(8):
                    ocs = slice(oc * 128, (oc + 1) * 128)
                    pw = psW[oc % 2]
                    pwbuf = B("psW%d" % (oc % 2))
                    pq = psS[oc % 2]
                    pqb = B("psS%d" % (oc % 2))
                    for fc in range(4):
                        O("pe", lambda e, pw=pw, fc=fc, ocs=ocs, sl=sl: e.matmul(pw[:, 0:256], lhsT=wA[:, fc, ocs], rhs=yaq[sl][:, fc, :], start=(fc == 0), stop=(fc == 3)),
                          reads=[B("wA"), B("yaq%d" % sl)], writes=[pwbuf])
                    for fc in range(4):
                        O("pe", lambda e, pw=pw, fc=fc, ocs=ocs: e.matmul(pw[:, 256:512], lhsT=wB[:, fc, ocs], rhs=ybT[:, fc, :], start=(fc == 0), stop=(fc == 3)),
                          reads=[B("wB"), B("ybT")], writes=[pwbuf])
                    for dc in range(8):
                        O("pe", lambda e, pq=pq, dc=dc, ocs=ocs: e.matmul(pq[:, 0:256], lhsT=wGA[:, dc, ocs], rhs=uTq[:, dc, :], start=(dc == 0), stop=(dc == 7)),
                          reads=[B("wGA"), B("uTq")], writes=[pqb])
                    for dc in range(8):
                        O("pe", lambda e, pq=pq, dc=dc, ocs=ocs: e.matmul(pq[:, 256:512], lhsT=wGB[:, dc, ocs], rhs=uTq[:, dc, :], start=(dc == 0), stop=(dc == 7)),
                          reads=[B("wGB"), B("uTq")], writes=[pqb])
                    O("act", lambda e, pq=pq: e.activation(out=sga[:], in_=pq[:, 0:256], func=AF.Sigmoid), reads=[pqb], writes=[B("sga")])
                    O("act", lambda e, pq=pq: e.activation(out=sgb[:], in_=pq[:, 256:512], func=AF.Sigmoid), reads=[pqb], writes=[B("sgb")])
                    O("dve", lambda e, pw=pw: e.tensor_tensor(out=tma[:], in0=pw[:, 0:256], in1=sga[:], op=ALU.mult), reads=[pwbuf, B("sga")], writes=[B("tma")])
                    O("dve", lambda e, pw=pw: e.tensor_tensor(out=sgb[:], in0=pw[:, 256:512], in1=sgb[:], op=ALU.mult), reads=[pwbuf, B("sgb")], writes=[B("sgb")])
                    O("dve", lambda e, oc=oc: e.tensor_tensor(out=mT[:, oc, :], in0=tma[:], in1=sgb[:], op=ALU.add), reads=[B("tma"), B("sgb")], writes=[B("mT")])
                if STOP_AFTER == "A5":
                    pg.emit(final_wait_ops=[])
                    return nc
                for qh in range(2):
                    tt = 2 * i + qh
                    for ch in range(2):
                        pw = psW[ch]
                        pwbuf = B("psW%d" % ch)
                        for dc in range(8):
                            O("pe", lambda e, pw=pw, dc=dc, ch=ch, qh=qh: e.matmul(pw[:, :], lhsT=mT[:, dc, qh * 128:(qh + 1) * 128], rhs=wO[:, dc, ch * 512:(ch + 1) * 512],
                                                                                   start=(dc == 0), stop=(dc == 7)), reads=[B("mT"), B("wO")], writes=[pwbuf])
                        O("act", lambda e, pw=pw, ch=ch: e.activation(out=scrA["sq"][:, ch * 512:(ch + 1) * 512], in_=pw[:, :], func=AF.Square, accum_out=ssM[:, ch:ch + 1]),
                          reads=[pwbuf], writes=[B("Asq"), B("ssM")])
                        O("dve", lambda e, pw=pw, ch=ch: e.tensor_tensor(out=mg[:, ch * 512:(ch + 1) * 512], in0=pw[:, :], in1=g2b[:, ch * 512:(ch + 1) * 512], op=ALU.mult),
                          reads=[pwbuf, B("g2b"), B("ssM")], writes=[B("Axn")])
                    O("dve", lambda e: e.tensor_tensor(out=rsM[:], in0=ssM[:, 0:1], in1=ssM[:, 1:2], op=ALU.add), reads=[B("ssM")], writes=[B("rsM")])
                    O("act", lambda e: e.activation(out=rsM[:], in_=rsM[:], func=AF.Sqrt, scale=1.0 / D, bias=epsb[:]), reads=[B("rsM"), B("epsb")], writes=[B("rsM")])
                    O("dve", lambda e: e.reciprocal(out=rsM[:], in_=rsM[:]), reads=[B("rsM")], writes=[B("rsM")])
                    hs = sl
                    O("dve", lambda e, qh=qh, sl=sl: e.scalar_tensor_tensor(out=xq[sl][:, qh, :], in0=mg, scalar=rsM[:, 0:1], in1=xq[sl][:, qh, :], op0=ALU.mult, op1=ALU.add),
                      reads=[B("Axn"), B("rsM"), B("xq%d" % sl)], writes=[B("xq%d" % sl)])
                    st_ = O("sp", lambda e, sl=sl, qh=qh, tt=tt: e.dma_start(out=out_d[tt * 128:(tt + 1) * 128, :], in_=xq[sl][:, qh, :]), reads=[B("xq%d" % sl)],
                            writes=[B("out%d" % tt)], dma_key="h1st%d" % hs)
                    h1_stores.append(st_)
        pg.barrier(dma_ops=h1_stores)
        sKV.close()
        if STOP_AFTER == "A":
            pg.emit(final_wait_ops=h1_stores)
            return nc

        with contextlib.ExitStack() as sB:
            w1 = T(sB, "w1", [128, 8, 4096], BF16)
            w2 = T(sB, "w2", [128, 32, D], BF16)
            g4b = T(sB, "g4b", [128, D], F32)
            load("sp", g4b[:], g4_d.partition_broadcast(128), "g4b", "g4b")
            for dc in range(8):
                rows = slice(dc * 128, (dc + 1) * 128)
                for cc in range(4):
                    load("pool", w1[:, dc, cc * 1024:(cc + 1) * 1024], w_ff1_d[rows, cc * 1024:(cc + 1) * 1024], "w1", "w1")
            for fc in range(32):
                rows = slice(fc * 128, (fc + 1) * 128)
                load("pool", w2[:, fc, :], w_ff2_d[rows, :], "w2", "w2")
            hx = [T(sB, "hx%d" % k, [128, 2, D], F32) for k in range(2)]
            fT = T(sB, "fT", [128, 8, 256], BF16)
            hT = T(sB, "hT", [128, 32, 256], BF16)
            rl2 = [T(sB, "rl2_%d" % k, [128, 512], BF16) for k in range(2)]
            scrB = {"sq": T(sB, "sqB", [128, D], BF16)[:], "ss": T(sB, "ssB", [128, 1], F32)[:], "rs": T(sB, "rsB", [128, 1], F32)[:],
                    "xn": T(sB, "xnB", [128, D], F32)[:], "eps": epsb[:]}
            mgB = scrB["xn"]
            ssF = T(sB, "ssF", [128, 2], F32)
            rsF = T(sB, "rsF", [128, 1], F32)
            psT = [PS(sB, "psTB%d" % k, [128, 512], F32) for k in range(2)]
            psH = [PS(sB, "psH%d" % k, [128, 512], F32) for k in range(3)]
            psW = [PS(sB, "psWB%d" % k, [128, 512], F32) for k in range(2)]
            psTb = [B("psTB0"), B("psTB1")]

            def issue_B_loads(gi):
                sl = gi % 2
                for t2 in range(2):
                    tt = gi * 2 + t2
                    load("sp", hx[sl][:, t2, :], out_d[tt * 128:(tt + 1) * 128, :], "hx%d" % sl, "hx%d" % sl)

            if STOP_AFTER == "B0":
                pg.emit(final_wait_ops=[])
                return nc
            issue_B_loads(0)
            hc = 0
            for gi in range(16):
                sl = gi % 2
                if gi + 1 < 16:
                    issue_B_loads(gi + 1)
                for t2 in range(2):
                    rmsnorm_T("B", hx[sl][:, t2, :], B("hx%d" % sl), g3sb,
                              lambda dc, t2=t2: fT[:, dc, t2 * 128:(t2 + 1) * 128], B("fT"), psT, psTb, scrB, "B", gbuf="gsb3")
                if STOP_AFTER == "B1":
                    pg.emit(final_wait_ops=[])
                    return nc
                for fc2 in range(16):
                    ph = psH[hc % 3]
                    phb = B("psH%d" % (hc % 3))
                    rlk = hc % 2
                    hc += 1
                    for u in range(2):
                        fc = fc2 * 2 + u
                        for dc in range(8):
                            O("pe", lambda e, ph=ph, dc=dc, fc=fc, u=u: e.matmul(ph[:, u * 256:(u + 1) * 256], lhsT=w1[:, dc, fc * 128:(fc + 1) * 128], rhs=fT[:, dc, :],
                                                                                 start=(dc == 0), stop=(dc == 7)), reads=[B("w1"), B("fT")], writes=[phb])
                    O("act", lambda e, ph=ph, rlk=rlk: e.activation(out=rl2[rlk][:], in_=ph[:, :], func=AF.Relu), reads=[phb], writes=[B("rl2_%d" % rlk)])
                    O("dve", lambda e, ph=ph, fc2=fc2, rlk=rlk: e.scalar_tensor_tensor(out=hT[:, fc2 * 2:fc2 * 2 + 2, :].rearrange("p a b -> p (a b)"), in0=ph[:, :], scalar=0.0,
                                                                                         in1=rl2[rlk][:], op0=ALU.max, op1=ALU.mult),
                      reads=[phb, B("rl2_%d" % rlk)], writes=[B("hT")])
                if STOP_AFTER == "B2":
                    pg.emit(final_wait_ops=[])
                    return nc
                for t2 in range(2):
                    tt = gi * 2 + t2
                    for ch in range(2):
                        pw = psW[ch]
                        pwbuf = B("psWB%d" % ch)
                        for fc in range(32):
                            O("pe", lambda e, pw=pw, fc=fc, ch=ch, t2=t2: e.matmul(pw[:, :], lhsT=hT[:, fc, t2 * 128:(t2 + 1) * 128], rhs=w2[:, fc, ch * 512:(ch + 1) * 512],
                                                                                   start=(fc == 0), stop=(fc == 31)), reads=[B("hT"), B("w2")], writes=[pwbuf])
                        O("act", lambda e, pw=pw, ch=ch: e.activation(out=scrB["sq"][:, ch * 512:(ch + 1) * 512], in_=pw[:, :], func=AF.Square, accum_out=ssF[:, ch:ch + 1]),
                          reads=[pwbuf], writes=[B("Bsq"), B("ssF")])
                        O("dve", lambda e, pw=pw, ch=ch: e.tensor_tensor(out=mgB[:, ch * 512:(ch + 1) * 512], in0=pw[:, :], in1=g4b[:, ch * 512:(ch + 1) * 512], op=ALU.mult),
                          reads=[pwbuf, B("g4b"), B("ssF")], writes=[B("Bxn")])
                    O("dve", lambda e: e.tensor_tensor(out=rsF[:], in0=ssF[:, 0:1], in1=ssF[:, 1:2], op=ALU.add), reads=[B("ssF")], writes=[B("rsF")])
                    O("act", lambda e: e.activation(out=rsF[:], in_=rsF[:], func=AF.Sqrt, scale=1.0 / D, bias=epsb[:]), reads=[B("rsF"), B("epsb")], writes=[B("rsF")])
                    O("dve", lambda e: e.reciprocal(out=rsF[:], in_=rsF[:]), reads=[B("rsF")], writes=[B("rsF")])
                    O("dve", lambda e, t2=t2, sl=sl: e.scalar_tensor_tensor(out=hx[sl][:, t2, :], in0=mgB, scalar=rsF[:, 0:1], in1=hx[sl][:, t2, :], op0=ALU.mult, op1=ALU.add),
                      reads=[B("Bxn"), B("rsF"), B("hx%d" % sl)], writes=[B("hx%d" % sl)])
                    st_ = O("sp", lambda e, sl=sl, t2=t2, tt=tt: e.dma_start(out=out_d[tt * 128:(tt + 1) * 128, :], in_=hx[sl][:, t2, :]), reads=[B("hx%d" % sl)],
                            writes=[B("out%d" % tt)], dma_key="ost%d" % sl)
                    final_stores.append(st_)
        pg.emit(final_wait_ops=final_stores)
    return nc


def _prep_shared(inp):
    f = lambda a: np.ascontiguousarray(np.asarray(a, dtype=np.float32))
    lam_re = f(inp["lam_re"])[0]
    lam_im = f(inp["lam_im"])[0]
    log_dt = f(inp["log_dt"])[0]
    b_re = f(inp["b_re"])[0]
    b_im = f(inp["b_im"])[0]
    c_re = f(inp["c_re"])[0]
    c_im = f(inp["c_im"])[0]
    d_skip = f(inp["d_skip"])[0]

    def pq(a):
        return f(a.reshape(16, 2, 64).transpose(1, 2, 0).reshape(128, 16))

    def gv(a):
        return f(a.reshape(8, 128).T)

    sh = {
        "w_in": f(inp["w_in"])[0],
        "w_glu": f(inp["w_glu"])[0],
        "w_a": f(inp["w_branch_a"])[0],
        "w_b": f(inp["w_branch_b"])[0],
        "w_out": f(inp["w_out"])[0],
        "w_ff1": f(inp["w_ff1"])[0],
        "w_ff2": f(inp["w_ff2"])[0],
        "g1": gv(f(inp["g_pre_mix"])[0]),
        "g3": gv(f(inp["g_pre_ffn"])[0]),
        "g2": f(inp["g_post_mix"]).reshape(1, D),
        "g4": f(inp["g_post_ffn"]).reshape(1, D),
        "bglu": f(f(inp["b_glu"])[0].reshape(4, 128).T),
        "LR": pq(lam_re),
        "LI": pq(lam_im),
        "LDT": f(np.broadcast_to(log_dt.reshape(16, 2).T[:, None, :], (2, 64, 16)).reshape(128, 16)),
        "BR": f(b_re.reshape(16, 2, 64, 16).transpose(1, 2, 0, 3).reshape(128, 256)),
        "BI": f(b_im.reshape(16, 2, 64, 16).transpose(1, 2, 0, 3).reshape(128, 256)),
        "CR": f(c_re.reshape(16, 2, 16, 64).transpose(1, 3, 0, 2).reshape(128, 256)),
        "CI": f(c_im.reshape(16, 2, 16, 64).transpose(1, 3, 0, 2).reshape(128, 256)),
        "DS": f(np.tile(d_skip.reshape(32, 16).T, (8, 1))),
    }
    return sh


def kernel(**inputs):
    x = np.asarray(inputs["x"], dtype=np.float32)
    sh = _prep_shared(inputs)
    nc = build_nc()
    in_maps = []
    for b in range(8):
        m = dict(sh)
        m["x"] = np.ascontiguousarray(x[b])
        in_maps.append(m)
    res = run_bass_kernel_spmd(nc, in_maps, core_ids=list(range(8)))
    out = np.stack([np.asarray(res.results[b]["out"], dtype=np.float32) for b in range(8)], axis=0)
    return out
```

```python
import contextlib
import math
import numpy as np
import concourse.bass as bass
import concourse.mybir as mybir
from concourse.bass_utils import run_bass_kernel_spmd

F32 = mybir.dt.float32
BF16 = mybir.dt.bfloat16
I32 = mybir.dt.int32
AF = mybir.ActivationFunctionType
ALU = mybir.AluOpType
AX = mybir.AxisListType

L = 4096
D = 1024
NEG = -1.0e30
EPS = 1e-6
ENGS = ("pe", "act", "dve", "pool", "sp")
STOP_AFTER = None
DBG_NOKS = False
DBG_NBLK = 16
DBG_QV = ""
DBG_ONLYQ = False
POOL_DMA_DEPTH = 8
DBG_NOYA = False
STRICT = False


class Buf:
    __slots__ = ("name", "last_w", "readers")

    def __init__(self, name):
        self.name = name
        self.last_w = None
        self.readers = []


class Op:
    __slots__ = ("eng", "fn", "deps", "idx", "is_dma", "dsem", "dval", "signal", "know")


class Prog:
    def __init__(self, nc):
        self.nc = nc
        self.ops = {e: [] for e in ENGS}
        self.all = []
        self.dma_sems = {}
        self.know = {e: {} for e in ENGS}

    def op(self, eng, fn, reads=(), writes=(), dma_key=None, extra_deps=()):
        o = Op()
        o.eng = eng
        o.fn = fn
        o.is_dma = dma_key is not None
        o.signal = False
        o.idx = len(self.ops[eng])
        cands = []
        for b in reads:
            if b.last_w is not None:
                cands.append((b.last_w, True))
        for b in writes:
            if b.last_w is not None:
                cands.append((b.last_w, False))
            for r in b.readers:
                cands.append((r, False))
        for p in extra_deps:
            cands.append((p, True))
        need = {}
        kn = self.know[eng]
        for (p, raw) in cands:
            if p.is_dma:
                if (not raw) and dma_key is not None and p.dsem == dma_key:
                    continue
                key = ("dma", p.dsem)
                val = p.dval
            else:
                if p.eng == eng and not raw and not STRICT:
                    continue
                key = ("eng", p.eng)
                val = p.idx
            if kn.get(key, -1) >= val:
                continue
            if key not in need or need[key][0] < val:
                need[key] = (val, p)
        o.deps = []
        for key, (val, p) in need.items():
            o.deps.append(p)
            if kn.get(key, -1) < val:
                kn[key] = val
            for k2, v2 in p.know.items():
                if kn.get(k2, -1) < v2:
                    kn[k2] = v2
        if o.is_dma:
            cnt = self.dma_sems.setdefault(dma_key, [0])
            cnt[0] += 1
            o.dsem = dma_key
            o.dval = cnt[0]
        o.know = dict(kn)
        if o.is_dma:
            o.know[("dma", o.dsem)] = o.dval
        else:
            o.know[("eng", eng)] = o.idx
        for b in reads:
            b.readers.append(o)
        for b in writes:
            b.last_w = o
            b.readers = []
        self.ops[eng].append(o)
        self.all.append(o)
        return o

    def barrier(self, dma_ops=()):
        lasts = [self.ops[e][-1] for e in ENGS if self.ops[e] and not self.ops[e][-1].is_dma]
        lasts = []
        for e in ENGS:
            for o in reversed(self.ops[e]):
                if not o.is_dma:
                    lasts.append(o)
                    break
        deps = list(lasts) + list(dma_ops)
        for e in ENGS:
            self.op(e, lambda eng: eng.nop(), extra_deps=deps)

    def emit(self, final_wait_ops=()):
        nc = self.nc
        for o in self.all:
            for p in o.deps:
                if not p.is_dma:
                    p.signal = True
        sigval = {}
        for e in ENGS:
            c = 0
            for o in self.ops[e]:
                if o.signal:
                    c += 1
                sigval[(e, o.idx)] = c
        with contextlib.ExitStack() as st:
            esem = {e: st.enter_context(nc.semaphore("sem_" + e)) for e in ENGS}
            dsem = {k: st.enter_context(nc.semaphore("dsem%d" % i)) for i, k in enumerate(self.dma_sems)}
            block = st.enter_context(nc.Block())
            engobj = {"pe": block.tensor, "act": block.scalar, "dve": block.vector,
                      "pool": block.gpsimd, "sp": block.sync}

            def run(ename, eng):
                for o in self.ops[ename]:
                    for p in o.deps:
                        if p.is_dma:
                            eng.wait_ge(dsem[p.dsem], 16 * p.dval)
                        else:
                            eng.wait_ge(esem[p.eng], sigval[(p.eng, p.idx)])
                    ins = o.fn(eng)
                    if o.is_dma:
                        ins.then_inc(dsem[o.dsem], 16)
                    elif o.signal:
                        ins.then_inc(esem[ename], 1)
                if ename == "sp":
                    fw = {}
                    for p in final_wait_ops:
                        fw[p.dsem] = max(fw.get(p.dsem, 0), p.dval)
                    for k, v in fw.items():
                        eng.wait_ge(dsem[k], 16 * v)

            for ename in ENGS:
                engobj[ename](lambda eng, ename=ename: run(ename, eng))


def build_nc(dbg=False):
    nc = bass.Bass("TRN2", target_bir_lowering=False)

    def din(name, shape, dt=F32):
        return nc.dram_tensor(name, list(shape), dt, kind="ExternalInput").ap()

    x_d = din("x", [L, D])
    w_in_d = din("w_in", [D, 4096])
    w_glu_d = din("w_glu", [512, 512])
    w_a_d = din("w_a", [512, D])
    w_b_d = din("w_b", [512, D])
    w_out_d = din("w_out", [D, D])
    w_ff1_d = din("w_ff1", [D, 4096])
    w_ff2_d = din("w_ff2", [4096, D])
    g1_d = din("g1", [128, 8])
    g3_d = din("g3", [128, 8])
    g2_d = din("g2", [1, D])
    g4_d = din("g4", [1, D])
    bglu_d = din("bglu", [128, 4])
    LR_d = din("LR", [128, 16])
    LI_d = din("LI", [128, 16])
    LDT_d = din("LDT", [128, 16])
    BR_d = din("BR", [128, 256])
    BI_d = din("BI", [128, 256])
    CR_d = din("CR", [128, 256])
    CI_d = din("CI", [128, 256])
    DS_d = din("DS", [128, 32])
    out_d = nc.dram_tensor("out", [L, D], F32, kind="ExternalOutput").ap()
    ya_d = nc.dram_tensor("ya_scr", [512, L], BF16, kind="ExternalOutput" if dbg else "Internal").ap()

    if dbg:
        dbgb_d = nc.dram_tensor("dbgb", [128, 8192], BF16, kind="ExternalOutput").ap()
        dbgf_d = nc.dram_tensor("dbgf", [128, 4096], F32, kind="ExternalOutput").ap()
    pg = Prog(nc)
    O = pg.op
    bufs = {}

    def B(name):
        if name not in bufs:
            bufs[name] = Buf(name)
        return bufs[name]

    final_stores = []
    TWO_PI = 2.0 * math.pi

    with contextlib.ExitStack() as top:
        def T(st, name, shape, dt):
            return st.enter_context(nc.sbuf_tensor("s_" + name, list(shape), dt))

        def PS(st, name, shape, dt):
            return st.enter_context(nc.psum_tensor("p_" + name, list(shape), dt))

        identf = T(top, "identf", [128, 128], F32)
        identb = T(top, "identb", [128, 128], BF16)
        O("pool", lambda e: e.memset(identf[:], 1.0), writes=[B("identf")])
        O("pool", lambda e: e.affine_select(out=identf[:], in_=identf[:], pattern=[[1, 128]], compare_op=ALU.is_equal,
                                            fill=0.0, base=0, channel_multiplier=-1), reads=[B("identf")], writes=[B("identf")])
        O("dve", lambda e: e.tensor_copy(out=identb[:], in_=identf[:]), reads=[B("identf")], writes=[B("identb")])

        pool_dmas = []

        def load(eng, dst, src, bname, key=None, after=()):
            ex = tuple(after)
            o = O(eng, lambda e: e.dma_start(out=dst, in_=src), writes=[B(bname)], dma_key=key or bname, extra_deps=ex)
            if eng == "pool":
                pool_dmas.append(o)
            return o

        def rmsnorm_T(st_name, xt, xbuf, gsb, uT_dst_fn, ubuf, ps_list, psbufs, scr, pfx, gbuf="gsb"):
            sq, ss, rs, xn = scr["sq"], scr["ss"], scr["rs"], scr["xn"]
            O("act", lambda e: e.activation(out=sq, in_=xt, func=AF.Square, accum_out=ss), reads=[xbuf], writes=[B(pfx + "sq"), B(pfx + "ss")])
            O("act", lambda e: e.activation(out=rs, in_=ss, func=AF.Sqrt, scale=1.0 / D, bias=scr["eps"]), reads=[B(pfx + "ss"), B("epsb")], writes=[B(pfx + "rs")])
            O("dve", lambda e: e.reciprocal(out=rs, in_=rs), reads=[B(pfx + "rs")], writes=[B(pfx + "rs")])
            O("dve", lambda e: e.tensor_scalar(out=xn, in0=xt, scalar1=rs, scalar2=None, op0=ALU.mult), reads=[xbuf, B(pfx + "rs")], writes=[B(pfx + "xn")])
            for half in range(2):
                ps = ps_list[half]
                pb = psbufs[half]
                for k in range(4):
                    dc = half * 4 + k
                    O("pe", lambda e, dc=dc, k=k, ps=ps: e.transpose(out=ps[:, k * 128:(k + 1) * 128], in_=xn[:, dc * 128:(dc + 1) * 128], identity=identf[:]),
                      reads=[B(pfx + "xn"), B("identf")], writes=[pb])
                for k in range(4):
                    dc = half * 4 + k
                    eng = "act" if k % 2 == 0 else "dve"
                    if eng == "act":
                        O("act", lambda e, dc=dc, k=k, ps=ps: e.activation(out=uT_dst_fn(dc), in_=ps[:, k * 128:(k + 1) * 128], func=AF.Copy, scale=gsb[:, dc:dc + 1]),
                          reads=[pb, B(gbuf)], writes=[ubuf])
                    else:
                        O("dve", lambda e, dc=dc, k=k, ps=ps: e.tensor_scalar(out=uT_dst_fn(dc), in0=ps[:, k * 128:(k + 1) * 128], scalar1=gsb[:, dc:dc + 1], scalar2=None, op0=ALU.mult),
                          reads=[pb, B(gbuf)], writes=[ubuf])

        epsb = T(top, "epsb", [128, 1], F32)
        O("dve", lambda e: e.memset(epsb[:], EPS), writes=[B("epsb")])
        g1sb = T(top, "g1sb", [128, 8], F32)
        g3sb = T(top, "g3sb", [128, 8], F32)
        bglu = T(top, "bglu", [128, 4], F32)
        load("sp", g1sb[:], g1_d, "gsb", "c0")
        load("sp", g3sb[:], g3_d, "gsb3", "c1")
        load("sp", bglu[:], bglu_d, "bglu", "c2")

        sKV = top.enter_context(contextlib.ExitStack())
        KT = T(sKV, "KT", [128, 4, L], BF16)
        VA = T(sKV, "VA", [128, 32, 8, 66], BF16)
        KS = T(sKV, "KS", [128, 4, 16], F32)
        O("pool", lambda e: e.memset(VA[:, :, :, 64:65], 1.0), writes=[B("VAones")])
        O("pool", lambda e: e.memset(VA[:, :, :, 65:66], 0.0), writes=[B("VAzeros")])

        with contextlib.ExitStack() as sS:
            MI = T(sS, "MI", [128, 32, 128], BF16)
            MinR = T(sS, "MinR", [128, 32, 64], BF16)
            MinI = T(sS, "MinI", [128, 32, 64], BF16)
            MoRp = T(sS, "MoRp", [128, 16, 2, 128], BF16)
            MoIp = T(sS, "MoIp", [128, 16, 2, 128], BF16)
            RHO8 = T(sS, "RHO8", [128, 16], F32)
            TH8 = T(sS, "TH8", [128, 16], F32)

            with contextlib.ExitStack() as s0:
                LR = T(s0, "LR", [128, 16], F32)
                LI = T(s0, "LI", [128, 16], F32)
                DT = T(s0, "DT", [128, 16], F32)
                BR = T(s0, "BR", [128, 16, 16], F32)
                BI = T(s0, "BI", [128, 16, 16], F32)
                CR = T(s0, "CR", [128, 16, 16], F32)
                CI = T(s0, "CI", [128, 16, 16], F32)
                DS = T(s0, "DS", [128, 32], F32)
                NVi = T(s0, "NVi", [128, 17], I32)
                NV = T(s0, "NV", [128, 17], F32)
                LRDT = T(s0, "LRDT", [128, 16], F32)
                TH = T(s0, "TH", [128, 16], F32)
                ARG = T(s0, "ARG", [128, 16, 17], F32)
                ARK = T(s0, "ARK", [128, 16, 17], F32)
                ARKi = T(s0, "ARKi", [128, 16, 17], I32)
                MAG = T(s0, "MAG", [128, 16, 17], F32)
                PWR = T(s0, "PWR", [128, 16, 17], F32)
                PWI = T(s0, "PWI", [128, 16, 17], F32)
                zt1 = T(s0, "zt1", [128, 16], F32)
                zt2 = T(s0, "zt2", [128, 16], F32)
                zt3 = T(s0, "zt3", [128, 16], F32)
                cfr = T(s0, "cfr", [128, 16], F32)
                cfi = T(s0, "cfi", [128, 16], F32)
                BBR = T(s0, "BBR", [128, 16, 16], F32)
                BBI = T(s0, "BBI", [128, 16, 16], F32)
                tb = T(s0, "tb", [128, 16, 16], F32)
                HR = T(s0, "HR", [128, 16, 8, 16], F32)
                HI = T(s0, "HI", [128, 16, 8, 16], F32)
                GR = T(s0, "GR", [128, 16, 8, 16], F32)
                GI = T(s0, "GI", [128, 16, 8, 16], F32)
                MOR = T(s0, "MOR", [128, 16, 8, 16], F32)
                MOI = T(s0, "MOI", [128, 16, 8, 16], F32)
                tq = T(s0, "tq", [128, 16, 8, 16], F32)
                msk = T(s0, "msk", [128, 8, 16], F32)
                tmi = T(s0, "tmi", [128, 128], F32)
                ps_s0 = [PS(s0, "ps_s0_%d" % k, [128, 512], F32) for k in range(4)]

                load("sp", LR[:], LR_d, "LR", "c3")
                load("sp", LI[:], LI_d, "LI", "c4")
                load("sp", DT[:], LDT_d, "DT", "c5")
                load("sp", BR[:].rearrange("p a b -> p (a b)"), BR_d, "BR", "c6")
                load("sp", BI[:].rearrange("p a b -> p (a b)"), BI_d, "BI", "c7")
                load("sp", CR[:].rearrange("p a b -> p (a b)"), CR_d, "CR", "c8")
                load("sp", CI[:].rearrange("p a b -> p (a b)"), CI_d, "CI", "c9")
                load("sp", DS[:], DS_d, "DS", "c10")

                def V_(fn, r, w, eng="dve"):
                    return O(eng, fn, reads=[B(n) for n in r], writes=[B(n) for n in w])

                V_(lambda e: e.iota(NVi[:, 0:8], pattern=[[-1, 8]], base=-1, channel_multiplier=0), [], ["NVi"], "pool")
                V_(lambda e: e.iota(NVi[:, 8:17], pattern=[[1, 9]], base=0, channel_multiplier=0), [], ["NVi"], "pool")
                V_(lambda e: e.tensor_copy(out=NV[:], in_=NVi[:]), ["NVi"], ["NV"])
                V_(lambda e: e.activation(out=DT[:], in_=DT[:], func=AF.Exp), ["DT"], ["DT"], "act")
                V_(lambda e: e.tensor_tensor(out=LRDT[:], in0=LR[:], in1=DT[:], op=ALU.mult), ["LR", "DT"], ["LRDT"])
                V_(lambda e: e.tensor_tensor(out=TH[:], in0=LI[:], in1=DT[:], op=ALU.mult), ["LI", "DT"], ["TH"])
                bc3 = lambda a: a.unsqueeze(2).to_broadcast([128, 16, 17])
                nvb = NV[:].unsqueeze(1).to_broadcast([128, 16, 17])
                V_(lambda e: e.tensor_tensor(out=ARG[:], in0=bc3(LRDT[:]), in1=nvb, op=ALU.mult), ["LRDT", "NV"], ["ARG"])
                V_(lambda e: e.activation(out=MAG[:], in_=ARG[:], func=AF.Exp), ["ARG"], ["MAG"], "act")

                def sin_of(dst, src_fn, srcbufs, dstbuf, shift, shape3):
                    scrF, scrI = shape3
                    V_(lambda e: e.tensor_scalar(out=scrF, in0=src_fn(), scalar1=shift, scalar2=1.0 / TWO_PI, op0=ALU.add, op1=ALU.mult), srcbufs, ["scrF"])
                    V_(lambda e: e.tensor_copy(out=scrI, in_=scrF), ["scrF"], ["scrI"])
                    V_(lambda e: e.tensor_copy(out=scrF, in_=scrI), ["scrI"], ["scrF"])
                    V_(lambda e: e.tensor_scalar(out=scrF, in0=scrF, scalar1=-TWO_PI, scalar2=shift, op0=ALU.mult, op1=ALU.add), ["scrF"], ["scrF"])
                    V_(lambda e: e.tensor_tensor(out=scrF, in0=scrF, in1=src_fn(), op=ALU.add), ["scrF"] + srcbufs, ["scrF"])
                    V_(lambda e: e.tensor_scalar(out=scrF, in0=scrF, scalar1=3.14159, scalar2=-3.14159, op0=ALU.min, op1=ALU.max), ["scrF"], ["scrF"])
                    V_(lambda e: e.activation(out=dst, in_=scrF, func=AF.Sin), ["scrF"], [dstbuf], "act")

                V_(lambda e: e.tensor_tensor(out=ARG[:], in0=bc3(TH[:]), in1=nvb, op=ALU.mult), ["TH", "NV", "MAG"], ["ARG"])
                sin_of(PWI[:], lambda: ARG[:], ["ARG"], "PWI", 0.0, (ARK[:], ARKi[:]))
                sin_of(PWR[:], lambda: ARG[:], ["ARG"], "PWR", math.pi / 2, (ARK[:], ARKi[:]))
                V_(lambda e: e.tensor_tensor(out=PWR[:], in0=PWR[:], in1=MAG[:], op=ALU.mult), ["PWR", "MAG"], ["PWR"])
                V_(lambda e: e.tensor_tensor(out=PWI[:], in0=PWI[:], in1=MAG[:], op=ALU.mult), ["PWI", "MAG"], ["PWI"])
                V_(lambda e: e.tensor_copy(out=RHO8[:], in_=MAG[:, :, 16]), ["MAG"], ["RHO8"])
                V_(lambda e: e.tensor_scalar(out=TH8[:], in0=TH[:], scalar1=8.0, scalar2=None, op0=ALU.mult), ["TH"], ["TH8"])
                abr = PWR[:, :, 9]
                abi = PWI[:, :, 9]
                V_(lambda e: e.tensor_scalar(out=zt1[:], in0=abr, scalar1=-1.0, scalar2=None, op0=ALU.add), ["PWR"], ["zt1"])
                V_(lambda e: e.tensor_tensor(out=zt2[:], in0=LR[:], in1=LR[:], op=ALU.mult), ["LR"], ["zt2"])
                V_(lambda e: e.tensor_tensor(out=zt3[:], in0=LI[:], in1=LI[:], op=ALU.mult), ["LI"], ["zt3"])
                V_(lambda e: e.tensor_tensor(out=zt2[:], in0=zt2[:], in1=zt3[:], op=ALU.add), ["zt2", "zt3"], ["zt2"])
                V_(lambda e: e.reciprocal(out=zt2[:], in_=zt2[:]), ["zt2"], ["zt2"])
                V_(lambda e: e.tensor_tensor(out=cfr[:], in0=zt1[:], in1=LR[:], op=ALU.mult), ["zt1", "LR"], ["cfr"])
                V_(lambda e: e.tensor_tensor(out=zt3[:], in0=abi, in1=LI[:], op=ALU.mult), ["PWI", "LI"], ["zt3"])
                V_(lambda e: e.tensor_tensor(out=cfr[:], in0=cfr[:], in1=zt3[:], op=ALU.add), ["cfr", "zt3"], ["cfr"])
                V_(lambda e: e.tensor_tensor(out=cfr[:], in0=cfr[:], in1=zt2[:], op=ALU.mult), ["cfr", "zt2"], ["cfr"])
                V_(lambda e: e.tensor_tensor(out=cfi[:], in0=abi, in1=LR[:], op=ALU.mult), ["PWI", "LR"], ["cfi"])
                V_(lambda e: e.tensor_tensor(out=zt3[:], in0=zt1[:], in1=LI[:], op=ALU.mult), ["zt1", "LI"], ["zt3"])
                V_(lambda e: e.tensor_tensor(out=cfi[:], in0=cfi[:], in1=zt3[:], op=ALU.subtract), ["cfi", "zt3"], ["cfi"])
                V_(lambda e: e.tensor_tensor(out=cfi[:], in0=cfi[:], in1=zt2[:], op=ALU.mult), ["cfi", "zt2"], ["cfi"])
                bh = lambda a: a.unsqueeze(2).to_broadcast([128, 16, 16])
                V_(lambda e: e.tensor_tensor(out=BBR[:], in0=bh(cfr[:]), in1=BR[:], op=ALU.mult), ["cfr", "BR"], ["BBR"])
                V_(lambda e: e.tensor_tensor(out=tb[:], in0=bh(cfi[:]), in1=BI[:], op=ALU.mult), ["cfi", "BI"], ["tb"])
                V_(lambda e: e.tensor_tensor(out=BBR[:], in0=BBR[:], in1=tb[:], op=ALU.subtract), ["BBR", "tb"], ["BBR"])
                V_(lambda e: e.tensor_tensor(out=BBI[:], in0=bh(cfr[:]), in1=BI[:], op=ALU.mult), ["cfr", "BI"], ["BBI"])
                V_(lambda e: e.tensor_tensor(out=tb[:], in0=bh(cfi[:]), in1=BR[:], op=ALU.mult), ["cfi", "BR"], ["tb"])
                V_(lambda e: e.tensor_tensor(out=BBI[:], in0=BBI[:], in1=tb[:], op=ALU.add), ["BBI", "tb"], ["BBI"])
                pwb = lambda a, lo: a[:, :, lo:lo + 8].unsqueeze(3).to_broadcast([128, 16, 8, 16])
                b4 = lambda a: a.unsqueeze(2).to_broadcast([128, 16, 8, 16])
                V_(lambda e: e.tensor_tensor(out=HR[:], in0=pwb(PWR, 0), in1=b4(BBR[:]), op=ALU.mult), ["PWR", "BBR"], ["HR"])
                V_(lambda e: e.tensor_tensor(out=tq[:], in0=pwb(PWI, 0), in1=b4(BBI[:]), op=ALU.mult), ["PWI", "BBI"], ["tq"])
                V_(lambda e: e.tensor_tensor(out=HR[:], in0=HR[:], in1=tq[:], op=ALU.subtract), ["HR", "tq"], ["HR"])
                V_(lambda e: e.tensor_tensor(out=HI[:], in0=pwb(PWR, 0), in1=b4(BBI[:]), op=ALU.mult), ["PWR", "BBI"], ["HI"])
                V_(lambda e: e.tensor_tensor(out=tq[:], in0=pwb(PWI, 0), in1=b4(BBR[:]), op=ALU.mult), ["PWI", "BBR"], ["tq"])
                V_(lambda e: e.tensor_tensor(out=HI[:], in0=HI[:], in1=tq[:], op=ALU.add), ["HI", "tq"], ["HI"])
                f3 = lambda a: a.rearrange("p q i h -> p q (i h)")
                a8 = lambda a: a[:, :, 16:17].to_broadcast([128, 16, 128])
                V_(lambda e: e.tensor_tensor(out=f3(GR[:]), in0=a8(PWR), in1=f3(HR[:]), op=ALU.mult), ["PWR", "HR"], ["GR"])
                V_(lambda e: e.tensor_tensor(out=f3(tq[:]), in0=a8(PWI), in1=f3(HI[:]), op=ALU.mult), ["PWI", "HI"], ["tq"])
                V_(lambda e: e.tensor_tensor(out=GR[:], in0=GR[:], in1=tq[:], op=ALU.subtract), ["GR", "tq"], ["GR"])
                V_(lambda e: e.tensor_tensor(out=f3(GI[:]), in0=a8(PWR), in1=f3(HI[:]), op=ALU.mult), ["PWR", "HI"], ["GI"])
                V_(lambda e: e.tensor_tensor(out=f3(tq[:]), in0=a8(PWI), in1=f3(HR[:]), op=ALU.mult), ["PWI", "HR"], ["tq"])
                V_(lambda e: e.tensor_tensor(out=GI[:], in0=GI[:], in1=tq[:], op=ALU.add), ["GI", "tq"], ["GI"])
                V_(lambda e: e.tensor_tensor(out=MOR[:], in0=pwb(PWR, 9), in1=b4(CR[:]), op=ALU.mult), ["PWR", "CR"], ["MOR"])
                V_(lambda e: e.tensor_tensor(out=tq[:], in0=pwb(PWI, 9), in1=b4(CI[:]), op=ALU.mult), ["PWI", "CI"], ["tq"])
                V_(lambda e: e.tensor_tensor(out=MOR[:], in0=MOR[:], in1=tq[:], op=ALU.subtract), ["MOR", "tq"], ["MOR"])
                V_(lambda e: e.tensor_tensor(out=MOI[:], in0=pwb(PWI, 9), in1=b4(CR[:]), op=ALU.mult), ["PWI", "CR"], ["MOI"])
                V_(lambda e: e.tensor_tensor(out=tq[:], in0=pwb(PWR, 9), in1=b4(CI[:]), op=ALU.mult), ["PWR", "CI"], ["tq"])
                V_(lambda e: e.tensor_tensor(out=MOI[:], in0=MOI[:], in1=tq[:], op=ALU.add), ["MOI", "tq"], ["MOI"])
                V_(lambda e: e.tensor_scalar(out=MOI[:], in0=MOI[:], scalar1=-1.0, scalar2=None, op0=ALU.mult), ["MOI"], ["MOI"])
                V_(lambda e: e.memset(MoRp[:], 0.0), [], ["MoRp"], "pool")
                V_(lambda e: e.memset(MoIp[:], 0.0), [], ["MoIp"], "pool")
                for m in range(2):
                    rows = slice(64 * m, 64 * m + 64)
                    V_(lambda e, rows=rows, m=m: e.tensor_copy(out=MoRp[rows, :, m, :], in_=f3(MOR[:])[rows, :, :]), ["MOR", "MoRp"], ["MoRp"])
                    V_(lambda e, rows=rows, m=m: e.tensor_copy(out=MoIp[rows, :, m, :], in_=f3(MOI[:])[rows, :, :]), ["MOI", "MoIp"], ["MoIp"])
                V_(lambda e: e.memset(msk[:], 1.0), [], ["msk"], "pool")
                V_(lambda e: e.affine_select(out=msk[:], in_=msk[:], pattern=[[16, 8], [0, 16]], compare_op=ALU.is_ge,
                                             fill=0.0, base=15, channel_multiplier=-1), ["msk"], ["msk"], "pool")
                for g in range(32):
                    q, m = g // 2, g % 2
                    rows = slice(64 * m, 64 * m + 64)
                    ps = ps_s0[g % 4]
                    pb = B("ps_s0_%d" % (g % 4))
                    O("pe", lambda e, ps=ps, rows=rows, q=q: e.transpose(out=ps[:, 0:64], in_=f3(GR[:])[rows, q, :], identity=identf[rows, rows]),
                      reads=[B("GR"), B("identf")], writes=[pb])
                    O("pe", lambda e, ps=ps, rows=rows, q=q: e.transpose(out=ps[:, 64:128], in_=f3(GI[:])[rows, q, :], identity=identf[rows, rows]),
                      reads=[B("GI"), B("identf")], writes=[pb])
                    O("pe", lambda e, ps=ps, rows=rows, q=q: e.matmul(ps[:, 128:256], lhsT=f3(HR[:])[rows, q, :], rhs=f3(MOR[:])[rows, q, :], start=True, stop=False),
                      reads=[B("HR"), B("MOR")], writes=[pb])
                    O("pe", lambda e, ps=ps, rows=rows, q=q: e.matmul(ps[:, 128:256], lhsT=f3(HI[:])[rows, q, :], rhs=f3(MOI[:])[rows, q, :], start=False, stop=True),
                      reads=[B("HI"), B("MOI")], writes=[pb])
                    O("act", lambda e, ps=ps, g=g: e.activation(func=AF.Identity, out=MinR[:, g, :], in_=ps[:, 0:64]), reads=[pb], writes=[B("MinR")])
                    O("act", lambda e, ps=ps, g=g: e.activation(func=AF.Identity, out=MinI[:, g, :], in_=ps[:, 64:128]), reads=[pb], writes=[B("MinI")])
                    O("dve", lambda e, ps=ps: e.tensor_tensor(out=tmi[:], in0=ps[:, 128:256], in1=msk[:].rearrange("p j h -> p (j h)"), op=ALU.mult),
                      reads=[pb, B("msk")], writes=[B("tmi")])
                    O("dve", lambda e, g=g: e.scalar_tensor_tensor(out=MI[:, g, :], in0=identf[:], scalar=DS[:, g:g + 1], in1=tmi[:], op0=ALU.mult, op1=ALU.add),
                      reads=[B("identf"), B("DS"), B("tmi")], writes=[B("MI")])
                if STOP_AFTER == "S0":
                    d1 = O("sp", lambda e: e.dma_start(out=dbgb_d[:, 0:4096], in_=MI[:].rearrange("p g f -> p (g f)")), reads=[B("MI")], dma_key="dbg1")
                    d2 = O("sp", lambda e: e.dma_start(out=dbgb_d[:, 4096:6144], in_=MinR[:].rearrange("p g f -> p (g f)")), reads=[B("MinR")], dma_key="dbg2")
                    d3 = O("sp", lambda e: e.dma_start(out=dbgb_d[:, 6144:8192], in_=MoRp[:, 0:8, :, :].rearrange("p q m f -> p (q m f)")), reads=[B("MoRp")], dma_key="dbg3")
                    d4 = O("sp", lambda e: e.dma_start(out=dbgf_d[:, 0:272], in_=PWR[:].rearrange("p q n -> p (q n)")), reads=[B("PWR")], dma_key="dbg4")
                    d5 = O("sp", lambda e: e.dma_start(out=dbgf_d[:, 272:544], in_=PWI[:].rearrange("p q n -> p (q n)")), reads=[B("PWI")], dma_key="dbg5")
                    d6 = O("sp", lambda e: e.dma_start(out=dbgf_d[:, 544:2592], in_=HR[:].rearrange("p q i h -> p (q i h)")), reads=[B("HR")], dma_key="dbg6")
                    pg.emit(final_wait_ops=[d1, d2, d3, d4, d5, d6])
                    return nc
            pg.barrier()

            with contextlib.ExitStack() as s1:
                Zt = T(s1, "Zt", [128, 4, 32, 8, 16], BF16)
                wS = T(s1, "wS", [128, 8, 512], BF16)
                wK = T(s1, "wK", [128, 8, 512], BF16)
                wV = T(s1, "wV", [128, 8, 512], BF16)
                wG = T(s1, "wG", [128, 4, 512], BF16)
                for dc in range(8):
                    rows = slice(dc * 128, (dc + 1) * 128)
                    load("pool", wS[:, dc, :], w_in_d[rows, 0:512], "wS", "wS")
                    load("pool", wK[:, dc, :], w_in_d[rows, 1024:1536], "wK", "wK")
                    load("pool", wV[:, dc, :], w_in_d[rows, 1536:2048], "wV", "wV")
                for fc in range(4):
                    load("pool", wG[:, fc, :], w_glu_d[fc * 128:(fc + 1) * 128, :], "wG", "wG")

                if STOP_AFTER == "S1a0":
                    d1 = O("sp", lambda e: e.dma_start(out=dbgb_d[:, 0:4096], in_=wS[:].rearrange("p a b -> p (a b)")), reads=[B("wS")], dma_key="dbg1")
                    d2 = O("sp", lambda e: e.dma_start(out=dbgb_d[:, 4096:6144], in_=wG[:].rearrange("p a b -> p (a b)")), reads=[B("wG")], dma_key="dbg2")
                    pg.emit(final_wait_ops=[d1, d2])
                    return nc
                with contextlib.ExitStack() as s1a:
                    uT = T(s1a, "uT", [128, 8, 1024], BF16)
                    xt = [T(s1a, "xt%d" % k, [128, D], F32) for k in range(2)]
                    scr = {"sq": T(s1a, "sq", [128, D], BF16)[:], "ss": T(s1a, "ss", [128, 1], F32)[:], "rs": T(s1a, "rs", [128, 1], F32)[:],
                           "xn": T(s1a, "xn", [128, D], F32)[:], "eps": epsb[:]}
                    psT = [PS(s1a, "psT%d" % k, [128, 512], F32) for k in range(2)]
                    psM = [PS(s1a, "psM%d" % k, [128, 512], F32) for k in range(4)]
                    psTb = [B("psT0"), B("psT1")]
                    nmm = [0]

                    def next_psM():
                        k = nmm[0] % 4
                        nmm[0] += 1
                        return psM[k], B("psM%d" % k)

                    x_loads = {}

                    def issue_xload(tt):
                        slot = tt % 2
                        x_loads[tt] = load("sp", xt[slot][:], x_d[tt * 128:(tt + 1) * 128, :], "xt%d" % slot, "xt%d" % slot)

                    issue_xload(0)
                    for s in range(4):
                        for t8 in range(8):
                            tt = s * 8 + t8
                            if tt + 1 < 32:
                                issue_xload(tt + 1)
                            slot = tt % 2
                            rmsnorm_T("S", xt[slot][:], B("xt%d" % slot), g1sb,
                                      lambda dc, t8=t8: uT[:, dc, t8 * 128:(t8 + 1) * 128], B("uT"), psT, psTb, scr, "S")
                        if STOP_AFTER == "S1a1":
                            d1 = O("sp", lambda e: e.dma_start(out=dbgb_d[:, 0:8192], in_=uT[:].rearrange("p a b -> p (a b)")), reads=[B("uT"), B("Zt0"), B("KT"), B("VA"), B("KS")], dma_key="dbg1")
                            pg.emit(final_wait_ops=[d1])
                            return nc
                        for i in range(8):
                            ps, pb = next_psM()
                            for dc in range(8):
                                O("pe", lambda e, ps=ps, dc=dc, i=i: e.matmul(ps[:, :], lhsT=uT[:, dc, :].rearrange("p (c i) -> p i c", i=8)[:, i, :],
                                                                               rhs=wS[:, dc, :], start=(dc == 0), stop=(dc == 7)),
                                  reads=[B("uT"), B("wS")], writes=[pb])
                            eng = "act" if i % 2 == 0 else "dve"
                            if eng == "act":
                                O("act", lambda e, ps=ps, s=s, i=i: e.activation(func=AF.Identity, out=Zt[:, s, :, i, :], in_=ps[:, :].rearrange("p (g h) -> p g h", g=32)), reads=[pb], writes=[B("Zt%d" % q_) for q_ in range(16)])
                            else:
                                O("dve", lambda e, ps=ps, s=s, i=i: e.tensor_copy(out=Zt[:, s, :, i, :], in_=ps[:, :].rearrange("p (g h) -> p g h", g=32)), reads=[pb], writes=[B("Zt%d" % q_) for q_ in range(16)])
                        if STOP_AFTER == "S1a2":
                            d1 = O("sp", lambda e: e.dma_start(out=dbgb_d[:, 0:8192], in_=uT[:].rearrange("p a b -> p (a b)")), reads=[B("uT"), B("Zt0"), B("KT"), B("VA"), B("KS")], dma_key="dbg1")
                            pg.emit(final_wait_ops=[d1])
                            return nc
                        for hp in range(4):
                            for th in range(2):
                                ps, pb = next_psM()
                                for dc in range(8):
                                    O("pe", lambda e, ps=ps, dc=dc, hp=hp, th=th: e.matmul(ps[:, :], lhsT=wK[:, dc, hp * 128:(hp + 1) * 128],
                                                                                           rhs=uT[:, dc, th * 512:(th + 1) * 512], start=(dc == 0), stop=(dc == 7)),
                                      reads=[B("uT"), B("wK")], writes=[pb])
                                tok0 = s * 1024 + th * 512
                                for bb in range(2):
                                    blk = tok0 // 256 + bb
                                    O("act", lambda e, ps=ps, hp=hp, tok0=tok0, bb=bb, blk=blk: e.activation(
                                        out=KT[:, hp, tok0 + bb * 256:tok0 + (bb + 1) * 256], in_=ps[:, bb * 256:(bb + 1) * 256], func=AF.Copy,
                                        accum_out=KS[:, hp, blk:blk + 1]), reads=[pb], writes=[B("KT"), B("KS")])
                        if STOP_AFTER == "S1a3":
                            d1 = O("sp", lambda e: e.dma_start(out=dbgb_d[:, 0:8192], in_=uT[:].rearrange("p a b -> p (a b)")), reads=[B("uT"), B("Zt0"), B("KT"), B("VA"), B("KS")], dma_key="dbg1")
                            pg.emit(final_wait_ops=[d1])
                            return nc
                        for t8 in range(8):
                            tt = s * 8 + t8
                            ps, pb = next_psM()
                            for dc in range(8):
                                O("pe", lambda e, ps=ps, dc=dc, t8=t8: e.matmul(ps[:, :], lhsT=uT[:, dc, t8 * 128:(t8 + 1) * 128], rhs=wV[:, dc, :],
                                                                                start=(dc == 0), stop=(dc == 7)), reads=[B("uT"), B("wV")], writes=[pb])
                            if t8 % 2 == 0:
                                O("act", lambda e, ps=ps, tt=tt: e.activation(func=AF.Identity, out=VA[:, tt, :, 0:64], in_=ps[:, :].rearrange("p (h d) -> p h d", h=8)), reads=[pb], writes=[B("VA")])
                            else:
                                O("dve", lambda e, ps=ps, tt=tt: e.tensor_copy(out=VA[:, tt, :, 0:64], in_=ps[:, :].rearrange("p (h d) -> p h d", h=8)), reads=[pb], writes=[B("VA")])
                pg.barrier()
                if STOP_AFTER == "S1a":
                    d1 = O("sp", lambda e: e.dma_start(out=dbgb_d[:, 0:4096], in_=KT[:, 1, :]), reads=[B("KT")], dma_key="dbg1")
                    d2 = O("sp", lambda e: e.dma_start(out=dbgb_d[:, 4096:8192], in_=Zt[:, 1, :, :, :].rearrange("p g i h -> p (g i h)")), reads=[B("Zt0")], dma_key="dbg2")
                    d3 = O("sp", lambda e: e.dma_start(out=dbgf_d[:, 0:64], in_=KS[:].rearrange("p a b -> p (a b)")), reads=[B("KS")], dma_key="dbg3")
                    pg.emit(final_wait_ops=[d1, d2, d3])
                    return nc

                with contextlib.ExitStack() as s1b:
                    Ug = [T(s1b, "Ug%d" % m, [128, 512], BF16) for m in range(2)]
                    CI_i = T(s1b, "CIi", [128, 512], I32)
                    CIf = T(s1b, "CIf", [128, 512], F32)
                    ANG = T(s1b, "ANG", [128, 512], F32)
                    sF = T(s1b, "sF", [128, 512], F32)
                    sI = T(s1b, "sI", [128, 512], I32)
                    COSC = T(s1b, "COSC", [128, 512], F32)
                    SINC = T(s1b, "SINC", [128, 512], F32)
                    WR = T(s1b, "WR", [128, 512], F32)
                    WI = T(s1b, "WI", [128, 512], F32)
                    ta = T(s1b, "ta", [128, 512], F32)
                    tbb = T(s1b, "tbb", [128, 512], F32)
                    XPR = T(s1b, "XPR", [128, 516], BF16)
                    XPI = T(s1b, "XPI", [128, 516], BF16)
                    gs = T(s1b, "gs", [128, 512], F32)
                    gu = T(s1b, "gu", [128, 512], F32)
                    psU = PS(s1b, "psU", [128, 1024], BF16)
                    psXr = PS(s1b, "psXr", [128, 512], F32)
                    psXi = PS(s1b, "psXi", [128, 512], F32)
                    psY = [PS(s1b, "psY%d" % m, [128, 512], F32) for m in range(2)]

                    O("pool", lambda e: e.iota(CI_i[:], pattern=[[1, 512]], base=0, channel_multiplier=0), writes=[B("CIi")])
                    O("dve", lambda e: e.tensor_copy(out=CIf[:], in_=CI_i[:]), reads=[B("CIi")], writes=[B("CIf")])
                    O("dve", lambda e: e.memset(XPR[:, 0:1], 0.0), writes=[B("XPR")])
                    O("dve", lambda e: e.memset(XPI[:, 0:1], 0.0), writes=[B("XPI")])

                    def V2(fn, r, w, eng="dve"):
                        return O(eng, fn, reads=[B(n) for n in r], writes=[B(n) for n in w])

                    def sin2(dst, dstbuf, shift):
                        V2(lambda e: e.tensor_scalar(out=sF[:], in0=ANG[:], scalar1=shift, scalar2=1.0 / TWO_PI, op0=ALU.add, op1=ALU.mult), ["ANG"], ["sF"])
                        V2(lambda e: e.tensor_copy(out=sI[:], in_=sF[:]), ["sF"], ["sI"])
                        V2(lambda e: e.tensor_copy(out=sF[:], in_=sI[:]), ["sI"], ["sF"])
                        V2(lambda e: e.tensor_scalar(out=sF[:], in0=sF[:], scalar1=-TWO_PI, scalar2=shift, op0=ALU.mult, op1=ALU.add), ["sF"], ["sF"])
                        V2(lambda e: e.tensor_tensor(out=sF[:], in0=sF[:], in1=ANG[:], op=ALU.add), ["sF", "ANG"], ["sF"])
                        V2(lambda e: e.tensor_scalar(out=sF[:], in0=sF[:], scalar1=3.14159, scalar2=-3.14159, op0=ALU.min, op1=ALU.max), ["sF"], ["sF"])
                        V2(lambda e: e.activation(out=dst, in_=sF[:], func=AF.Sin), ["sF"], [dstbuf], "act")

                    for q in range(16):
                        zb = B("Zt%d" % q)
                        for m in range(2):
                            g = 2 * q + m
                            for s in range(4):
                                O("pe", lambda e, s=s, g=g, m=m: e.transpose(out=psU[:, m * 512 + s * 128: m * 512 + (s + 1) * 128],
                                                                             in_=Zt[:, s, g, :, :].rearrange("p i h -> p (i h)"), identity=identb[:]),
                                  reads=[zb, B("identb")], writes=[B("psU")])
                            if m == 0:
                                O("act", lambda e, m=m: e.activation(func=AF.Identity, out=Ug[m][:], in_=psU[:, m * 512:(m + 1) * 512]), reads=[B("psU")], writes=[B("Ug%d" % m)])
                            else:
                                O("dve", lambda e, m=m: e.tensor_copy(out=Ug[m][:], in_=psU[:, m * 512:(m + 1) * 512]), reads=[B("psU")], writes=[B("Ug%d" % m)])
                        for m in range(2):
                            g = 2 * q + m
                            O("pe", lambda e, m=m, g=g: e.matmul(psXr[64 * m:64 * m + 64, :], lhsT=MinR[:, g, :], rhs=Ug[m][:], start=True, stop=True),
                              reads=[B("MinR"), B("Ug%d" % m)], writes=[B("psXr")])
                            O("pe", lambda e, m=m, g=g: e.matmul(psXi[64 * m:64 * m + 64, :], lhsT=MinI[:, g, :], rhs=Ug[m][:], start=True, stop=True),
                              reads=[B("MinI"), B("Ug%d" % m)], writes=[B("psXi")])
                        V2(lambda e, q=q: e.tensor_scalar(out=ANG[:], in0=CIf[:], scalar1=TH8[:, q:q + 1], scalar2=None, op0=ALU.mult), ["CIf", "TH8"], ["ANG"])
                        sin2(SINC[:], "SINC", 0.0)
                        sin2(COSC[:], "COSC", math.pi / 2)
                        V2(lambda e: e.tensor_tensor(out=ta[:], in0=psXr[:], in1=COSC[:], op=ALU.mult), ["psXr", "COSC"], ["ta"])
                        V2(lambda e: e.tensor_tensor(out=tbb[:], in0=psXi[:], in1=SINC[:], op=ALU.mult), ["psXi", "SINC"], ["tbb"])
                        V2(lambda e: e.tensor_tensor(out=ta[:], in0=ta[:], in1=tbb[:], op=ALU.add), ["ta", "tbb"], ["ta"])
                        V2(lambda e, q=q: e.tensor_tensor_scan(out=WR[:], data0=RHO8[:, q:q + 1].to_broadcast([128, 512]), data1=ta[:], initial=0.0,
                                                                op0=ALU.mult, op1=ALU.add), ["ta", "RHO8"], ["WR"])
                        V2(lambda e: e.tensor_tensor(out=ta[:], in0=psXi[:], in1=COSC[:], op=ALU.mult), ["psXi", "COSC", "WR"], ["ta"])
                        V2(lambda e: e.tensor_tensor(out=tbb[:], in0=psXr[:], in1=SINC[:], op=ALU.mult), ["psXr", "SINC"], ["tbb"])
                        V2(lambda e: e.tensor_tensor(out=ta[:], in0=ta[:], in1=tbb[:], op=ALU.subtract), ["ta", "tbb"], ["ta"])
                        V2(lambda e, q=q: e.tensor_tensor_scan(out=WI[:], data0=RHO8[:, q:q + 1].to_broadcast([128, 512]), data1=ta[:], initial=0.0,
                                                                op0=ALU.mult, op1=ALU.add), ["ta", "RHO8"], ["WI"])
                        V2(lambda e: e.tensor_tensor(out=ta[:], in0=WR[:], in1=COSC[:], op=ALU.mult), ["WR", "COSC", "WI"], ["ta"])
                        V2(lambda e: e.tensor_tensor(out=tbb[:], in0=WI[:], in1=SINC[:], op=ALU.mult), ["WI", "SINC"], ["tbb"])
                        V2(lambda e: e.tensor_tensor(out=XPR[:, 1:513], in0=ta[:], in1=tbb[:], op=ALU.subtract), ["ta", "tbb"], ["XPR"])
                        V2(lambda e: e.tensor_tensor(out=ta[:], in0=WI[:], in1=COSC[:], op=ALU.mult), ["WI", "COSC", "XPR"], ["ta"])
                        V2(lambda e: e.tensor_tensor(out=tbb[:], in0=WR[:], in1=SINC[:], op=ALU.mult), ["WR", "SINC"], ["tbb"])
                        V2(lambda e: e.tensor_tensor(out=XPI[:, 1:513], in0=ta[:], in1=tbb[:], op=ALU.add), ["ta", "tbb"], ["XPI"])
                        for m in range(2):
                            g = 2 * q + m
                            pbY = B("psY%d" % m)
                            for s in range(4):
                                cs = slice(s * 128, (s + 1) * 128)
                                outp = psY[m][:, s * 128:(s + 1) * 128]
                                O("pe", lambda e, outp=outp, cs=cs, g=g, m=m: e.matmul(outp, lhsT=Ug[m][:, cs], rhs=MI[:, g, :], start=True, stop=False),
                                  reads=[B("Ug%d" % m), B("MI")], writes=[pbY])
                                O("pe", lambda e, outp=outp, cs=cs, q=q, m=m: e.matmul(outp, lhsT=XPR[:, cs], rhs=MoRp[:, q, m, :], start=False, stop=False),
                                  reads=[B("XPR"), B("MoRp")], writes=[pbY])
                                O("pe", lambda e, outp=outp, cs=cs, q=q, m=m: e.matmul(outp, lhsT=XPI[:, cs], rhs=MoIp[:, q, m, :], start=False, stop=True),
                                  reads=[B("XPI"), B("MoIp")], writes=[pbY])
                            yv = psY[m][:, :]
                            V2(lambda e, yv=yv: e.activation(out=gs[:], in_=yv, func=AF.Square), ["psY%d" % m], ["gs"], "act")
                            V2(lambda e: e.tensor_scalar(out=gs[:], in0=gs[:], scalar1=0.044715, scalar2=1.0, op0=ALU.mult, op1=ALU.add), ["gs"], ["gs"])
                            V2(lambda e, yv=yv: e.tensor_tensor(out=gu[:], in0=yv, in1=gs[:], op=ALU.mult), ["psY%d" % m, "gs"], ["gu"])
                            V2(lambda e: e.activation(out=gu[:], in_=gu[:], func=AF.Sigmoid, scale=1.5957691216), ["gu"], ["gu"], "act")
                            O("dve", lambda e, yv=yv, g=g: e.tensor_tensor(out=Zt[:, :, g, :, :],
                                                                            in0=yv.rearrange("p (s j h) -> p s j h", s=4, j=8),
                                                                            in1=gu[:].rearrange("p (s j h) -> p s j h", s=4, j=8), op=ALU.mult),
                              reads=[pbY, B("gu")], writes=[zb])
                pg.barrier()

                with contextlib.ExitStack() as s1c:
                    vT = [T(s1c, "vT%d" % k, [128, 4, 1024], BF16) for k in range(2)]
                    yaT = [T(s1c, "yaT%d" % k, [128, 4, 1024], BF16) for k in range(2)]
                    sg = T(s1c, "sg", [128, 512], F32)
                    tmpV = T(s1c, "tmpV", [128, 8, 512], BF16)
                    psV = [PS(s1c, "psV%d" % k, [128, 1024], BF16) for k in range(2)]
                    psG = [PS(s1c, "psG%d" % k, [128, 512], F32) for k in range(2)]
                    zall = [B("Zt%d" % q_) for q_ in range(16)]
                    cnt = 0
                    for s in range(4):
                        sl = s % 2
                        O("pool", lambda e, s=s: e.tensor_copy(out=tmpV[:].rearrange("p j (g h) -> p j g h", g=32), in_=Zt[:, s, :, :, :].rearrange("p g j h -> p j g h")),
                          reads=zall, writes=[B("tmpV")])
                        for fc in range(4):
                            pv = psV[fc % 2]
                            pvb = B("psV%d" % (fc % 2))
                            for j in range(8):
                                O("pe", lambda e, pv=pv, s=s, j=j, fc=fc: e.transpose(out=pv[:, j * 128:(j + 1) * 128], in_=tmpV[:, j, fc * 128:(fc + 1) * 128], identity=identb[:]),
                                  reads=[B("tmpV"), B("identb")], writes=[pvb])
                            dst = vT[sl][:, fc, :].rearrange("p (c j) -> p j c", j=8)
                            srcv = pv[:, :].rearrange("p (j c) -> p j c", j=8)
                            if fc % 2 == 0:
                                O("act", lambda e, dst=dst, srcv=srcv: e.activation(func=AF.Identity, out=dst, in_=srcv), reads=[pvb], writes=[B("vT%d" % sl)])
                            else:
                                O("dve", lambda e, dst=dst, srcv=srcv: e.tensor_copy(out=dst, in_=srcv), reads=[pvb], writes=[B("vT%d" % sl)])
                        for oc in range(4):
                            for th in range(2):
                                pgk = cnt % 2
                                cnt += 1
                                pG = psG[pgk]
                                pGb = B("psG%d" % pgk)
                                ts_ = slice(th * 512, (th + 1) * 512)
                                for fc in range(4):
                                    O("pe", lambda e, pG=pG, fc=fc, oc=oc, ts_=ts_, sl=sl: e.matmul(pG[:, :], lhsT=wG[:, fc, oc * 128:(oc + 1) * 128], rhs=vT[sl][:, fc, ts_],
                                                                                                   start=(fc == 0), stop=(fc == 3)), reads=[B("wG"), B("vT%d" % sl)], writes=[pGb])
                                O("act", lambda e, pG=pG, oc=oc: e.activation(out=sg[:], in_=pG[:, :], func=AF.Sigmoid, bias=bglu[:, oc:oc + 1]),
                                  reads=[pGb, B("bglu")], writes=[B("sg")])
                                O("dve", lambda e, oc=oc, ts_=ts_, sl=sl: e.tensor_tensor(out=yaT[sl][:, oc, ts_], in0=vT[sl][:, oc, ts_], in1=sg[:], op=ALU.mult),
                                  reads=[B("vT%d" % sl), B("sg")], writes=[B("yaT%d" % sl)])
                        for oc in range(4):
                            stp = O("sp", lambda e, oc=oc, s=s, sl=sl: e.dma_start(out=ya_d[oc * 128:(oc + 1) * 128, s * 1024:(s + 1) * 1024], in_=yaT[sl][:, oc, :]),
                                    reads=[B("yaT%d" % sl)], writes=[B("ya_d_%d_%d" % (s, oc))], dma_key="yast%d" % sl)
                    ya_last = [pg.ops["sp"][-1]]
                    ya_stores = [o for o in pg.ops["sp"] if o.is_dma and isinstance(o.dsem, str) and o.dsem.startswith("yast")]
            pg.barrier(dma_ops=ya_stores[-8:])

        if STOP_AFTER == "S":
            pg.emit(final_wait_ops=ya_stores[-8:])
            return nc

        with contextlib.ExitStack() as sA:
            wQ = T(sA, "wQ", [128, 8, 512], BF16)
            wGA = T(sA, "wGA", [128, 8, 1024], BF16)
            wGB = T(sA, "wGB", [128, 8, 1024], BF16)
            wA = T(sA, "wA", [128, 4, 1024], BF16)
            wB = T(sA, "wB", [128, 4, 1024], BF16)
            wO = T(sA, "wO", [128, 8, 1024], BF16)
            g2b = T(sA, "g2b", [128, D], F32)
            load("sp", g2b[:], g2_d.partition_broadcast(128), "g2b", "g2b")
            for dc in range(8):
                rows = slice(dc * 128, (dc + 1) * 128)
                load("pool", wQ[:, dc, :], w_in_d[rows, 512:1024], "wQ", "wQ")
            for dc in range(8 if not DBG_ONLYQ else 0):
                rows = slice(dc * 128, (dc + 1) * 128)
                load("pool", wGA[:, dc, :], w_in_d[rows, 2048:3072], "wGA", "wGA")
                load("pool", wGB[:, dc, :], w_in_d[rows, 3072:4096], "wGB", "wGB")
            for fc in range(4 if not DBG_ONLYQ else 0):
                rows = slice(fc * 128, (fc + 1) * 128)
                load("pool", wA[:, fc, :], w_a_d[rows, :], "wA", "wA")
                load("pool", wB[:, fc, :], w_b_d[rows, :], "wB", "wB")
            for dc in range(8 if not DBG_ONLYQ else 0):
                rows = slice(dc * 128, (dc + 1) * 128)
                load("pool", wO[:, dc, :], w_out_d[rows, :], "wO", "wO")

            xq = [T(sA, "xq%d" % k, [128, 2, D], F32) for k in range(2)]
            yaq = [T(sA, "yaq%d" % k, [128, 4, 256], BF16) for k in range(2)]
            uTq = T(sA, "uTq", [128, 8, 256], BF16)
            scrA = {"sq": T(sA, "sqA", [128, D], BF16)[:], "ss": T(sA, "ssA", [128, 1], F32)[:], "rs": T(sA, "rsA", [128, 1], F32)[:],
                    "xn": T(sA, "xnA", [128, D], F32)[:], "eps": epsb[:]}
            QT = T(sA, "QT", [128, 4, 256], BF16)
            QTf = T(sA, "QTf", [128, 4, 256], F32)
            gate = T(sA, "gate", [128, 2, 8, 16], F32)
            top8 = T(sA, "top8", [128, 2, 8, 8], F32)
            sel = T(sA, "sel", [128, 2, 8, 16], F32)
            trim = T(sA, "trim", [128, 2, 256], BF16)
            PT = [T(sA, "PT%d" % k, [128, 2, 256], BF16) for k in range(3)]
            acc = T(sA, "acc", [128, 2, 8, 65], F32)
            rden = T(sA, "rden", [128, 2, 8], F32)
            ybt = T(sA, "ybt", [128, 2, 512], BF16)
            ybT = T(sA, "ybT", [128, 4, 256], BF16)
            sga = T(sA, "sga", [128, 256], F32)
            sgb = T(sA, "sgb", [128, 256], F32)
            tma = T(sA, "tma", [128, 256], F32)
            mT = T(sA, "mT", [128, 8, 256], BF16)
            mg = scrA["xn"]
            ssM = T(sA, "ssM", [128, 2], F32)
            rsM = T(sA, "rsM", [128, 1], F32)
            psT = [PS(sA, "psTA%d" % k, [128, 512], F32) for k in range(2)]
            psS = [PS(sA, "psS%d" % k, [128, 512], F32) for k in range(2)]
            psO = [PS(sA, "psO%d" % k, [128, 512], F32) for k in range(2)]
            psW = [PS(sA, "psW%d" % k, [128, 512], F32) for k in range(2)]
            psTb = [B("psTA0"), B("psTA1")]

            O("pool", lambda e: e.memset(trim[:], 1.0), writes=[B("trim")])
            for kh in range(2):
                O("pool", lambda e, kh=kh: e.affine_select(out=trim[:, kh, :], in_=trim[:, kh, :], pattern=[[1, 256]], compare_op=ALU.is_ge,
                                                           fill=0.0, base=-128 * kh, channel_multiplier=-1), reads=[B("trim")], writes=[B("trim")])

            def issue_A_loads(i):
                sl = i % 2
                for qh in range(2):
                    tt = 2 * i + qh
                    load("sp", xq[sl][:, qh, :], x_d[tt * 128:(tt + 1) * 128, :], "xq%d" % sl, "xq%d" % sl)
                if not DBG_NOYA:
                    for fc in range(4):
                        load("sp", yaq[sl][:, fc, :], ya_d[fc * 128:(fc + 1) * 128, i * 256:(i + 1) * 256], "yaq%d" % sl, "yaq%d" % sl)

            if STOP_AFTER == "A0":
                pg.emit(final_wait_ops=[])
                return nc
            issue_A_loads(0)
            ptc = [0]
            h1_stores = []
            for i in range(DBG_NBLK):
                sl = i % 2
                if i + 1 < DBG_NBLK:
                    issue_A_loads(i + 1)
                for qh in range(2):
                    rmsnorm_T("A", xq[sl][:, qh, :], B("xq%d" % sl), g1sb,
                              lambda dc, qh=qh: uTq[:, dc, qh * 128:(qh + 1) * 128], B("uTq"), psT, psTb, scrA, "A")
                if STOP_AFTER == "A1":
                    pg.emit(final_wait_ops=[])
                    return nc
                for hp in range(4):
                    ps = psS[hp % 2]
                    pb = B("psS%d" % (hp % 2))
                    for dc in range(8):
                        O("pe", lambda e, ps=ps, dc=dc, hp=hp: e.matmul(ps[:, 0:256], lhsT=wQ[:, dc, hp * 128:(hp + 1) * 128], rhs=uTq[:, dc, :],
                                                                        start=(dc == 0), stop=(dc == 7)), reads=[B("wQ"), B("uTq")], writes=[pb])
                    O("dve", lambda e, ps=ps, hp=hp: e.tensor_copy(out=QTf[:, hp, :], in_=ps[:, 0:256]), reads=[pb], writes=[B("QTf")])
                    O("pool", lambda e, hp=hp: e.tensor_copy(out=QT[:, hp, :], in_=QTf[:, hp, :]), reads=[B("QTf")], writes=[B("QT")])
                if STOP_AFTER == "A2":
                    pg.emit(final_wait_ops=[])
                    return nc
                use_sel = i >= 4
                if use_sel:
                    for qh in range(2):
                        pgt = psO[qh]
                        pgb = B("psO%d" % qh)
                        for h in range(8):
                            hp, par = h // 2, h % 2
                            rows = slice(64 * par, 64 * par + 64)
                            O("pe", lambda e, pgt=pgt, h=h, hp=hp, rows=rows, qh=qh: e.matmul(pgt[:, h * 16:(h + 1) * 16], lhsT=QTf[rows, hp, qh * 128:(qh + 1) * 128],
                                                                                            rhs=KS[rows, hp, :], start=True, stop=True),
                              reads=[B("QTf"), B("KS")], writes=[pgb])
                        O("dve", lambda e, qh=qh: e.memset(gate[:, qh, :, :], NEG), writes=[B("gate")])
                        O("dve", lambda e, qh=qh, pgt=pgt, i=i: e.tensor_copy(out=gate[:, qh, :, 0:i], in_=pgt[:, 0:128].rearrange("p (h j) -> p h j", h=8)[:, :, 0:i]),
                          reads=[pgb], writes=[B("gate")])
                        for h in range(8):
                            O("dve", lambda e, qh=qh, h=h: e.max(out=top8[:, qh, h, :], in_=gate[:, qh, h, :]), reads=[B("gate")], writes=[B("top8")])
                        O("dve", lambda e, qh=qh: e.tensor_tensor(out=sel[:, qh, :, :], in0=gate[:, qh, :, :], in1=top8[:, qh, :, 2:3].to_broadcast([128, 8, 16]), op=ALU.is_ge),
                          reads=[B("gate"), B("top8")], writes=[B("sel")])
                for h in range(8):
                    hp, par = h // 2, h % 2
                    rows = slice(64 * par, 64 * par + 64)
                    for j in range(i + 1):
                        k3 = ptc[0] % 3
                        k2 = ptc[0] % 2
                        ptc[0] += 1
                        pS = psS[k2]
                        pSb = B("psS%d" % k2)
                        pt = PT[k3]
                        ptb = B("PT%d" % k3)
                        for kh in range(2):
                            key0 = j * 256 + kh * 128
                            O("pe", lambda e, pS=pS, kh=kh, rows=rows, hp=hp, key0=key0: e.matmul(pS[:, kh * 256:(kh + 1) * 256], lhsT=KT[rows, hp, key0:key0 + 128],
                                                                                                 rhs=QT[rows, hp, :], start=True, stop=True),
                              reads=[B("KT"), B("QT")], writes=[pSb])
                        if "noexp" in DBG_QV:
                            continue
                        O("act", lambda e, pS=pS, pt=pt: e.activation(out=pt[:].rearrange("p a b -> p (a b)"), in_=pS[:, :], func=AF.Exp, scale=0.125),
                          reads=[pSb], writes=[ptb])
                        if j == i and "nomask" not in DBG_QV:
                            O("pool", lambda e, pt=pt: e.tensor_tensor(out=pt[:], in0=pt[:], in1=trim[:], op=ALU.mult), reads=[ptb, B("trim")], writes=[ptb])
                        if "nopv" in DBG_QV:
                            continue
                        pO = psO[k2]
                        pOb = B("psO%d" % k2)
                        for qh in range(2):
                            for kh in range(2):
                                if j == i and kh == 1 and qh == 0:
                                    continue
                                first = (kh == 0)
                                last = (kh == 1) or (j == i and qh == 0)
                                O("pe", lambda e, pO=pO, qh=qh, kh=kh, pt=pt, j=j, h=h, first=first, last=last: e.matmul(
                                    pO[:, qh * 128:qh * 128 + 66], lhsT=pt[:, kh, qh * 128:(qh + 1) * 128], rhs=VA[:, 2 * j + kh, h, :], start=first, stop=last),
                                  reads=[ptb, B("VA"), B("VAones"), B("VAzeros")], writes=[pOb])
                        for qh in range(2):
                            src = pO[:, qh * 128:qh * 128 + 65]
                            dsta = acc[:, qh, h, :]
                            if j == 0:
                                if use_sel and j < i:
                                    O("dve", lambda e, src=src, dsta=dsta, qh=qh, h=h, j=j: e.tensor_scalar(out=dsta, in0=src, scalar1=sel[:, qh, h, j:j + 1], scalar2=None, op0=ALU.mult),
                                      reads=[pOb, B("sel")], writes=[B("acc")])
                                else:
                                    O("dve", lambda e, src=src, dsta=dsta: e.tensor_copy(out=dsta, in_=src), reads=[pOb], writes=[B("acc")])
                            else:
                                if use_sel and j < i:
                                    O("dve", lambda e, src=src, dsta=dsta, qh=qh, h=h, j=j: e.scalar_tensor_tensor(out=dsta, in0=src, scalar=sel[:, qh, h, j:j + 1], in1=dsta,
                                                                                                                      op0=ALU.mult, op1=ALU.add),
                                      reads=[pOb, B("sel"), B("acc")], writes=[B("acc")])
                                else:
                                    O("dve", lambda e, src=src, dsta=dsta: e.tensor_tensor(out=dsta, in0=src, in1=dsta, op=ALU.add), reads=[pOb, B("acc")], writes=[B("acc")])
                if STOP_AFTER == "A3":
                    pg.emit(final_wait_ops=[])
                    return nc
                for qh in range(2):
                    O("dve", lambda e, qh=qh: e.reciprocal(out=rden[:, qh, :], in_=acc[:, qh, :, 64]), reads=[B("acc")], writes=[B("rden")])
                    O("dve", lambda e, qh=qh: e.tensor_tensor(out=ybt[:, qh, :].rearrange("p (h d) -> p h d", h=8), in0=acc[:, qh, :, 0:64],
                                                              in1=rden[:, qh, :].unsqueeze(2).to_broadcast([128, 8, 64]), op=ALU.mult),
                      reads=[B("acc"), B("rden")], writes=[B("ybt")])
                pvT = psT[0]
                for qh in range(2):
                    for fc in range(4):
                        O("pe", lambda e, qh=qh, fc=fc: e.transpose(out=pvT[:].bitcast(BF16)[:, (qh * 4 + fc) * 128:(qh * 4 + fc + 1) * 128],
                                                                     in_=ybt[:, qh, fc * 128:(fc + 1) * 128], identity=identb[:]),
                          reads=[B("ybt"), B("identb")], writes=[psTb[0]])
                for qh in range(2):
                    O("act", lambda e, qh=qh: e.activation(func=AF.Identity, out=ybT[:, :, qh * 128:(qh + 1) * 128], in_=pvT[:].bitcast(BF16)[:, qh * 512:(qh + 1) * 512].rearrange("p (f t) -> p f t", f=4)),
                      reads=[psTb[0]], writes=[B("ybT")])
                if STOP_AFTER == "A4":
                    pg.emit(final_wait_ops=[])
                    return nc
                for oc in range(8):
                    ocs = slice(oc * 128, (oc + 1) * 128)
                    pw = psW[oc % 2]
                    pwbuf = B("psW%d" % (oc % 2))
                    pq = psS[oc % 2]
                    pqb = B("psS%d" % (oc % 2))
                    for fc in range(4):
                        O("pe", lambda e, pw=pw, fc=fc, ocs=ocs, sl=sl: e.matmul(pw[:, 0:256], lhsT=wA[:, fc, ocs], rhs=yaq[sl][:, fc, :], start=(fc == 0), stop=(fc == 3)),
                          reads=[B("wA"), B("yaq%d" % sl)], writes=[pwbuf])
                    for fc in range(4):
                        O("pe", lambda e, pw=pw, fc=fc, ocs=ocs: e.matmul(pw[:, 256:512], lhsT=wB[:, fc, ocs], rhs=ybT[:, fc, :], start=(fc == 0), stop=(fc == 3)),
                          reads=[B("wB"), B("ybT")], writes=[pwbuf])
                    for dc in range(8):
                        O("pe", lambda e, pq=pq, dc=dc, ocs=ocs: e.matmul(pq[:, 0:256], lhsT=wGA[:, dc, ocs], rhs=uTq[:, dc, :], start=(dc == 0), stop=(dc == 7)),
                          reads=[B("wGA"), B("uTq")], writes=[pqb])
                    for dc in range(8):
                        O("pe", lambda e, pq=pq, dc=dc, ocs=ocs: e.matmul(pq[:, 256:512], lhsT=wGB[:, dc, ocs], rhs=uTq[:, dc, :], start=(dc == 0), stop=(dc == 7)),
                          reads=[B("wGB"), B("uTq")], writes=[pqb])
                    O("act", lambda e, pq=pq: e.activation(out=sga[:], in_=pq[:, 0:256], func=AF.Sigmoid), reads=[pqb], writes=[B("sga")])
                    O("act", lambda e, pq=pq: e.activation(out=sgb[:], in_=pq[:, 256:512], func=AF.Sigmoid), reads=[pqb], writes=[B("sgb")])
                    O("dve", lambda e, pw=pw: e.tensor_tensor(out=tma[:], in0=pw[:, 0:256], in1=sga[:], op=ALU.mult), reads=[pwbuf, B("sga")], writes=[B("tma")])
                    O("dve", lambda e, pw=pw: e.tensor_tensor(out=sgb[:], in0=pw[:, 256:512], in1=sgb[:], op=ALU.mult), reads=[pwbuf, B("sgb")], writes=[B("sgb")])
                    O("dve", lambda e, oc=oc: e.tensor_tensor(out=mT[:, oc, :], in0=tma[:], in1=sgb[:], op=ALU.add), reads=[B("tma"), B("sgb")], writes=[B("mT")])
                if STOP_AFTER == "A5":
                    pg.emit(final_wait_ops=[])
                    return nc
                for qh in range(2):
                    tt = 2 * i + qh
                    for ch in range(2):
                        pw = psW[ch]
                        pwbuf = B("psW%d" % ch)
                        for dc in range(8):
                            O("pe", lambda e, pw=pw, dc=dc, ch=ch, qh=qh: e.matmul(pw[:, :], lhsT=mT[:, dc, qh * 128:(qh + 1) * 128], rhs=wO[:, dc, ch * 512:(ch + 1) * 512],
                                                                                   start=(dc == 0), stop=(dc == 7)), reads=[B("mT"), B("wO")], writes=[pwbuf])
                        O("act", lambda e, pw=pw, ch=ch: e.activation(out=scrA["sq"][:, ch * 512:(ch + 1) * 512], in_=pw[:, :], func=AF.Square, accum_out=ssM[:, ch:ch + 1]),
                          reads=[pwbuf], writes=[B("Asq"), B("ssM")])
                        O("dve", lambda e, pw=pw, ch=ch: e.tensor_tensor(out=mg[:, ch * 512:(ch + 1) * 512], in0=pw[:, :], in1=g2b[:, ch * 512:(ch + 1) * 512], op=ALU.mult),
                          reads=[pwbuf, B("g2b"), B("ssM")], writes=[B("Axn")])
                    O("dve", lambda e: e.tensor_tensor(out=rsM[:], in0=ssM[:, 0:1], in1=ssM[:, 1:2], op=ALU.add), reads=[B("ssM")], writes=[B("rsM")])
                    O("act", lambda e: e.activation(out=rsM[:], in_=rsM[:], func=AF.Sqrt, scale=1.0 / D, bias=epsb[:]), reads=[B("rsM"), B("epsb")], writes=[B("rsM")])
                    O("dve", lambda e: e.reciprocal(out=rsM[:], in_=rsM[:]), reads=[B("rsM")], writes=[B("rsM")])
                    hs = sl
                    O("dve", lambda e, qh=qh, sl=sl: e.scalar_tensor_tensor(out=xq[sl][:, qh, :], in0=mg, scalar=rsM[:, 0:1], in1=xq[sl][:, qh, :], op0=ALU.mult, op1=ALU.add),
                      reads=[B("Axn"), B("rsM"), B("xq%d" % sl)], writes=[B("xq%d" % sl)])
                    st_ = O("sp", lambda e, sl=sl, qh=qh, tt=tt: e.dma_start(out=out_d[tt * 128:(tt + 1) * 128, :], in_=xq[sl][:, qh, :]), reads=[B("xq%d" % sl)],
                            writes=[B("out%d" % tt)], dma_key="h1st%d" % hs)
                    h1_stores.append(st_)
        pg.barrier(dma_ops=h1_stores)
        sKV.close()
        if STOP_AFTER == "A":
            pg.emit(final_wait_ops=h1_stores)
            return nc

        with contextlib.ExitStack() as sB:
            w1 = T(sB, "w1", [128, 8, 4096], BF16)
            w2 = T(sB, "w2", [128, 32, D], BF16)
            g4b = T(sB, "g4b", [128, D], F32)
            load("sp", g4b[:], g4_d.partition_broadcast(128), "g4b", "g4b")
            grp_last = []

            def w1_group(cc):
                aft = [grp_last[-2]] if len(grp_last) >= 2 else []
                for dc in range(8):
                    rows = slice(dc * 128, (dc + 1) * 128)
                    o_ = load("pool", w1[:, dc, cc * 1024:(cc + 1) * 1024], w_ff1_d[rows, cc * 1024:(cc + 1) * 1024], "w1_%d" % cc, "w1_%d" % cc,
                              after=aft if dc == 0 else ())
                grp_last.append(o_)

            def w2_group(fg):
                aft = [grp_last[-2]] if len(grp_last) >= 2 else []
                for fc in range(fg * 8, fg * 8 + 8):
                    rows = slice(fc * 128, (fc + 1) * 128)
                    o_ = load("pool", w2[:, fc, :], w_ff2_d[rows, :], "w2_%d" % fg, "w2_%d" % fg, after=aft if fc == fg * 8 else ())
                grp_last.append(o_)

            w1_group(0)
            w2_group(0)
            w1_group(1)
            w1_group(2)
            w1_group(3)
            w2_group(1)
            w2_group(2)
            w2_group(3)
            hx = [T(sB, "hx%d" % k, [128, 2, D], F32) for k in range(2)]
            fT = T(sB, "fT", [128, 8, 256], BF16)
            hT = T(sB, "hT", [128, 32, 256], BF16)
            rl2 = [T(sB, "rl2_%d" % k, [128, 512], BF16) for k in range(2)]
            scrB = {"sq": T(sB, "sqB", [128, D], BF16)[:], "ss": T(sB, "ssB", [128, 1], F32)[:], "rs": T(sB, "rsB", [128, 1], F32)[:],
                    "xn": T(sB, "xnB", [128, D], F32)[:], "eps": epsb[:]}
            mgB = scrB["xn"]
            ssF = T(sB, "ssF", [128, 2], F32)
            rsF = T(sB, "rsF", [128, 1], F32)
            psT = [PS(sB, "psTB%d" % k, [128, 512], F32) for k in range(2)]
            psH = [PS(sB, "psH%d" % k, [128, 512], F32) for k in range(3)]
            psW = [PS(sB, "psWB%d" % k, [128, 512], F32) for k in range(2)]
            psTb = [B("psTB0"), B("psTB1")]

            def issue_B_loads(gi):
                sl = gi % 2
                for t2 in range(2):
                    tt = gi * 2 + t2
                    load("sp", hx[sl][:, t2, :], out_d[tt * 128:(tt + 1) * 128, :], "hx%d" % sl, "hx%d" % sl)

            if STOP_AFTER == "B0":
                pg.emit(final_wait_ops=[])
                return nc
            issue_B_loads(0)
            hc = 0
            for gi in range(16):
                sl = gi % 2
                if gi + 1 < 16:
                    issue_B_loads(gi + 1)
                for t2 in range(2):
                    rmsnorm_T("B", hx[sl][:, t2, :], B("hx%d" % sl), g3sb,
                              lambda dc, t2=t2: fT[:, dc, t2 * 128:(t2 + 1) * 128], B("fT"), psT, psTb, scrB, "B", gbuf="gsb3")
                if STOP_AFTER == "B1":
                    pg.emit(final_wait_ops=[])
                    return nc
                for fc2 in range(16):
                    ph = psH[hc % 3]
                    phb = B("psH%d" % (hc % 3))
                    rlk = hc % 2
                    hc += 1
                    for u in range(2):
                        fc = fc2 * 2 + u
                        for dc in range(8):
                            O("pe", lambda e, ph=ph, dc=dc, fc=fc, u=u: e.matmul(ph[:, u * 256:(u + 1) * 256], lhsT=w1[:, dc, fc * 128:(fc + 1) * 128], rhs=fT[:, dc, :],
                                                                                 start=(dc == 0), stop=(dc == 7)), reads=[B("w1_%d" % (fc // 8)), B("fT")], writes=[phb])
                    O("act", lambda e, ph=ph, rlk=rlk: e.activation(out=rl2[rlk][:], in_=ph[:, :], func=AF.Relu), reads=[phb], writes=[B("rl2_%d" % rlk)])
                    O("dve", lambda e, ph=ph, fc2=fc2, rlk=rlk: e.scalar_tensor_tensor(out=hT[:, fc2 * 2:fc2 * 2 + 2, :].rearrange("p a b -> p (a b)"), in0=ph[:, :], scalar=0.0,
                                                                                         in1=rl2[rlk][:], op0=ALU.max, op1=ALU.mult),
                      reads=[phb, B("rl2_%d" % rlk)], writes=[B("hT")])
                if STOP_AFTER == "B2":
                    pg.emit(final_wait_ops=[])
                    return nc
                for t2 in range(2):
                    tt = gi * 2 + t2
                    for ch in range(2):
                        pw = psW[ch]
                        pwbuf = B("psWB%d" % ch)
                        for fc in range(32):
                            O("pe", lambda e, pw=pw, fc=fc, ch=ch, t2=t2: e.matmul(pw[:, :], lhsT=hT[:, fc, t2 * 128:(t2 + 1) * 128], rhs=w2[:, fc, ch * 512:(ch + 1) * 512],
                                                                                   start=(fc == 0), stop=(fc == 31)), reads=[B("hT"), B("w2_%d" % (fc // 8))], writes=[pwbuf])
                        O("act", lambda e, pw=pw, ch=ch: e.activation(out=scrB["sq"][:, ch * 512:(ch + 1) * 512], in_=pw[:, :], func=AF.Square, accum_out=ssF[:, ch:ch + 1]),
                          reads=[pwbuf], writes=[B("Bsq"), B("ssF")])
                        O("dve", lambda e, pw=pw, ch=ch: e.tensor_tensor(out=mgB[:, ch * 512:(ch + 1) * 512], in0=pw[:, :], in1=g4b[:, ch * 512:(ch + 1) * 512], op=ALU.mult),
                          reads=[pwbuf, B("g4b"), B("ssF")], writes=[B("Bxn")])
                    O("dve", lambda e: e.tensor_tensor(out=rsF[:], in0=ssF[:, 0:1], in1=ssF[:, 1:2], op=ALU.add), reads=[B("ssF")], writes=[B("rsF")])
                    O("act", lambda e: e.activation(out=rsF[:], in_=rsF[:], func=AF.Sqrt, scale=1.0 / D, bias=epsb[:]), reads=[B("rsF"), B("epsb")], writes=[B("rsF")])
                    O("dve", lambda e: e.reciprocal(out=rsF[:], in_=rsF[:]), reads=[B("rsF")], writes=[B("rsF")])
                    O("dve", lambda e, t2=t2, sl=sl: e.scalar_tensor_tensor(out=hx[sl][:, t2, :], in0=mgB, scalar=rsF[:, 0:1], in1=hx[sl][:, t2, :], op0=ALU.mult, op1=ALU.add),
                      reads=[B("Bxn"), B("rsF"), B("hx%d" % sl)], writes=[B("hx%d" % sl)])
                    st_ = O("sp", lambda e, sl=sl, t2=t2, tt=tt: e.dma_start(out=out_d[tt * 128:(tt + 1) * 128, :], in_=hx[sl][:, t2, :]), reads=[B("hx%d" % sl)],
                            writes=[B("out%d" % tt)], dma_key="ost%d" % sl)
                    final_stores.append(st_)
        pg.emit(final_wait_ops=final_stores)
    return nc


def _prep_shared(inp):
    f = lambda a: np.ascontiguousarray(np.asarray(a, dtype=np.float32))
    lam_re = f(inp["lam_re"])[0]
    lam_im = f(inp["lam_im"])[0]
    log_dt = f(inp["log_dt"])[0]
    b_re = f(inp["b_re"])[0]
    b_im = f(inp["b_im"])[0]
    c_re = f(inp["c_re"])[0]
    c_im = f(inp["c_im"])[0]
    d_skip = f(inp["d_skip"])[0]

    def pq(a):
        return f(a.reshape(16, 2, 64).transpose(1, 2, 0).reshape(128, 16))

    def gv(a):
        return f(a.reshape(8, 128).T)

    sh = {
        "w_in": f(inp["w_in"])[0],
        "w_glu": f(inp["w_glu"])[0],
        "w_a": f(inp["w_branch_a"])[0],
        "w_b": f(inp["w_branch_b"])[0],
        "w_out": f(inp["w_out"])[0],
        "w_ff1": f(inp["w_ff1"])[0],
        "w_ff2": f(inp["w_ff2"])[0],
        "g1": gv(f(inp["g_pre_mix"])[0]),
        "g3": gv(f(inp["g_pre_ffn"])[0]),
        "g2": f(inp["g_post_mix"]).reshape(1, D),
        "g4": f(inp["g_post_ffn"]).reshape(1, D),
        "bglu": f(f(inp["b_glu"])[0].reshape(4, 128).T),
        "LR": pq(lam_re),
        "LI": pq(lam_im),
        "LDT": f(np.broadcast_to(log_dt.reshape(16, 2).T[:, None, :], (2, 64, 16)).reshape(128, 16)),
        "BR": f(b_re.reshape(16, 2, 64, 16).transpose(1, 2, 0, 3).reshape(128, 256)),
        "BI": f(b_im.reshape(16, 2, 64, 16).transpose(1, 2, 0, 3).reshape(128, 256)),
        "CR": f(c_re.reshape(16, 2, 16, 64).transpose(1, 3, 0, 2).reshape(128, 256)),
        "CI": f(c_im.reshape(16, 2, 16, 64).transpose(1, 3, 0, 2).reshape(128, 256)),
        "DS": f(np.tile(d_skip.reshape(32, 16).T, (8, 1))),
    }
    return sh


def kernel(**inputs):
    x = np.asarray(inputs["x"], dtype=np.float32)
    sh = _prep_shared(inputs)
    nc = build_nc()
    in_maps = []
    for b in range(8):
        m = dict(sh)
        m["x"] = np.ascontiguousarray(x[b])
        in_maps.append(m)
    res = run_bass_kernel_spmd(nc, in_maps, core_ids=list(range(8)))
    out = np.stack([np.asarray(res.results[b]["out"], dtype=np.float32) for b in range(8)], axis=0)
    return out
```

```python
import contextlib
import math
import numpy as np
import concourse.bass as bass
import concourse.mybir as mybir
from concourse.bass_utils import run_bass_kernel_spmd

F32 = mybir.dt.float32
BF16 = mybir.dt.bfloat16
I32 = mybir.dt.int32
AF = mybir.ActivationFunctionType
ALU = mybir.AluOpType
AX = mybir.AxisListType

L = 4096
D = 1024
NEG = -1.0e30
EPS = 1e-6
ENGS = ("pe", "act", "dve", "pool", "sp")
STOP_AFTER = None
DBG_NOKS = False
DBG_NBLK = 16
DBG_QV = ""
DBG_ONLYQ = False
POOL_DMA_DEPTH = 8
DBG_NOYA = False
STRICT = False


class Buf:
    __slots__ = ("name", "last_w", "readers")

    def __init__(self, name):
        self.name = name
        self.last_w = None
        self.readers = []


class Op:
    __slots__ = ("eng", "fn", "deps", "idx", "is_dma", "dsem", "dval", "signal", "know")


class Prog:
    def __init__(self, nc):
        self.nc = nc
        self.ops = {e: [] for e in ENGS}
        self.all = []
        self.dma_sems = {}
        self.know = {e: {} for e in ENGS}

    def op(self, eng, fn, reads=(), writes=(), dma_key=None, extra_deps=()):
        o = Op()
        o.eng = eng
        o.fn = fn
        o.is_dma = dma_key is not None
        o.signal = False
        o.idx = len(self.ops[eng])
        cands = []
        for b in reads:
            if b.last_w is not None:
                cands.append((b.last_w, True))
        for b in writes:
            if b.last_w is not None:
                cands.append((b.last_w, False))
            for r in b.readers:
                cands.append((r, False))
        for p in extra_deps:
            cands.append((p, True))
        need = {}
        kn = self.know[eng]
        for (p, raw) in cands:
            if p.is_dma:
                if (not raw) and dma_key is not None and p.dsem == dma_key:
                    continue
                key = ("dma", p.dsem)
                val = p.dval
            else:
                if p.eng == eng and not raw and not STRICT:
                    continue
                key = ("eng", p.eng)
                val = p.idx
            if kn.get(key, -1) >= val:
                continue
            if key not in need or need[key][0] < val:
                need[key] = (val, p)
        o.deps = []
        for key, (val, p) in need.items():
            o.deps.append(p)
            if kn.get(key, -1) < val:
                kn[key] = val
            for k2, v2 in p.know.items():
                if kn.get(k2, -1) < v2:
                    kn[k2] = v2
        if o.is_dma:
            cnt = self.dma_sems.setdefault(dma_key, [0])
            cnt[0] += 1
            o.dsem = dma_key
            o.dval = cnt[0]
        o.know = dict(kn)
        if o.is_dma:
            o.know[("dma", o.dsem)] = o.dval
        else:
            o.know[("eng", eng)] = o.idx
        for b in reads:
            b.readers.append(o)
        for b in writes:
            b.last_w = o
            b.readers = []
        self.ops[eng].append(o)
        self.all.append(o)
        return o

    def barrier(self, dma_ops=()):
        lasts = [self.ops[e][-1] for e in ENGS if self.ops[e] and not self.ops[e][-1].is_dma]
        lasts = []
        for e in ENGS:
            for o in reversed(self.ops[e]):
                if not o.is_dma:
                    lasts.append(o)
                    break
        deps = list(lasts) + list(dma_ops)
        for e in ENGS:
            self.op(e, lambda eng: eng.nop(), extra_deps=deps)

    def emit(self, final_wait_ops=()):
        nc = self.nc
        for o in self.all:
            for p in o.deps:
                if not p.is_dma:
                    p.signal = True
        sigval = {}
        for e in ENGS:
            c = 0
            for o in self.ops[e]:
                if o.signal:
                    c += 1
                sigval[(e, o.idx)] = c
        with contextlib.ExitStack() as st:
            esem = {e: st.enter_context(nc.semaphore("sem_" + e)) for e in ENGS}
            dsem = {k: st.enter_context(nc.semaphore("dsem%d" % i)) for i, k in enumerate(self.dma_sems)}
            block = st.enter_context(nc.Block())
            engobj = {"pe": block.tensor, "act": block.scalar, "dve": block.vector,
                      "pool": block.gpsimd, "sp": block.sync}

            def run(ename, eng):
                for o in self.ops[ename]:
                    for p in o.deps:
                        if p.is_dma:
                            eng.wait_ge(dsem[p.dsem], 16 * p.dval)
                        else:
                            eng.wait_ge(esem[p.eng], sigval[(p.eng, p.idx)])
                    ins = o.fn(eng)
                    if o.is_dma:
                        ins.then_inc(dsem[o.dsem], 16)
                    elif o.signal:
                        ins.then_inc(esem[ename], 1)
                if ename == "sp":
                    fw = {}
                    for p in final_wait_ops:
                        fw[p.dsem] = max(fw.get(p.dsem, 0), p.dval)
                    for k, v in fw.items():
                        eng.wait_ge(dsem[k], 16 * v)

            for ename in ENGS:
                engobj[ename](lambda eng, ename=ename: run(ename, eng))


def build_nc(dbg=False):
    nc = bass.Bass("TRN2", target_bir_lowering=False)

    def din(name, shape, dt=F32):
        return nc.dram_tensor(name, list(shape), dt, kind="ExternalInput").ap()

    x_d = din("x", [L, D])
    w_in_d = din("w_in", [D, 4096])
    w_glu_d = din("w_glu", [512, 512])
    w_a_d = din("w_a", [512, D])
    w_b_d = din("w_b", [512, D])
    w_out_d = din("w_out", [D, D])
    w_ff1_d = din("w_ff1", [D, 4096])
    w_ff2_d = din("w_ff2", [4096, D])
    g1_d = din("g1", [128, 8])
    g3_d = din("g3", [128, 8])
    g2_d = din("g2", [1, D])
    g4_d = din("g4", [1, D])
    bglu_d = din("bglu", [128, 4])
    LR_d = din("LR", [128, 16])
    LI_d = din("LI", [128, 16])
    LDT_d = din("LDT", [128, 16])
    BR_d = din("BR", [128, 256])
    BI_d = din("BI", [128, 256])
    CR_d = din("CR", [128, 256])
    CI_d = din("CI", [128, 256])
    DS_d = din("DS", [128, 32])
    out_d = nc.dram_tensor("out", [L, D], F32, kind="ExternalOutput").ap()
    ya_d = nc.dram_tensor("ya_scr", [512, L], BF16, kind="ExternalOutput" if dbg else "Internal").ap()

    if dbg:
        dbgb_d = nc.dram_tensor("dbgb", [128, 8192], BF16, kind="ExternalOutput").ap()
        dbgf_d = nc.dram_tensor("dbgf", [128, 4096], F32, kind="ExternalOutput").ap()
    pg = Prog(nc)
    O = pg.op
    bufs = {}

    def B(name):
        if name not in bufs:
            bufs[name] = Buf(name)
        return bufs[name]

    final_stores = []
    TWO_PI = 2.0 * math.pi

    with contextlib.ExitStack() as top:
        def T(st, name, shape, dt):
            return st.enter_context(nc.sbuf_tensor("s_" + name, list(shape), dt))

        def PS(st, name, shape, dt):
            return st.enter_context(nc.psum_tensor("p_" + name, list(shape), dt))

        identf = T(top, "identf", [128, 128], F32)
        identb = T(top, "identb", [128, 128], BF16)
        O("pool", lambda e: e.memset(identf[:], 1.0), writes=[B("identf")])
        O("pool", lambda e: e.affine_select(out=identf[:], in_=identf[:], pattern=[[1, 128]], compare_op=ALU.is_equal,
                                            fill=0.0, base=0, channel_multiplier=-1), reads=[B("identf")], writes=[B("identf")])
        O("dve", lambda e: e.tensor_copy(out=identb[:], in_=identf[:]), reads=[B("identf")], writes=[B("identb")])

        pool_dmas = []

        def load(eng, dst, src, bname, key=None, after=()):
            ex = tuple(after)
            o = O(eng, lambda e: e.dma_start(out=dst, in_=src), writes=[B(bname)], dma_key=key or bname, extra_deps=ex)
            if eng == "pool":
                pool_dmas.append(o)
            return o

        def rmsnorm_T(st_name, xt, xbuf, gsb, uT_dst_fn, ubuf, ps_list, psbufs, scr, pfx, gbuf="gsb"):
            sq, ss, rs, xn = scr["sq"], scr["ss"], scr["rs"], scr["xn"]
            O("act", lambda e: e.activation(out=sq, in_=xt, func=AF.Square, accum_out=ss), reads=[xbuf], writes=[B(pfx + "sq"), B(pfx + "ss")])
            O("act", lambda e: e.activation(out=rs, in_=ss, func=AF.Sqrt, scale=1.0 / D, bias=scr["eps"]), reads=[B(pfx + "ss"), B("epsb")], writes=[B(pfx + "rs")])
            O("dve", lambda e: e.reciprocal(out=rs, in_=rs), reads=[B(pfx + "rs")], writes=[B(pfx + "rs")])
            O("dve", lambda e: e.tensor_scalar(out=xn, in0=xt, scalar1=rs, scalar2=None, op0=ALU.mult), reads=[xbuf, B(pfx + "rs")], writes=[B(pfx + "xn")])
            for half in range(2):
                ps = ps_list[half]
                pb = psbufs[half]
                for k in range(4):
                    dc = half * 4 + k
                    O("pe", lambda e, dc=dc, k=k, ps=ps: e.transpose(out=ps[:, k * 128:(k + 1) * 128], in_=xn[:, dc * 128:(dc + 1) * 128], identity=identf[:]),
                      reads=[B(pfx + "xn"), B("identf")], writes=[pb])
                for k in range(4):
                    dc = half * 4 + k
                    eng = "act" if k % 2 == 0 else "dve"
                    if eng == "act":
                        O("act", lambda e, dc=dc, k=k, ps=ps: e.activation(out=uT_dst_fn(dc), in_=ps[:, k * 128:(k + 1) * 128], func=AF.Copy, scale=gsb[:, dc:dc + 1]),
                          reads=[pb, B(gbuf)], writes=[ubuf])
                    else:
                        O("dve", lambda e, dc=dc, k=k, ps=ps: e.tensor_scalar(out=uT_dst_fn(dc), in0=ps[:, k * 128:(k + 1) * 128], scalar1=gsb[:, dc:dc + 1], scalar2=None, op0=ALU.mult),
                          reads=[pb, B(gbuf)], writes=[ubuf])

        epsb = T(top, "epsb", [128, 1], F32)
        O("dve", lambda e: e.memset(epsb[:], EPS), writes=[B("epsb")])
        g1sb = T(top, "g1sb", [128, 8], F32)
        g3sb = T(top, "g3sb", [128, 8], F32)
        bglu = T(top, "bglu", [128, 4], F32)
        load("sp", g1sb[:], g1_d, "gsb", "c0")
        load("sp", g3sb[:], g3_d, "gsb3", "c1")
        load("sp", bglu[:], bglu_d, "bglu", "c2")

        sKV = top.enter_context(contextlib.ExitStack())
        KT = T(sKV, "KT", [128, 4, L], BF16)
        VA = T(sKV, "VA", [128, 32, 8, 66], BF16)
        KS = T(sKV, "KS", [128, 4, 16], F32)
        O("pool", lambda e: e.memset(VA[:, :, :, 64:65], 1.0), writes=[B("VAones")])
        O("pool", lambda e: e.memset(VA[:, :, :, 65:66], 0.0), writes=[B("VAzeros")])

        with contextlib.ExitStack() as sS:
            MI = T(sS, "MI", [128, 32, 128], BF16)
            MinR = T(sS, "MinR", [128, 32, 64], BF16)
            MinI = T(sS, "MinI", [128, 32, 64], BF16)
            MoRp = T(sS, "MoRp", [128, 16, 2, 128], BF16)
            MoIp = T(sS, "MoIp", [128, 16, 2, 128], BF16)
            RHO8 = T(sS, "RHO8", [128, 16], F32)
            TH8 = T(sS, "TH8", [128, 16], F32)

            with contextlib.ExitStack() as s0:
                LR = T(s0, "LR", [128, 16], F32)
                LI = T(s0, "LI", [128, 16], F32)
                DT = T(s0, "DT", [128, 16], F32)
                BR = T(s0, "BR", [128, 16, 16], F32)
                BI = T(s0, "BI", [128, 16, 16], F32)
                CR = T(s0, "CR", [128, 16, 16], F32)
                CI = T(s0, "CI", [128, 16, 16], F32)
                DS = T(s0, "DS", [128, 32], F32)
                NVi = T(s0, "NVi", [128, 17], I32)
                NV = T(s0, "NV", [128, 17], F32)
                LRDT = T(s0, "LRDT", [128, 16], F32)
                TH = T(s0, "TH", [128, 16], F32)
                ARG = T(s0, "ARG", [128, 16, 17], F32)
                ARK = T(s0, "ARK", [128, 16, 17], F32)
                ARKi = T(s0, "ARKi", [128, 16, 17], I32)
                MAG = T(s0, "MAG", [128, 16, 17], F32)
                PWR = T(s0, "PWR", [128, 16, 17], F32)
                PWI = T(s0, "PWI", [128, 16, 17], F32)
                zt1 = T(s0, "zt1", [128, 16], F32)
                zt2 = T(s0, "zt2", [128, 16], F32)
                zt3 = T(s0, "zt3", [128, 16], F32)
                cfr = T(s0, "cfr", [128, 16], F32)
                cfi = T(s0, "cfi", [128, 16], F32)
                BBR = T(s0, "BBR", [128, 16, 16], F32)
                BBI = T(s0, "BBI", [128, 16, 16], F32)
                tb = T(s0, "tb", [128, 16, 16], F32)
                HR = T(s0, "HR", [128, 16, 8, 16], F32)
                HI = T(s0, "HI", [128, 16, 8, 16], F32)
                GR = T(s0, "GR", [128, 16, 8, 16], F32)
                GI = T(s0, "GI", [128, 16, 8, 16], F32)
                MOR = T(s0, "MOR", [128, 16, 8, 16], F32)
                MOI = T(s0, "MOI", [128, 16, 8, 16], F32)
                tq = T(s0, "tq", [128, 16, 8, 16], F32)
                msk = T(s0, "msk", [128, 8, 16], F32)
                tmi = T(s0, "tmi", [128, 128], F32)
                ps_s0 = [PS(s0, "ps_s0_%d" % k, [128, 512], F32) for k in range(4)]

                load("sp", LR[:], LR_d, "LR", "c3")
                load("sp", LI[:], LI_d, "LI", "c4")
                load("sp", DT[:], LDT_d, "DT", "c5")
                load("sp", BR[:].rearrange("p a b -> p (a b)"), BR_d, "BR", "c6")
                load("sp", BI[:].rearrange("p a b -> p (a b)"), BI_d, "BI", "c7")
                load("sp", CR[:].rearrange("p a b -> p (a b)"), CR_d, "CR", "c8")
                load("sp", CI[:].rearrange("p a b -> p (a b)"), CI_d, "CI", "c9")
                load("sp", DS[:], DS_d, "DS", "c10")

                def V_(fn, r, w, eng="dve"):
                    return O(eng, fn, reads=[B(n) for n in r], writes=[B(n) for n in w])

                V_(lambda e: e.iota(NVi[:, 0:8], pattern=[[-1, 8]], base=-1, channel_multiplier=0), [], ["NVi"], "pool")
                V_(lambda e: e.iota(NVi[:, 8:17], pattern=[[1, 9]], base=0, channel_multiplier=0), [], ["NVi"], "pool")
                V_(lambda e: e.tensor_copy(out=NV[:], in_=NVi[:]), ["NVi"], ["NV"])
                V_(lambda e: e.activation(out=DT[:], in_=DT[:], func=AF.Exp), ["DT"], ["DT"], "act")
                V_(lambda e: e.tensor_tensor(out=LRDT[:], in0=LR[:], in1=DT[:], op=ALU.mult), ["LR", "DT"], ["LRDT"])
                V_(lambda e: e.tensor_tensor(out=TH[:], in0=LI[:], in1=DT[:], op=ALU.mult), ["LI", "DT"], ["TH"])
                bc3 = lambda a: a.unsqueeze(2).to_broadcast([128, 16, 17])
                nvb = NV[:].unsqueeze(1).to_broadcast([128, 16, 17])
                V_(lambda e: e.tensor_tensor(out=ARG[:], in0=bc3(LRDT[:]), in1=nvb, op=ALU.mult), ["LRDT", "NV"], ["ARG"])
                V_(lambda e: e.activation(out=MAG[:], in_=ARG[:], func=AF.Exp), ["ARG"], ["MAG"], "act")

                def sin_of(dst, src_fn, srcbufs, dstbuf, shift, shape3):
                    scrF, scrI = shape3
                    V_(lambda e: e.tensor_scalar(out=scrF, in0=src_fn(), scalar1=shift, scalar2=1.0 / TWO_PI, op0=ALU.add, op1=ALU.mult), srcbufs, ["scrF"])
                    V_(lambda e: e.tensor_copy(out=scrI, in_=scrF), ["scrF"], ["scrI"])
                    V_(lambda e: e.tensor_copy(out=scrF, in_=scrI), ["scrI"], ["scrF"])
                    V_(lambda e: e.tensor_scalar(out=scrF, in0=scrF, scalar1=-TWO_PI, scalar2=shift, op0=ALU.mult, op1=ALU.add), ["scrF"], ["scrF"])
                    V_(lambda e: e.tensor_tensor(out=scrF, in0=scrF, in1=src_fn(), op=ALU.add), ["scrF"] + srcbufs, ["scrF"])
                    V_(lambda e: e.tensor_scalar(out=scrF, in0=scrF, scalar1=3.14159, scalar2=-3.14159, op0=ALU.min, op1=ALU.max), ["scrF"], ["scrF"])
                    V_(lambda e: e.activation(out=dst, in_=scrF, func=AF.Sin), ["scrF"], [dstbuf], "act")

                V_(lambda e: e.tensor_tensor(out=ARG[:], in0=bc3(TH[:]), in1=nvb, op=ALU.mult), ["TH", "NV", "MAG"], ["ARG"])
                sin_of(PWI[:], lambda: ARG[:], ["ARG"], "PWI", 0.0, (ARK[:], ARKi[:]))
                sin_of(PWR[:], lambda: ARG[:], ["ARG"], "PWR", math.pi / 2, (ARK[:], ARKi[:]))
                V_(lambda e: e.tensor_tensor(out=PWR[:], in0=PWR[:], in1=MAG[:], op=ALU.mult), ["PWR", "MAG"], ["PWR"])
                V_(lambda e: e.tensor_tensor(out=PWI[:], in0=PWI[:], in1=MAG[:], op=ALU.mult), ["PWI", "MAG"], ["PWI"])
                V_(lambda e: e.tensor_copy(out=RHO8[:], in_=MAG[:, :, 16]), ["MAG"], ["RHO8"])
                V_(lambda e: e.tensor_scalar(out=TH8[:], in0=TH[:], scalar1=8.0, scalar2=None, op0=ALU.mult), ["TH"], ["TH8"])
                abr = PWR[:, :, 9]
                abi = PWI[:, :, 9]
                V_(lambda e: e.tensor_scalar(out=zt1[:], in0=abr, scalar1=-1.0, scalar2=None, op0=ALU.add), ["PWR"], ["zt1"])
                V_(lambda e: e.tensor_tensor(out=zt2[:], in0=LR[:], in1=LR[:], op=ALU.mult), ["LR"], ["zt2"])
                V_(lambda e: e.tensor_tensor(out=zt3[:], in0=LI[:], in1=LI[:], op=ALU.mult), ["LI"], ["zt3"])
                V_(lambda e: e.tensor_tensor(out=zt2[:], in0=zt2[:], in1=zt3[:], op=ALU.add), ["zt2", "zt3"], ["zt2"])
                V_(lambda e: e.reciprocal(out=zt2[:], in_=zt2[:]), ["zt2"], ["zt2"])
                V_(lambda e: e.tensor_tensor(out=cfr[:], in0=zt1[:], in1=LR[:], op=ALU.mult), ["zt1", "LR"], ["cfr"])
                V_(lambda e: e.tensor_tensor(out=zt3[:], in0=abi, in1=LI[:], op=ALU.mult), ["PWI", "LI"], ["zt3"])
                V_(lambda e: e.tensor_tensor(out=cfr[:], in0=cfr[:], in1=zt3[:], op=ALU.add), ["cfr", "zt3"], ["cfr"])
                V_(lambda e: e.tensor_tensor(out=cfr[:], in0=cfr[:], in1=zt2[:], op=ALU.mult), ["cfr", "zt2"], ["cfr"])
                V_(lambda e: e.tensor_tensor(out=cfi[:], in0=abi, in1=LR[:], op=ALU.mult), ["PWI", "LR"], ["cfi"])
                V_(lambda e: e.tensor_tensor(out=zt3[:], in0=zt1[:], in1=LI[:], op=ALU.mult), ["zt1", "LI"], ["zt3"])
                V_(lambda e: e.tensor_tensor(out=cfi[:], in0=cfi[:], in1=zt3[:], op=ALU.subtract), ["cfi", "zt3"], ["cfi"])
                V_(lambda e: e.tensor_tensor(out=cfi[:], in0=cfi[:], in1=zt2[:], op=ALU.mult), ["cfi", "zt2"], ["cfi"])
                bh = lambda a: a.unsqueeze(2).to_broadcast([128, 16, 16])
                V_(lambda e: e.tensor_tensor(out=BBR[:], in0=bh(cfr[:]), in1=BR[:], op=ALU.mult), ["cfr", "BR"], ["BBR"])
                V_(lambda e: e.tensor_tensor(out=tb[:], in0=bh(cfi[:]), in1=BI[:], op=ALU.mult), ["cfi", "BI"], ["tb"])
                V_(lambda e: e.tensor_tensor(out=BBR[:], in0=BBR[:], in1=tb[:], op=ALU.subtract), ["BBR", "tb"], ["BBR"])
                V_(lambda e: e.tensor_tensor(out=BBI[:], in0=bh(cfr[:]), in1=BI[:], op=ALU.mult), ["cfr", "BI"], ["BBI"])
                V_(lambda e: e.tensor_tensor(out=tb[:], in0=bh(cfi[:]), in1=BR[:], op=ALU.mult), ["cfi", "BR"], ["tb"])
                V_(lambda e: e.tensor_tensor(out=BBI[:], in0=BBI[:], in1=tb[:], op=ALU.add), ["BBI", "tb"], ["BBI"])
                pwb = lambda a, lo: a[:, :, lo:lo + 8].unsqueeze(3).to_broadcast([128, 16, 8, 16])
                b4 = lambda a: a.unsqueeze(2).to_broadcast([128, 16, 8, 16])
                V_(lambda e: e.tensor_tensor(out=HR[:], in0=pwb(PWR, 0), in1=b4(BBR[:]), op=ALU.mult), ["PWR", "BBR"], ["HR"])
                V_(lambda e: e.tensor_tensor(out=tq[:], in0=pwb(PWI, 0), in1=b4(BBI[:]), op=ALU.mult), ["PWI", "BBI"], ["tq"])
                V_(lambda e: e.tensor_tensor(out=HR[:], in0=HR[:], in1=tq[:], op=ALU.subtract), ["HR", "tq"], ["HR"])
                V_(lambda e: e.tensor_tensor(out=HI[:], in0=pwb(PWR, 0), in1=b4(BBI[:]), op=ALU.mult), ["PWR", "BBI"], ["HI"])
                V_(lambda e: e.tensor_tensor(out=tq[:], in0=pwb(PWI, 0), in1=b4(BBR[:]), op=ALU.mult), ["PWI", "BBR"], ["tq"])
                V_(lambda e: e.tensor_tensor(out=HI[:], in0=HI[:], in1=tq[:], op=ALU.add), ["HI", "tq"], ["HI"])
                f3 = lambda a: a.rearrange("p q i h -> p q (i h)")
                a8 = lambda a: a[:, :, 16:17].to_broadcast([128, 16, 128])
                V_(lambda e: e.tensor_tensor(out=f3(GR[:]), in0=a8(PWR), in1=f3(HR[:]), op=ALU.mult), ["PWR", "HR"], ["GR"])
                V_(lambda e: e.tensor_tensor(out=f3(tq[:]), in0=a8(PWI), in1=f3(HI[:]), op=ALU.mult), ["PWI", "HI"], ["tq"])
                V_(lambda e: e.tensor_tensor(out=GR[:], in0=GR[:], in1=tq[:], op=ALU.subtract), ["GR", "tq"], ["GR"])
                V_(lambda e: e.tensor_tensor(out=f3(GI[:]), in0=a8(PWR), in1=f3(HI[:]), op=ALU.mult), ["PWR", "HI"], ["GI"])
                V_(lambda e: e.tensor_tensor(out=f3(tq[:]), in0=a8(PWI), in1=f3(HR[:]), op=ALU.mult), ["PWI", "HR"], ["tq"])
                V_(lambda e: e.tensor_tensor(out=GI[:], in0=GI[:], in1=tq[:], op=ALU.add), ["GI", "tq"], ["GI"])
                V_(lambda e: e.tensor_tensor(out=MOR[:], in0=pwb(PWR, 9), in1=b4(CR[:]), op=ALU.mult), ["PWR", "CR"], ["MOR"])
                V_(lambda e: e.tensor_tensor(out=tq[:], in0=pwb(PWI, 9), in1=b4(CI[:]), op=ALU.mult), ["PWI", "CI"], ["tq"])
                V_(lambda e: e.tensor_tensor(out=MOR[:], in0=MOR[:], in1=tq[:], op=ALU.subtract), ["MOR", "tq"], ["MOR"])
                V_(lambda e: e.tensor_tensor(out=MOI[:], in0=pwb(PWI, 9), in1=b4(CR[:]), op=ALU.mult), ["PWI", "CR"], ["MOI"])
                V_(lambda e: e.tensor_tensor(out=tq[:], in0=pwb(PWR, 9), in1=b4(CI[:]), op=ALU.mult), ["PWR", "CI"], ["tq"])
                V_(lambda e: e.tensor_tensor(out=MOI[:], in0=MOI[:], in1=tq[:], op=ALU.add), ["MOI", "tq"], ["MOI"])
                V_(lambda e: e.tensor_scalar(out=MOI[:], in0=MOI[:], scalar1=-1.0, scalar2=None, op0=ALU.mult), ["MOI"], ["MOI"])
                V_(lambda e: e.memset(MoRp[:], 0.0), [], ["MoRp"], "pool")
                V_(lambda e: e.memset(MoIp[:], 0.0), [], ["MoIp"], "pool")
                for m in range(2):
                    rows = slice(64 * m, 64 * m + 64)
                    V_(lambda e, rows=rows, m=m: e.tensor_copy(out=MoRp[rows, :, m, :], in_=f3(MOR[:])[rows, :, :]), ["MOR", "MoRp"], ["MoRp"])
                    V_(lambda e, rows=rows, m=m: e.tensor_copy(out=MoIp[rows, :, m, :], in_=f3(MOI[:])[rows, :, :]), ["MOI", "MoIp"], ["MoIp"])
                V_(lambda e: e.memset(msk[:], 1.0), [], ["msk"], "pool")
                V_(lambda e: e.affine_select(out=msk[:], in_=msk[:], pattern=[[16, 8], [0, 16]], compare_op=ALU.is_ge,
                                             fill=0.0, base=15, channel_multiplier=-1), ["msk"], ["msk"], "pool")
                for g in range(32):
                    q, m = g // 2, g % 2
                    rows = slice(64 * m, 64 * m + 64)
                    ps = ps_s0[g % 4]
                    pb = B("ps_s0_%d" % (g % 4))
                    O("pe", lambda e, ps=ps, rows=rows, q=q: e.transpose(out=ps[:, 0:64], in_=f3(GR[:])[rows, q, :], identity=identf[rows, rows]),
                      reads=[B("GR"), B("identf")], writes=[pb])
                    O("pe", lambda e, ps=ps, rows=rows, q=q: e.transpose(out=ps[:, 64:128], in_=f3(GI[:])[rows, q, :], identity=identf[rows, rows]),
                      reads=[B("GI"), B("identf")], writes=[pb])
                    O("pe", lambda e, ps=ps, rows=rows, q=q: e.matmul(ps[:, 128:256], lhsT=f3(HR[:])[rows, q, :], rhs=f3(MOR[:])[rows, q, :], start=True, stop=False),
                      reads=[B("HR"), B("MOR")], writes=[pb])
                    O("pe", lambda e, ps=ps, rows=rows, q=q: e.matmul(ps[:, 128:256], lhsT=f3(HI[:])[rows, q, :], rhs=f3(MOI[:])[rows, q, :], start=False, stop=True),
                      reads=[B("HI"), B("MOI")], writes=[pb])
                    O("act", lambda e, ps=ps, g=g: e.activation(func=AF.Identity, out=MinR[:, g, :], in_=ps[:, 0:64]), reads=[pb], writes=[B("MinR")])
                    O("act", lambda e, ps=ps, g=g: e.activation(func=AF.Identity, out=MinI[:, g, :], in_=ps[:, 64:128]), reads=[pb], writes=[B("MinI")])
                    O("dve", lambda e, ps=ps: e.tensor_tensor(out=tmi[:], in0=ps[:, 128:256], in1=msk[:].rearrange("p j h -> p (j h)"), op=ALU.mult),
                      reads=[pb, B("msk")], writes=[B("tmi")])
                    O("dve", lambda e, g=g: e.scalar_tensor_tensor(out=MI[:, g, :], in0=identf[:], scalar=DS[:, g:g + 1], in1=tmi[:], op0=ALU.mult, op1=ALU.add),
                      reads=[B("identf"), B("DS"), B("tmi")], writes=[B("MI")])
                if STOP_AFTER == "S0":
                    d1 = O("sp", lambda e: e.dma_start(out=dbgb_d[:, 0:4096], in_=MI[:].rearrange("p g f -> p (g f)")), reads=[B("MI")], dma_key="dbg1")
                    d2 = O("sp", lambda e: e.dma_start(out=dbgb_d[:, 4096:6144], in_=MinR[:].rearrange("p g f -> p (g f)")), reads=[B("MinR")], dma_key="dbg2")
                    d3 = O("sp", lambda e: e.dma_start(out=dbgb_d[:, 6144:8192], in_=MoRp[:, 0:8, :, :].rearrange("p q m f -> p (q m f)")), reads=[B("MoRp")], dma_key="dbg3")
                    d4 = O("sp", lambda e: e.dma_start(out=dbgf_d[:, 0:272], in_=PWR[:].rearrange("p q n -> p (q n)")), reads=[B("PWR")], dma_key="dbg4")
                    d5 = O("sp", lambda e: e.dma_start(out=dbgf_d[:, 272:544], in_=PWI[:].rearrange("p q n -> p (q n)")), reads=[B("PWI")], dma_key="dbg5")
                    d6 = O("sp", lambda e: e.dma_start(out=dbgf_d[:, 544:2592], in_=HR[:].rearrange("p q i h -> p (q i h)")), reads=[B("HR")], dma_key="dbg6")
                    pg.emit(final_wait_ops=[d1, d2, d3, d4, d5, d6])
                    return nc
            pg.barrier()

            with contextlib.ExitStack() as s1:
                Zt = T(s1, "Zt", [128, 4, 32, 8, 16], BF16)
                wS = T(s1, "wS", [128, 8, 512], BF16)
                wK = T(s1, "wK", [128, 8, 512], BF16)
                wV = T(s1, "wV", [128, 8, 512], BF16)
                wG = T(s1, "wG", [128, 4, 512], BF16)
                for dc in range(8):
                    rows = slice(dc * 128, (dc + 1) * 128)
                    load("pool", wS[:, dc, :], w_in_d[rows, 0:512], "wS", "wS")
                    load("pool", wK[:, dc, :], w_in_d[rows, 1024:1536], "wK", "wK")
                    load("pool", wV[:, dc, :], w_in_d[rows, 1536:2048], "wV", "wV")
                for fc in range(4):
                    load("pool", wG[:, fc, :], w_glu_d[fc * 128:(fc + 1) * 128, :], "wG", "wG")

                if STOP_AFTER == "S1a0":
                    d1 = O("sp", lambda e: e.dma_start(out=dbgb_d[:, 0:4096], in_=wS[:].rearrange("p a b -> p (a b)")), reads=[B("wS")], dma_key="dbg1")
                    d2 = O("sp", lambda e: e.dma_start(out=dbgb_d[:, 4096:6144], in_=wG[:].rearrange("p a b -> p (a b)")), reads=[B("wG")], dma_key="dbg2")
                    pg.emit(final_wait_ops=[d1, d2])
                    return nc
                with contextlib.ExitStack() as s1a:
                    uT = T(s1a, "uT", [128, 8, 1024], BF16)
                    xt = [T(s1a, "xt%d" % k, [128, D], F32) for k in range(2)]
                    scr = {"sq": T(s1a, "sq", [128, D], BF16)[:], "ss": T(s1a, "ss", [128, 1], F32)[:], "rs": T(s1a, "rs", [128, 1], F32)[:],
                           "xn": T(s1a, "xn", [128, D], F32)[:], "eps": epsb[:]}
                    psT = [PS(s1a, "psT%d" % k, [128, 512], F32) for k in range(2)]
                    psM = [PS(s1a, "psM%d" % k, [128, 512], F32) for k in range(4)]
                    psTb = [B("psT0"), B("psT1")]
                    nmm = [0]

                    def next_psM():
                        k = nmm[0] % 4
                        nmm[0] += 1
                        return psM[k], B("psM%d" % k)

                    x_loads = {}

                    def issue_xload(tt):
                        slot = tt % 2
                        x_loads[tt] = load("sp", xt[slot][:], x_d[tt * 128:(tt + 1) * 128, :], "xt%d" % slot, "xt%d" % slot)

                    issue_xload(0)
                    for s in range(4):
                        for t8 in range(8):
                            tt = s * 8 + t8
                            if tt + 1 < 32:
                                issue_xload(tt + 1)
                            slot = tt % 2
                            rmsnorm_T("S", xt[slot][:], B("xt%d" % slot), g1sb,
                                      lambda dc, t8=t8: uT[:, dc, t8 * 128:(t8 + 1) * 128], B("uT"), psT, psTb, scr, "S")
                        if STOP_AFTER == "S1a1":
                            d1 = O("sp", lambda e: e.dma_start(out=dbgb_d[:, 0:8192], in_=uT[:].rearrange("p a b -> p (a b)")), reads=[B("uT"), B("Zt0"), B("KT"), B("VA"), B("KS")], dma_key="dbg1")
                            pg.emit(final_wait_ops=[d1])
                            return nc
                        for i in range(8):
                            ps, pb = next_psM()
                            for dc in range(8):
                                O("pe", lambda e, ps=ps, dc=dc, i=i: e.matmul(ps[:, :], lhsT=uT[:, dc, :].rearrange("p (c i) -> p i c", i=8)[:, i, :],
                                                                               rhs=wS[:, dc, :], start=(dc == 0), stop=(dc == 7)),
                                  reads=[B("uT"), B("wS")], writes=[pb])
                            eng = "act" if i % 2 == 0 else "dve"
                            if eng == "act":
                                O("act", lambda e, ps=ps, s=s, i=i: e.activation(func=AF.Identity, out=Zt[:, s, :, i, :], in_=ps[:, :].rearrange("p (g h) -> p g h", g=32)), reads=[pb], writes=[B("Zt%d" % q_) for q_ in range(16)])
                            else:
                                O("dve", lambda e, ps=ps, s=s, i=i: e.tensor_copy(out=Zt[:, s, :, i, :], in_=ps[:, :].rearrange("p (g h) -> p g h", g=32)), reads=[pb], writes=[B("Zt%d" % q_) for q_ in range(16)])
                        if STOP_AFTER == "S1a2":
                            d1 = O("sp", lambda e: e.dma_start(out=dbgb_d[:, 0:8192], in_=uT[:].rearrange("p a b -> p (a b)")), reads=[B("uT"), B("Zt0"), B("KT"), B("VA"), B("KS")], dma_key="dbg1")
                            pg.emit(final_wait_ops=[d1])
                            return nc
                        for hp in range(4):
                            for th in range(2):
                                ps, pb = next_psM()
                                for dc in range(8):
                                    O("pe", lambda e, ps=ps, dc=dc, hp=hp, th=th: e.matmul(ps[:, :], lhsT=wK[:, dc, hp * 128:(hp + 1) * 128],
                                                                                           rhs=uT[:, dc, th * 512:(th + 1) * 512], start=(dc == 0), stop=(dc == 7)),
                                      reads=[B("uT"), B("wK")], writes=[pb])
                                tok0 = s * 1024 + th * 512
                                for bb in range(2):
                                    blk = tok0 // 256 + bb
                                    O("act", lambda e, ps=ps, hp=hp, tok0=tok0, bb=bb, blk=blk: e.activation(
                                        out=KT[:, hp, tok0 + bb * 256:tok0 + (bb + 1) * 256], in_=ps[:, bb * 256:(bb + 1) * 256], func=AF.Copy,
                                        accum_out=KS[:, hp, blk:blk + 1]), reads=[pb], writes=[B("KT"), B("KS")])
                        if STOP_AFTER == "S1a3":
                            d1 = O("sp", lambda e: e.dma_start(out=dbgb_d[:, 0:8192], in_=uT[:].rearrange("p a b -> p (a b)")), reads=[B("uT"), B("Zt0"), B("KT"), B("VA"), B("KS")], dma_key="dbg1")
                            pg.emit(final_wait_ops=[d1])
                            return nc
                        for t8 in range(8):
                            tt = s * 8 + t8
                            ps, pb = next_psM()
                            for dc in range(8):
                                O("pe", lambda e, ps=ps, dc=dc, t8=t8: e.matmul(ps[:, :], lhsT=uT[:, dc, t8 * 128:(t8 + 1) * 128], rhs=wV[:, dc, :],
                                                                                start=(dc == 0), stop=(dc == 7)), reads=[B("uT"), B("wV")], writes=[pb])
                            if t8 % 2 == 0:
                                O("act", lambda e, ps=ps, tt=tt: e.activation(func=AF.Identity, out=VA[:, tt, :, 0:64], in_=ps[:, :].rearrange("p (h d) -> p h d", h=8)), reads=[pb], writes=[B("VA")])
                            else:
                                O("dve", lambda e, ps=ps, tt=tt: e.tensor_copy(out=VA[:, tt, :, 0:64], in_=ps[:, :].rearrange("p (h d) -> p h d", h=8)), reads=[pb], writes=[B("VA")])
                pg.barrier()
                if STOP_AFTER == "S1a":
                    d1 = O("sp", lambda e: e.dma_start(out=dbgb_d[:, 0:4096], in_=KT[:, 1, :]), reads=[B("KT")], dma_key="dbg1")
                    d2 = O("sp", lambda e: e.dma_start(out=dbgb_d[:, 4096:8192], in_=Zt[:, 1, :, :, :].rearrange("p g i h -> p (g i h)")), reads=[B("Zt0")], dma_key="dbg2")
                    d3 = O("sp", lambda e: e.dma_start(out=dbgf_d[:, 0:64], in_=KS[:].rearrange("p a b -> p (a b)")), reads=[B("KS")], dma_key="dbg3")
                    pg.emit(final_wait_ops=[d1, d2, d3])
                    return nc

                with contextlib.ExitStack() as s1b:
                    Ug = [T(s1b, "Ug%d" % m, [128, 512], BF16) for m in range(2)]
                    CI_i = T(s1b, "CIi", [128, 512], I32)
                    CIf = T(s1b, "CIf", [128, 512], F32)
                    ANG = T(s1b, "ANG", [128, 512], F32)
                    sF = T(s1b, "sF", [128, 512], F32)
                    sI = T(s1b, "sI", [128, 512], I32)
                    COSC = T(s1b, "COSC", [128, 512], F32)
                    SINC = T(s1b, "SINC", [128, 512], F32)
                    WR = T(s1b, "WR", [128, 512], F32)
                    WI = T(s1b, "WI", [128, 512], F32)
                    ta = T(s1b, "ta", [128, 512], F32)
                    tbb = T(s1b, "tbb", [128, 512], F32)
                    XPR = T(s1b, "XPR", [128, 516], BF16)
                    XPI = T(s1b, "XPI", [128, 516], BF16)
                    gs = T(s1b, "gs", [128, 512], F32)
                    gu = T(s1b, "gu", [128, 512], F32)
                    psU = PS(s1b, "psU", [128, 1024], BF16)
                    psXr = PS(s1b, "psXr", [128, 512], F32)
                    psXi = PS(s1b, "psXi", [128, 512], F32)
                    psY = [PS(s1b, "psY%d" % m, [128, 512], F32) for m in range(2)]

                    O("pool", lambda e: e.iota(CI_i[:], pattern=[[1, 512]], base=0, channel_multiplier=0), writes=[B("CIi")])
                    O("dve", lambda e: e.tensor_copy(out=CIf[:], in_=CI_i[:]), reads=[B("CIi")], writes=[B("CIf")])
                    O("dve", lambda e: e.memset(XPR[:, 0:1], 0.0), writes=[B("XPR")])
                    O("dve", lambda e: e.memset(XPI[:, 0:1], 0.0), writes=[B("XPI")])

                    def V2(fn, r, w, eng="dve"):
                        return O(eng, fn, reads=[B(n) for n in r], writes=[B(n) for n in w])

                    def sin2(dst, dstbuf, shift):
                        V2(lambda e: e.tensor_scalar(out=sF[:], in0=ANG[:], scalar1=shift, scalar2=1.0 / TWO_PI, op0=ALU.add, op1=ALU.mult), ["ANG"], ["sF"])
                        V2(lambda e: e.tensor_copy(out=sI[:], in_=sF[:]), ["sF"], ["sI"])
                        V2(lambda e: e.tensor_copy(out=sF[:], in_=sI[:]), ["sI"], ["sF"])
                        V2(lambda e: e.tensor_scalar(out=sF[:], in0=sF[:], scalar1=-TWO_PI, scalar2=shift, op0=ALU.mult, op1=ALU.add), ["sF"], ["sF"])
                        V2(lambda e: e.tensor_tensor(out=sF[:], in0=sF[:], in1=ANG[:], op=ALU.add), ["sF", "ANG"], ["sF"])
                        V2(lambda e: e.tensor_scalar(out=sF[:], in0=sF[:], scalar1=3.14159, scalar2=-3.14159, op0=ALU.min, op1=ALU.max), ["sF"], ["sF"])
                        V2(lambda e: e.activation(out=dst, in_=sF[:], func=AF.Sin), ["sF"], [dstbuf], "act")

                    for q in range(16):
                        zb = B("Zt%d" % q)
                        for m in range(2):
                            g = 2 * q + m
                            for s in range(4):
                                O("pe", lambda e, s=s, g=g, m=m: e.transpose(out=psU[:, m * 512 + s * 128: m * 512 + (s + 1) * 128],
                                                                             in_=Zt[:, s, g, :, :].rearrange("p i h -> p (i h)"), identity=identb[:]),
                                  reads=[zb, B("identb")], writes=[B("psU")])
                            if m == 0:
                                O("act", lambda e, m=m: e.activation(func=AF.Identity, out=Ug[m][:], in_=psU[:, m * 512:(m + 1) * 512]), reads=[B("psU")], writes=[B("Ug%d" % m)])
                            else:
                                O("dve", lambda e, m=m: e.tensor_copy(out=Ug[m][:], in_=psU[:, m * 512:(m + 1) * 512]), reads=[B("psU")], writes=[B("Ug%d" % m)])
                        for m in range(2):
                            g = 2 * q + m
                            O("pe", lambda e, m=m, g=g: e.matmul(psXr[64 * m:64 * m + 64, :], lhsT=MinR[:, g, :], rhs=Ug[m][:], start=True, stop=True),
                              reads=[B("MinR"), B("Ug%d" % m)], writes=[B("psXr")])
                            O("pe", lambda e, m=m, g=g: e.matmul(psXi[64 * m:64 * m + 64, :], lhsT=MinI[:, g, :], rhs=Ug[m][:], start=True, stop=True),
                              reads=[B("MinI"), B("Ug%d" % m)], writes=[B("psXi")])
                        V2(lambda e, q=q: e.tensor_scalar(out=ANG[:], in0=CIf[:], scalar1=TH8[:, q:q + 1], scalar2=None, op0=ALU.mult), ["CIf", "TH8"], ["ANG"])
                        sin2(SINC[:], "SINC", 0.0)
                        sin2(COSC[:], "COSC", math.pi / 2)
                        V2(lambda e: e.tensor_tensor(out=ta[:], in0=psXr[:], in1=COSC[:], op=ALU.mult), ["psXr", "COSC"], ["ta"])
                        V2(lambda e: e.tensor_tensor(out=tbb[:], in0=psXi[:], in1=SINC[:], op=ALU.mult), ["psXi", "SINC"], ["tbb"])
                        V2(lambda e: e.tensor_tensor(out=ta[:], in0=ta[:], in1=tbb[:], op=ALU.add), ["ta", "tbb"], ["ta"])
                        V2(lambda e, q=q: e.tensor_tensor_scan(out=WR[:], data0=RHO8[:, q:q + 1].to_broadcast([128, 512]), data1=ta[:], initial=0.0,
                                                                op0=ALU.mult, op1=ALU.add), ["ta", "RHO8"], ["WR"])
                        V2(lambda e: e.tensor_tensor(out=ta[:], in0=psXi[:], in1=COSC[:], op=ALU.mult), ["psXi", "COSC", "WR"], ["ta"])
                        V2(lambda e: e.tensor_tensor(out=tbb[:], in0=psXr[:], in1=SINC[:], op=ALU.mult), ["psXr", "SINC"], ["tbb"])
                        V2(lambda e: e.tensor_tensor(out=ta[:], in0=ta[:], in1=tbb[:], op=ALU.subtract), ["ta", "tbb"], ["ta"])
                        V2(lambda e, q=q: e.tensor_tensor_scan(out=WI[:], data0=RHO8[:, q:q + 1].to_broadcast([128, 512]), data1=ta[:], initial=0.0,
                                                                op0=ALU.mult, op1=ALU.add), ["ta", "RHO8"], ["WI"])
                        V2(lambda e: e.tensor_tensor(out=ta[:], in0=WR[:], in1=COSC[:], op=ALU.mult), ["WR", "COSC", "WI"], ["ta"])
                        V2(lambda e: e.tensor_tensor(out=tbb[:], in0=WI[:], in1=SINC[:], op=ALU.mult), ["WI", "SINC"], ["tbb"])
                        V2(lambda e: e.tensor_tensor(out=XPR[:, 1:513], in0=ta[:], in1=tbb[:], op=ALU.subtract), ["ta", "tbb"], ["XPR"])
                        V2(lambda e: e.tensor_tensor(out=ta[:], in0=WI[:], in1=COSC[:], op=ALU.mult), ["WI", "COSC", "XPR"], ["ta"])
                        V2(lambda e: e.tensor_tensor(out=tbb[:], in0=WR[:], in1=SINC[:], op=ALU.mult), ["WR", "SINC"], ["tbb"])
                        V2(lambda e: e.tensor_tensor(out=XPI[:, 1:513], in0=ta[:], in1=tbb[:], op=ALU.add), ["ta", "tbb"], ["XPI"])
                        for m in range(2):
                            g = 2 * q + m
                            pbY = B("psY%d" % m)
                            for s in range(4):
                                cs = slice(s * 128, (s + 1) * 128)
                                outp = psY[m][:, s * 128:(s + 1) * 128]
                                O("pe", lambda e, outp=outp, cs=cs, g=g, m=m: e.matmul(outp, lhsT=Ug[m][:, cs], rhs=MI[:, g, :], start=True, stop=False),
                                  reads=[B("Ug%d" % m), B("MI")], writes=[pbY])
                                O("pe", lambda e, outp=outp, cs=cs, q=q, m=m: e.matmul(outp, lhsT=XPR[:, cs], rhs=MoRp[:, q, m, :], start=False, stop=False),
                                  reads=[B("XPR"), B("MoRp")], writes=[pbY])
                                O("pe", lambda e, outp=outp, cs=cs, q=q, m=m: e.matmul(outp, lhsT=XPI[:, cs], rhs=MoIp[:, q, m, :], start=False, stop=True),
                                  reads=[B("XPI"), B("MoIp")], writes=[pbY])
                            yv = psY[m][:, :]
                            V2(lambda e, yv=yv: e.activation(out=gs[:], in_=yv, func=AF.Square), ["psY%d" % m], ["gs"], "act")
                            V2(lambda e: e.tensor_scalar(out=gs[:], in0=gs[:], scalar1=0.044715, scalar2=1.0, op0=ALU.mult, op1=ALU.add), ["gs"], ["gs"])
                            V2(lambda e, yv=yv: e.tensor_tensor(out=gu[:], in0=yv, in1=gs[:], op=ALU.mult), ["psY%d" % m, "gs"], ["gu"])
                            V2(lambda e: e.activation(out=gu[:], in_=gu[:], func=AF.Sigmoid, scale=1.5957691216), ["gu"], ["gu"], "act")
                            O("dve", lambda e, yv=yv, g=g: e.tensor_tensor(out=Zt[:, :, g, :, :],
                                                                            in0=yv.rearrange("p (s j h) -> p s j h", s=4, j=8),
                                                                            in1=gu[:].rearrange("p (s j h) -> p s j h", s=4, j=8), op=ALU.mult),
                              reads=[pbY, B("gu")], writes=[zb])
                pg.barrier()

                with contextlib.ExitStack() as s1c:
                    vT = [T(s1c, "vT%d" % k, [128, 4, 1024], BF16) for k in range(2)]
                    yaT = [T(s1c, "yaT%d" % k, [128, 4, 1024], BF16) for k in range(2)]
                    sg = T(s1c, "sg", [128, 512], F32)
                    tmpV = T(s1c, "tmpV", [128, 8, 512], BF16)
                    psV = [PS(s1c, "psV%d" % k, [128, 1024], BF16) for k in range(2)]
                    psG = [PS(s1c, "psG%d" % k, [128, 512], F32) for k in range(2)]
                    zall = [B("Zt%d" % q_) for q_ in range(16)]
                    cnt = 0
                    for s in range(4):
                        sl = s % 2
                        O("pool", lambda e, s=s: e.tensor_copy(out=tmpV[:].rearrange("p j (g h) -> p j g h", g=32), in_=Zt[:, s, :, :, :].rearrange("p g j h -> p j g h")),
                          reads=zall, writes=[B("tmpV")])
                        for fc in range(4):
                            pv = psV[fc % 2]
                            pvb = B("psV%d" % (fc % 2))
                            for j in range(8):
                                O("pe", lambda e, pv=pv, s=s, j=j, fc=fc: e.transpose(out=pv[:, j * 128:(j + 1) * 128], in_=tmpV[:, j, fc * 128:(fc + 1) * 128], identity=identb[:]),
                                  reads=[B("tmpV"), B("identb")], writes=[pvb])
                            dst = vT[sl][:, fc, :].rearrange("p (c j) -> p j c", j=8)
                            srcv = pv[:, :].rearrange("p (j c) -> p j c", j=8)
                            if fc % 2 == 0:
                                O("act", lambda e, dst=dst, srcv=srcv: e.activation(func=AF.Identity, out=dst, in_=srcv), reads=[pvb], writes=[B("vT%d" % sl)])
                            else:
                                O("dve", lambda e, dst=dst, srcv=srcv: e.tensor_copy(out=dst, in_=srcv), reads=[pvb], writes=[B("vT%d" % sl)])
                        for oc in range(4):
                            for th in range(2):
                                pgk = cnt % 2
                                cnt += 1
                                pG = psG[pgk]
                                pGb = B("psG%d" % pgk)
                                ts_ = slice(th * 512, (th + 1) * 512)
                                for fc in range(4):
                                    O("pe", lambda e, pG=pG, fc=fc, oc=oc, ts_=ts_, sl=sl: e.matmul(pG[:, :], lhsT=wG[:, fc, oc * 128:(oc + 1) * 128], rhs=vT[sl][:, fc, ts_],
                                                                                                   start=(fc == 0), stop=(fc == 3)), reads=[B("wG"), B("vT%d" % sl)], writes=[pGb])
                                O("act", lambda e, pG=pG, oc=oc: e.activation(out=sg[:], in_=pG[:, :], func=AF.Sigmoid, bias=bglu[:, oc:oc + 1]),
                                  reads=[pGb, B("bglu")], writes=[B("sg")])
                                O("dve", lambda e, oc=oc, ts_=ts_, sl=sl: e.tensor_tensor(out=yaT[sl][:, oc, ts_], in0=vT[sl][:, oc, ts_], in1=sg[:], op=ALU.mult),
                                  reads=[B("vT%d" % sl), B("sg")], writes=[B("yaT%d" % sl)])
                        for oc in range(4):
                            stp = O("sp", lambda e, oc=oc, s=s, sl=sl: e.dma_start(out=ya_d[oc * 128:(oc + 1) * 128, s * 1024:(s + 1) * 1024], in_=yaT[sl][:, oc, :]),
                                    reads=[B("yaT%d" % sl)], writes=[B("ya_d_%d_%d" % (s, oc))], dma_key="yast%d" % sl)
                    ya_last = [pg.ops["sp"][-1]]
                    ya_stores = [o for o in pg.ops["sp"] if o.is_dma and isinstance(o.dsem, str) and o.dsem.startswith("yast")]
            pg.barrier(dma_ops=ya_stores[-8:])

        if STOP_AFTER == "S":
            pg.emit(final_wait_ops=ya_stores[-8:])
            return nc

        with contextlib.ExitStack() as sA:
            wQ = T(sA, "wQ", [128, 8, 512], BF16)
            wGA = T(sA, "wGA", [128, 8, 1024], BF16)
            wGB = T(sA, "wGB", [128, 8, 1024], BF16)
            wA = T(sA, "wA", [128, 4, 1024], BF16)
            wB = T(sA, "wB", [128, 4, 1024], BF16)
            wO = T(sA, "wO", [128, 8, 1024], BF16)
            g2b = T(sA, "g2b", [128, D], F32)
            load("sp", g2b[:], g2_d.partition_broadcast(128), "g2b", "g2b")
            for dc in range(8):
                rows = slice(dc * 128, (dc + 1) * 128)
                load("pool", wQ[:, dc, :], w_in_d[rows, 512:1024], "wQ", "wQ")
            for dc in range(8 if not DBG_ONLYQ else 0):
                rows = slice(dc * 128, (dc + 1) * 128)
                load("pool", wGA[:, dc, :], w_in_d[rows, 2048:3072], "wGA", "wGA")
                load("pool", wGB[:, dc, :], w_in_d[rows, 3072:4096], "wGB", "wGB")
            for fc in range(4 if not DBG_ONLYQ else 0):
                rows = slice(fc * 128, (fc + 1) * 128)
                load("pool", wA[:, fc, :], w_a_d[rows, :], "wA", "wA")
                load("pool", wB[:, fc, :], w_b_d[rows, :], "wB", "wB")
            for dc in range(8 if not DBG_ONLYQ else 0):
                rows = slice(dc * 128, (dc + 1) * 128)
                load("pool", wO[:, dc, :], w_out_d[rows, :], "wO", "wO")

            xq = [T(sA, "xq%d" % k, [128, 2, D], F32) for k in range(2)]
            yaq = [T(sA, "yaq%d" % k, [128, 4, 256], BF16) for k in range(2)]
            uTq = T(sA, "uTq", [128, 8, 256], BF16)
            scrA = {"sq": T(sA, "sqA", [128, D], BF16)[:], "ss": T(sA, "ssA", [128, 1], F32)[:], "rs": T(sA, "rsA", [128, 1], F32)[:],
                    "xn": T(sA, "xnA", [128, D], F32)[:], "eps": epsb[:]}
            QT = T(sA, "QT", [128, 4, 256], BF16)
            QTf = T(sA, "QTf", [128, 4, 256], F32)
            gate = T(sA, "gate", [128, 2, 8, 16], F32)
            top8 = T(sA, "top8", [128, 2, 8, 8], F32)
            sel = T(sA, "sel", [128, 2, 8, 16], F32)
            trim = T(sA, "trim", [128, 2, 256], BF16)
            PT = [T(sA, "PT%d" % k, [128, 2, 256], BF16) for k in range(3)]
            acc = T(sA, "acc", [128, 2, 8, 65], F32)
            rden = T(sA, "rden", [128, 2, 8], F32)
            ybt = T(sA, "ybt", [128, 2, 512], BF16)
            ybT = T(sA, "ybT", [128, 4, 256], BF16)
            sga = T(sA, "sga", [128, 256], F32)
            sgb = T(sA, "sgb", [128, 256], F32)
            tma = T(sA, "tma", [128, 256], F32)
            mT = T(sA, "mT", [128, 8, 256], BF16)
            mg = scrA["xn"]
            ssM = T(sA, "ssM", [128, 2], F32)
            rsM = T(sA, "rsM", [128, 1], F32)
            psT = [PS(sA, "psTA%d" % k, [128, 512], F32) for k in range(2)]
            psS = [PS(sA, "psS%d" % k, [128, 512], F32) for k in range(2)]
            psO = [PS(sA, "psO%d" % k, [128, 512], F32) for k in range(2)]
            psW = [PS(sA, "psW%d" % k, [128, 512], F32) for k in range(2)]
            psTb = [B("psTA0"), B("psTA1")]

            O("pool", lambda e: e.memset(trim[:], 1.0), writes=[B("trim")])
            for kh in range(2):
                O("pool", lambda e, kh=kh: e.affine_select(out=trim[:, kh, :], in_=trim[:, kh, :], pattern=[[1, 256]], compare_op=ALU.is_ge,
                                                           fill=0.0, base=-128 * kh, channel_multiplier=-1), reads=[B("trim")], writes=[B("trim")])

            def issue_A_loads(i):
                sl = i % 2
                for qh in range(2):
                    tt = 2 * i + qh
                    load("sp", xq[sl][:, qh, :], x_d[tt * 128:(tt + 1) * 128, :], "xq%d" % sl, "xq%d" % sl)
                if not DBG_NOYA:
                    for fc in range(4):
                        load("sp", yaq[sl][:, fc, :], ya_d[fc * 128:(fc + 1) * 128, i * 256:(i + 1) * 256], "yaq%d" % sl, "yaq%d" % sl)

            if STOP_AFTER == "A0":
                pg.emit(final_wait_ops=[])
                return nc
            issue_A_loads(0)
            ptc = [0]
            h1_stores = []
            for i in range(DBG_NBLK):
                sl = i % 2
                if i + 1 < DBG_NBLK:
                    issue_A_loads(i + 1)
                for qh in range(2):
                    rmsnorm_T("A", xq[sl][:, qh, :], B("xq%d" % sl), g1sb,
                              lambda dc, qh=qh: uTq[:, dc, qh * 128:(qh + 1) * 128], B("uTq"), psT, psTb, scrA, "A")
                if STOP_AFTER == "A1":
                    pg.emit(final_wait_ops=[])
                    return nc
                for hp in range(4):
                    ps = psS[hp % 2]
                    pb = B("psS%d" % (hp % 2))
                    for dc in range(8):
                        O("pe", lambda e, ps=ps, dc=dc, hp=hp: e.matmul(ps[:, 0:256], lhsT=wQ[:, dc, hp * 128:(hp + 1) * 128], rhs=uTq[:, dc, :],
                                                                        start=(dc == 0), stop=(dc == 7)), reads=[B("wQ"), B("uTq")], writes=[pb])
                    O("dve", lambda e, ps=ps, hp=hp: e.tensor_copy(out=QTf[:, hp, :], in_=ps[:, 0:256]), reads=[pb], writes=[B("QTf")])
                    O("pool", lambda e, hp=hp: e.tensor_copy(out=QT[:, hp, :], in_=QTf[:, hp, :]), reads=[B("QTf")], writes=[B("QT")])
                if STOP_AFTER == "A2":
                    pg.emit(final_wait_ops=[])
                    return nc
                use_sel = i >= 4
                if use_sel:
                    for qh in range(2):
                        pgt = psO[qh]
                        pgb = B("psO%d" % qh)
                        for h in range(8):
                            hp, par = h // 2, h % 2
                            rows = slice(64 * par, 64 * par + 64)
                            O("pe", lambda e, pgt=pgt, h=h, hp=hp, rows=rows, qh=qh: e.matmul(pgt[:, h * 16:(h + 1) * 16], lhsT=QTf[rows, hp, qh * 128:(qh + 1) * 128],
                                                                                            rhs=KS[rows, hp, :], start=True, stop=True),
                              reads=[B("QTf"), B("KS")], writes=[pgb])
                        O("dve", lambda e, qh=qh: e.memset(gate[:, qh, :, :], NEG), writes=[B("gate")])
                        O("dve", lambda e, qh=qh, pgt=pgt, i=i: e.tensor_copy(out=gate[:, qh, :, 0:i], in_=pgt[:, 0:128].rearrange("p (h j) -> p h j", h=8)[:, :, 0:i]),
                          reads=[pgb], writes=[B("gate")])
                        for h in range(8):
                            O("dve", lambda e, qh=qh, h=h: e.max(out=top8[:, qh, h, :], in_=gate[:, qh, h, :]), reads=[B("gate")], writes=[B("top8")])
                        O("dve", lambda e, qh=qh: e.tensor_tensor(out=sel[:, qh, :, :], in0=gate[:, qh, :, :], in1=top8[:, qh, :, 2:3].to_broadcast([128, 8, 16]), op=ALU.is_ge),
                          reads=[B("gate"), B("top8")], writes=[B("sel")])
                items = [(h, j) for h in range(8) for j in range(i + 1)]
                slots = {}

                def emit_qk(n):
                    h, j = items[n]
                    hp, par = h // 2, h % 2
                    rows = slice(64 * par, 64 * par + 64)
                    k3 = ptc[0] % 3
                    k2 = ptc[0] % 2
                    ptc[0] += 1
                    slots[n] = (k2, k3)
                    pS = psS[k2]
                    pSb = B("psS%d" % k2)
                    for kh in range(2):
                        key0 = j * 256 + kh * 128
                        O("pe", lambda e, pS=pS, kh=kh, rows=rows, hp=hp, key0=key0: e.matmul(pS[:, kh * 256:(kh + 1) * 256], lhsT=KT[rows, hp, key0:key0 + 128],
                                                                                             rhs=QT[rows, hp, :], start=True, stop=True),
                          reads=[B("KT"), B("QT")], writes=[pSb])

                def emit_rest(n):
                    h, j = items[n]
                    k2, k3 = slots[n]
                    pS = psS[k2]
                    pSb = B("psS%d" % k2)
                    pt = PT[k3]
                    ptb = B("PT%d" % k3)
                    O("act", lambda e, pS=pS, pt=pt: e.activation(out=pt[:].rearrange("p a b -> p (a b)"), in_=pS[:, :], func=AF.Exp, scale=0.125),
                      reads=[pSb], writes=[ptb])
                    if j == i:
                        O("pool", lambda e, pt=pt: e.tensor_tensor(out=pt[:], in0=pt[:], in1=trim[:], op=ALU.mult), reads=[ptb, B("trim")], writes=[ptb])
                    pO = psO[k2]
                    pOb = B("psO%d" % k2)
                    for qh in range(2):
                        for kh in range(2):
                            if j == i and kh == 1 and qh == 0:
                                continue
                            first = (kh == 0)
                            last = (kh == 1) or (j == i and qh == 0)
                            O("pe", lambda e, pO=pO, qh=qh, kh=kh, pt=pt, j=j, h=h, first=first, last=last: e.matmul(
                                pO[:, qh * 128:qh * 128 + 66], lhsT=pt[:, kh, qh * 128:(qh + 1) * 128], rhs=VA[:, 2 * j + kh, h, :], start=first, stop=last),
                              reads=[ptb, B("VA"), B("VAones"), B("VAzeros")], writes=[pOb])
                    for qh in range(2):
                        src = pO[:, qh * 128:qh * 128 + 65]
                        dsta = acc[:, qh, h, :]
                        ab = B("acc_%d_%d" % (qh, h))
                        if j == 0:
                            if use_sel and j < i:
                                O("dve", lambda e, src=src, dsta=dsta, qh=qh, h=h, j=j: e.tensor_scalar(out=dsta, in0=src, scalar1=sel[:, qh, h, j:j + 1], scalar2=None, op0=ALU.mult),
                                  reads=[pOb, B("sel")], writes=[ab])
                            else:
                                O("dve", lambda e, src=src, dsta=dsta: e.tensor_copy(out=dsta, in_=src), reads=[pOb], writes=[ab])
                        else:
                            if use_sel and j < i:
                                O("dve", lambda e, src=src, dsta=dsta, qh=qh, h=h, j=j: e.scalar_tensor_tensor(out=dsta, in0=src, scalar=sel[:, qh, h, j:j + 1], in1=dsta,
                                                                                                                  op0=ALU.mult, op1=ALU.add),
                                  reads=[pOb, B("sel"), ab], writes=[ab])
                            else:
                                O("dve", lambda e, src=src, dsta=dsta: e.tensor_tensor(out=dsta, in0=src, in1=dsta, op=ALU.add), reads=[pOb, ab], writes=[ab])

                emit_qk(0)
                for n in range(len(items)):
                    if n + 1 < len(items):
                        emit_qk(n + 1)
                    emit_rest(n)
                if STOP_AFTER == "A3":
                    pg.emit(final_wait_ops=[])
                    return nc
                for qh in range(2):
                    accbufs = [B("acc_%d_%d" % (qh, h_)) for h_ in range(8)]
                    O("dve", lambda e, qh=qh: e.reciprocal(out=rden[:, qh, :], in_=acc[:, qh, :, 64]), reads=accbufs, writes=[B("rden")])
                    O("dve", lambda e, qh=qh: e.tensor_tensor(out=ybt[:, qh, :].rearrange("p (h d) -> p h d", h=8), in0=acc[:, qh, :, 0:64],
                                                              in1=rden[:, qh, :].unsqueeze(2).to_broadcast([128, 8, 64]), op=ALU.mult),
                      reads=accbufs + [B("rden")], writes=[B("ybt")])
                pvT = psT[0]
                for qh in range(2):
                    for fc in range(4):
                        O("pe", lambda e, qh=qh, fc=fc: e.transpose(out=pvT[:].bitcast(BF16)[:, (qh * 4 + fc) * 128:(qh * 4 + fc + 1) * 128],
                                                                     in_=ybt[:, qh, fc * 128:(fc + 1) * 128], identity=identb[:]),
                          reads=[B("ybt"), B("identb")], writes=[psTb[0]])
                for qh in range(2):
                    O("act", lambda e, qh=qh: e.activation(func=AF.Identity, out=ybT[:, :, qh * 128:(qh + 1) * 128], in_=pvT[:].bitcast(BF16)[:, qh * 512:(qh + 1) * 512].rearrange("p (f t) -> p f t", f=4)),
                      reads=[psTb[0]], writes=[B("ybT")])
                if STOP_AFTER == "A4":
                    pg.emit(final_wait_ops=[])
                    return nc
                for oc in range(8):
                    ocs = slice(oc * 128, (oc + 1) * 128)
                    pw = psW[oc % 2]
                    pwbuf = B("psW%d" % (oc % 2))
                    pq = psS[oc % 2]
                    pqb = B("psS%d" % (oc % 2))
                    for fc in range(4):
                        O("pe", lambda e, pw=pw, fc=fc, ocs=ocs, sl=sl: e.matmul(pw[:, 0:256], lhsT=wA[:, fc, ocs], rhs=yaq[sl][:, fc, :], start=(fc == 0), stop=(fc == 3)),
                          reads=[B("wA"), B("yaq%d" % sl)], writes=[pwbuf])
                    for fc in range(4):
                        O("pe", lambda e, pw=pw, fc=fc, ocs=ocs: e.matmul(pw[:, 256:512], lhsT=wB[:, fc, ocs], rhs=ybT[:, fc, :], start=(fc == 0), stop=(fc == 3)),
                          reads=[B("wB"), B("ybT")], writes=[pwbuf])
                    for dc in range(8):
                        O("pe", lambda e, pq=pq, dc=dc, ocs=ocs: e.matmul(pq[:, 0:256], lhsT=wGA[:, dc, ocs], rhs=uTq[:, dc, :], start=(dc == 0), stop=(dc == 7)),
                          reads=[B("wGA"), B("uTq")], writes=[pqb])
                    for dc in range(8):
                        O("pe", lambda e, pq=pq, dc=dc, ocs=ocs: e.matmul(pq[:, 256:512], lhsT=wGB[:, dc, ocs], rhs=uTq[:, dc, :], start=(dc == 0), stop=(dc == 7)),
                          reads=[B("wGB"), B("uTq")], writes=[pqb])
                    O("act", lambda e, pq=pq: e.activation(out=sga[:], in_=pq[:, 0:256], func=AF.Sigmoid), reads=[pqb], writes=[B("sga")])
                    O("act", lambda e, pq=pq: e.activation(out=sgb[:], in_=pq[:, 256:512], func=AF.Sigmoid), reads=[pqb], writes=[B("sgb")])
                    O("dve", lambda e, pw=pw: e.tensor_tensor(out=tma[:], in0=pw[:, 0:256], in1=sga[:], op=ALU.mult), reads=[pwbuf, B("sga")], writes=[B("tma")])
                    O("dve", lambda e, pw=pw: e.tensor_tensor(out=sgb[:], in0=pw[:, 256:512], in1=sgb[:], op=ALU.mult), reads=[pwbuf, B("sgb")], writes=[B("sgb")])
                    O("dve", lambda e, oc=oc: e.tensor_tensor(out=mT[:, oc, :], in0=tma[:], in1=sgb[:], op=ALU.add), reads=[B("tma"), B("sgb")], writes=[B("mT")])
                if STOP_AFTER == "A5":
                    pg.emit(final_wait_ops=[])
                    return nc
                for qh in range(2):
                    tt = 2 * i + qh
                    for ch in range(2):
                        pw = psW[ch]
                        pwbuf = B("psW%d" % ch)
                        for dc in range(8):
                            O("pe", lambda e, pw=pw, dc=dc, ch=ch, qh=qh: e.matmul(pw[:, :], lhsT=mT[:, dc, qh * 128:(qh + 1) * 128], rhs=wO[:, dc, ch * 512:(ch + 1) * 512],
                                                                                   start=(dc == 0), stop=(dc == 7)), reads=[B("mT"), B("wO")], writes=[pwbuf])
                        O("act", lambda e, pw=pw, ch=ch: e.activation(out=scrA["sq"][:, ch * 512:(ch + 1) * 512], in_=pw[:, :], func=AF.Square, accum_out=ssM[:, ch:ch + 1]),
                          reads=[pwbuf], writes=[B("Asq"), B("ssM")])
                        O("dve", lambda e, pw=pw, ch=ch: e.tensor_tensor(out=mg[:, ch * 512:(ch + 1) * 512], in0=pw[:, :], in1=g2b[:, ch * 512:(ch + 1) * 512], op=ALU.mult),
                          reads=[pwbuf, B("g2b"), B("ssM")], writes=[B("Axn")])
                    O("dve", lambda e: e.tensor_tensor(out=rsM[:], in0=ssM[:, 0:1], in1=ssM[:, 1:2], op=ALU.add), reads=[B("ssM")], writes=[B("rsM")])
                    O("act", lambda e: e.activation(out=rsM[:], in_=rsM[:], func=AF.Sqrt, scale=1.0 / D, bias=epsb[:]), reads=[B("rsM"), B("epsb")], writes=[B("rsM")])
                    O("dve", lambda e: e.reciprocal(out=rsM[:], in_=rsM[:]), reads=[B("rsM")], writes=[B("rsM")])
                    hs = sl
                    O("dve", lambda e, qh=qh, sl=sl: e.scalar_tensor_tensor(out=xq[sl][:, qh, :], in0=mg, scalar=rsM[:, 0:1], in1=xq[sl][:, qh, :], op0=ALU.mult, op1=ALU.add),
                      reads=[B("Axn"), B("rsM"), B("xq%d" % sl)], writes=[B("xq%d" % sl)])
                    st_ = O("sp", lambda e, sl=sl, qh=qh, tt=tt: e.dma_start(out=out_d[tt * 128:(tt + 1) * 128, :], in_=xq[sl][:, qh, :]), reads=[B("xq%d" % sl)],
                            writes=[B("out%d" % tt)], dma_key="h1st%d" % hs)
                    h1_stores.append(st_)
        pg.barrier(dma_ops=h1_stores)
        sKV.close()
        if STOP_AFTER == "A":
            pg.emit(final_wait_ops=h1_stores)
            return nc

        with contextlib.ExitStack() as sB:
            w1 = T(sB, "w1", [128, 8, 4096], BF16)
            w2 = T(sB, "w2", [128, 32, D], BF16)
            g4b = T(sB, "g4b", [128, D], F32)
            load("sp", g4b[:], g4_d.partition_broadcast(128), "g4b", "g4b")
            grp_last = []

            def w1_group(cc):
                aft = [grp_last[-2]] if len(grp_last) >= 2 else []
                for dc in range(8):
                    rows = slice(dc * 128, (dc + 1) * 128)
                    o_ = load("pool", w1[:, dc, cc * 1024:(cc + 1) * 1024], w_ff1_d[rows, cc * 1024:(cc + 1) * 1024], "w1_%d" % cc, "w1_%d" % cc,
                              after=aft if dc == 0 else ())
                grp_last.append(o_)

            def w2_group(fg):
                aft = [grp_last[-2]] if len(grp_last) >= 2 else []
                for fc in range(fg * 8, fg * 8 + 8):
                    rows = slice(fc * 128, (fc + 1) * 128)
                    o_ = load("pool", w2[:, fc, :], w_ff2_d[rows, :], "w2_%d" % fg, "w2_%d" % fg, after=aft if fc == fg * 8 else ())
                grp_last.append(o_)

            w1_group(0)
            w2_group(0)
            w1_group(1)
            w1_group(2)
            w1_group(3)
            w2_group(1)
            w2_group(2)
            w2_group(3)
            hx = [T(sB, "hx%d" % k, [128, 2, D], F32) for k in range(2)]
            fT = T(sB, "fT", [128, 8, 256], BF16)
            hT = T(sB, "hT", [128, 32, 256], BF16)
            rl2 = [T(sB, "rl2_%d" % k, [128, 512], BF16) for k in range(2)]
            scrB = {"sq": T(sB, "sqB", [128, D], BF16)[:], "ss": T(sB, "ssB", [128, 1], F32)[:], "rs": T(sB, "rsB", [128, 1], F32)[:],
                    "xn": T(sB, "xnB", [128, D], F32)[:], "eps": epsb[:]}
            mgB = scrB["xn"]
            ssF = T(sB, "ssF", [128, 2], F32)
            rsF = T(sB, "rsF", [128, 1], F32)
            psT = [PS(sB, "psTB%d" % k, [128, 512], F32) for k in range(2)]
            psH = [PS(sB, "psH%d" % k, [128, 512], F32) for k in range(3)]
            psW = [PS(sB, "psWB%d" % k, [128, 512], F32) for k in range(2)]
            psTb = [B("psTB0"), B("psTB1")]

            def issue_B_loads(gi):
                sl = gi % 2
                for t2 in range(2):
                    tt = gi * 2 + t2
                    load("sp", hx[sl][:, t2, :], out_d[tt * 128:(tt + 1) * 128, :], "hx%d" % sl, "hx%d" % sl)

            if STOP_AFTER == "B0":
                pg.emit(final_wait_ops=[])
                return nc
            issue_B_loads(0)
            hc = 0
            for gi in range(16):
                sl = gi % 2
                if gi + 1 < 16:
                    issue_B_loads(gi + 1)
                for t2 in range(2):
                    rmsnorm_T("B", hx[sl][:, t2, :], B("hx%d" % sl), g3sb,
                              lambda dc, t2=t2: fT[:, dc, t2 * 128:(t2 + 1) * 128], B("fT"), psT, psTb, scrB, "B", gbuf="gsb3")
                if STOP_AFTER == "B1":
                    pg.emit(final_wait_ops=[])
                    return nc
                for fc2 in range(16):
                    ph = psH[hc % 3]
                    phb = B("psH%d" % (hc % 3))
                    rlk = hc % 2
                    hc += 1
                    for u in range(2):
                        fc = fc2 * 2 + u
                        for dc in range(8):
                            O("pe", lambda e, ph=ph, dc=dc, fc=fc, u=u: e.matmul(ph[:, u * 256:(u + 1) * 256], lhsT=w1[:, dc, fc * 128:(fc + 1) * 128], rhs=fT[:, dc, :],
                                                                                 start=(dc == 0), stop=(dc == 7)), reads=[B("w1_%d" % (fc // 8)), B("fT")], writes=[phb])
                    O("act", lambda e, ph=ph, rlk=rlk: e.activation(out=rl2[rlk][:], in_=ph[:, :], func=AF.Relu), reads=[phb], writes=[B("rl2_%d" % rlk)])
                    O("dve", lambda e, ph=ph, fc2=fc2, rlk=rlk: e.scalar_tensor_tensor(out=hT[:, fc2 * 2:fc2 * 2 + 2, :].rearrange("p a b -> p (a b)"), in0=ph[:, :], scalar=0.0,
                                                                                         in1=rl2[rlk][:], op0=ALU.max, op1=ALU.mult),
                      reads=[phb, B("rl2_%d" % rlk)], writes=[B("hT")])
                if STOP_AFTER == "B2":
                    pg.emit(final_wait_ops=[])
                    return nc
                for t2 in range(2):
                    tt = gi * 2 + t2
                    for ch in range(2):
                        pw = psW[ch]
                        pwbuf = B("psWB%d" % ch)
                        for fc in range(32):
                            O("pe", lambda e, pw=pw, fc=fc, ch=ch, t2=t2: e.matmul(pw[:, :], lhsT=hT[:, fc, t2 * 128:(t2 + 1) * 128], rhs=w2[:, fc, ch * 512:(ch + 1) * 512],
                                                                                   start=(fc == 0), stop=(fc == 31)), reads=[B("hT"), B("w2_%d" % (fc // 8))], writes=[pwbuf])
                        O("act", lambda e, pw=pw, ch=ch: e.activation(out=scrB["sq"][:, ch * 512:(ch + 1) * 512], in_=pw[:, :], func=AF.Square, accum_out=ssF[:, ch:ch + 1]),
                          reads=[pwbuf], writes=[B("Bsq"), B("ssF")])
                        O("dve", lambda e, pw=pw, ch=ch: e.tensor_tensor(out=mgB[:, ch * 512:(ch + 1) * 512], in0=pw[:, :], in1=g4b[:, ch * 512:(ch + 1) * 512], op=ALU.mult),
                          reads=[pwbuf, B("g4b"), B("ssF")], writes=[B("Bxn")])
                    O("dve", lambda e: e.tensor_tensor(out=rsF[:], in0=ssF[:, 0:1], in1=ssF[:, 1:2], op=ALU.add), reads=[B("ssF")], writes=[B("rsF")])
                    O("act", lambda e: e.activation(out=rsF[:], in_=rsF[:], func=AF.Sqrt, scale=1.0 / D, bias=epsb[:]), reads=[B("rsF"), B("epsb")], writes=[B("rsF")])
                    O("dve", lambda e: e.reciprocal(out=rsF[:], in_=rsF[:]), reads=[B("rsF")], writes=[B("rsF")])
                    O("dve", lambda e, t2=t2, sl=sl: e.scalar_tensor_tensor(out=hx[sl][:, t2, :], in0=mgB, scalar=rsF[:, 0:1], in1=hx[sl][:, t2, :], op0=ALU.mult, op1=ALU.add),
                      reads=[B("Bxn"), B("rsF"), B("hx%d" % sl)], writes=[B("hx%d" % sl)])
                    st_ = O("sp", lambda e, sl=sl, t2=t2, tt=tt: e.dma_start(out=out_d[tt * 128:(tt + 1) * 128, :], in_=hx[sl][:, t2, :]), reads=[B("hx%d" % sl)],
                            writes=[B("out%d" % tt)], dma_key="ost%d" % sl)
                    final_stores.append(st_)
        pg.emit(final_wait_ops=final_stores)
    return nc


def _prep_shared(inp):
    f = lambda a: np.ascontiguousarray(np.asarray(a, dtype=np.float32))
    lam_re = f(inp["lam_re"])[0]
    lam_im = f(inp["lam_im"])[0]
    log_dt = f(inp["log_dt"])[0]
    b_re = f(inp["b_re"])[0]
    b_im = f(inp["b_im"])[0]
    c_re = f(inp["c_re"])[0]
    c_im = f(inp["c_im"])[0]
    d_skip = f(inp["d_skip"])[0]

    def pq(a):
        return f(a.reshape(16, 2, 64).transpose(1, 2, 0).reshape(128, 16))

    def gv(a):
        return f(a.reshape(8, 128).T)

    sh = {
        "w_in": f(inp["w_in"])[0],
        "w_glu": f(inp["w_glu"])[0],
        "w_a": f(inp["w_branch_a"])[0],
        "w_b": f(inp["w_branch_b"])[0],
        "w_out": f(inp["w_out"])[0],
        "w_ff1": f(inp["w_ff1"])[0],
        "w_ff2": f(inp["w_ff2"])[0],
        "g1": gv(f(inp["g_pre_mix"])[0]),
        "g3": gv(f(inp["g_pre_ffn"])[0]),
        "g2": f(inp["g_post_mix"]).reshape(1, D),
        "g4": f(inp["g_post_ffn"]).reshape(1, D),
        "bglu": f(f(inp["b_glu"])[0].reshape(4, 128).T),
        "LR": pq(lam_re),
        "LI": pq(lam_im),
        "LDT": f(np.broadcast_to(log_dt.reshape(16, 2).T[:, None, :], (2, 64, 16)).reshape(128, 16)),
        "BR": f(b_re.reshape(16, 2, 64, 16).transpose(1, 2, 0, 3).reshape(128, 256)),
        "BI": f(b_im.reshape(16, 2, 64, 16).transpose(1, 2, 0, 3).reshape(128, 256)),
        "CR": f(c_re.reshape(16, 2, 16, 64).transpose(1, 3, 0, 2).reshape(128, 256)),
        "CI": f(c_im.reshape(16, 2, 16, 64).transpose(1, 3, 0, 2).reshape(128, 256)),
        "DS": f(np.tile(d_skip.reshape(32, 16).T, (8, 1))),
    }
    return sh


def kernel(**inputs):
    x = np.asarray(inputs["x"], dtype=np.float32)
    sh = _prep_shared(inputs)
    nc = build_nc()
    in_maps = []
    for b in range(8):
        m = dict(sh)
        m["x"] = np.ascontiguousarray(x[b])
        in_maps.append(m)
    res = run_bass_kernel_spmd(nc, in_maps, core_ids=list(range(8)))
    out = np.stack([np.asarray(res.results[b]["out"], dtype=np.float32) for b in range(8)], axis=0)
    return out
```

```python
import contextlib
import math
import numpy as np
import concourse.bass as bass
import concourse.mybir as mybir
from concourse.bass_utils import run_bass_kernel_spmd

F32 = mybir.dt.float32
BF16 = mybir.dt.bfloat16
I32 = mybir.dt.int32
AF = mybir.ActivationFunctionType
ALU = mybir.AluOpType
AX = mybir.AxisListType

L = 4096
D = 1024
NEG = -1.0e30
EPS = 1e-6
ENGS = ("pe", "act", "dve", "pool", "sp")
STOP_AFTER = None
DBG_NOKS = False
DBG_NBLK = 16
DBG_QV = ""
DBG_ONLYQ = False
POOL_DMA_DEPTH = 8
DBG_NOYA = False
STRICT = False


class Buf:
    __slots__ = ("name", "last_w", "readers")

    def __init__(self, name):
        self.name = name
        self.last_w = None
        self.readers = []


class Op:
    __slots__ = ("eng", "fn", "deps", "idx", "is_dma", "dsem", "dval", "signal", "know")


class Prog:
    def __init__(self, nc):
        self.nc = nc
        self.ops = {e: [] for e in ENGS}
        self.all = []
        self.dma_sems = {}
        self.know = {e: {} for e in ENGS}

    def op(self, eng, fn, reads=(), writes=(), dma_key=None, extra_deps=()):
        o = Op()
        o.eng = eng
        o.fn = fn
        o.is_dma = dma_key is not None
        o.signal = False
        o.idx = len(self.ops[eng])
        cands = []
        for b in reads:
            if b.last_w is not None:
                cands.append((b.last_w, True))
        for b in writes:
            if b.last_w is not None:
                cands.append((b.last_w, False))
            for r in b.readers:
                cands.append((r, False))
        for p in extra_deps:
            cands.append((p, True))
        need = {}
        kn = self.know[eng]
        for (p, raw) in cands:
            if p.is_dma:
                if (not raw) and dma_key is not None and p.dsem == dma_key:
                    continue
                key = ("dma", p.dsem)
                val = p.dval
            else:
                if p.eng == eng and not raw and not STRICT:
                    continue
                key = ("eng", p.eng)
                val = p.idx
            if kn.get(key, -1) >= val:
                continue
            if key not in need or need[key][0] < val:
                need[key] = (val, p)
        o.deps = []
        for key, (val, p) in need.items():
            o.deps.append(p)
            if kn.get(key, -1) < val:
                kn[key] = val
            for k2, v2 in p.know.items():
                if kn.get(k2, -1) < v2:
                    kn[k2] = v2
        if o.is_dma:
            cnt = self.dma_sems.setdefault(dma_key, [0])
            cnt[0] += 1
            o.dsem = dma_key
            o.dval = cnt[0]
        o.know = dict(kn)
        if o.is_dma:
            o.know[("dma", o.dsem)] = o.dval
        else:
            o.know[("eng", eng)] = o.idx
        for b in reads:
            b.readers.append(o)
        for b in writes:
            b.last_w = o
            b.readers = []
        self.ops[eng].append(o)
        self.all.append(o)
        return o

    def barrier(self, dma_ops=()):
        lasts = [self.ops[e][-1] for e in ENGS if self.ops[e] and not self.ops[e][-1].is_dma]
        lasts = []
        for e in ENGS:
            for o in reversed(self.ops[e]):
                if not o.is_dma:
                    lasts.append(o)
                    break
        deps = list(lasts) + list(dma_ops)
        for e in ENGS:
            self.op(e, lambda eng: eng.nop(), extra_deps=deps)

    def emit(self, final_wait_ops=()):
        nc = self.nc
        for o in self.all:
            for p in o.deps:
                if not p.is_dma:
                    p.signal = True
        sigval = {}
        for e in ENGS:
            c = 0
            for o in self.ops[e]:
                if o.signal:
                    c += 1
                sigval[(e, o.idx)] = c
        with contextlib.ExitStack() as st:
            esem = {e: st.enter_context(nc.semaphore("sem_" + e)) for e in ENGS}
            dsem = {k: st.enter_context(nc.semaphore("dsem%d" % i)) for i, k in enumerate(self.dma_sems)}
            block = st.enter_context(nc.Block())
            engobj = {"pe": block.tensor, "act": block.scalar, "dve": block.vector,
                      "pool": block.gpsimd, "sp": block.sync}

            def run(ename, eng):
                for o in self.ops[ename]:
                    for p in o.deps:
                        if p.is_dma:
                            eng.wait_ge(dsem[p.dsem], 16 * p.dval)
                        else:
                            eng.wait_ge(esem[p.eng], sigval[(p.eng, p.idx)])
                    ins = o.fn(eng)
                    if o.is_dma:
                        ins.then_inc(dsem[o.dsem], 16)
                    elif o.signal:
                        ins.then_inc(esem[ename], 1)
                if ename == "sp":
                    fw = {}
                    for p in final_wait_ops:
                        fw[p.dsem] = max(fw.get(p.dsem, 0), p.dval)
                    for k, v in fw.items():
                        eng.wait_ge(dsem[k], 16 * v)

            for ename in ENGS:
                engobj[ename](lambda eng, ename=ename: run(ename, eng))


def build_nc(dbg=False):
    nc = bass.Bass("TRN2", target_bir_lowering=False)

    def din(name, shape, dt=F32):
        return nc.dram_tensor(name, list(shape), dt, kind="ExternalInput").ap()

    x_d = din("x", [L, D])
    w_in_d = din("w_in", [D, 4096])
    w_glu_d = din("w_glu", [512, 512])
    w_a_d = din("w_a", [512, D])
    w_b_d = din("w_b", [512, D])
    w_out_d = din("w_out", [D, D])
    w_ff1_d = din("w_ff1", [D, 4096])
    w_ff2_d = din("w_ff2", [4096, D])
    g1_d = din("g1", [128, 8])
    g3_d = din("g3", [128, 8])
    g2_d = din("g2", [1, D])
    g4_d = din("g4", [1, D])
    bglu_d = din("bglu", [128, 4])
    LR_d = din("LR", [128, 16])
    LI_d = din("LI", [128, 16])
    LDT_d = din("LDT", [128, 16])
    BR_d = din("BR", [128, 256])
    BI_d = din("BI", [128, 256])
    CR_d = din("CR", [128, 256])
    CI_d = din("CI", [128, 256])
    DS_d = din("DS", [128, 32])
    out_d = nc.dram_tensor("out", [L, D], F32, kind="ExternalOutput").ap()
    ya_d = nc.dram_tensor("ya_scr", [512, L], BF16, kind="ExternalOutput" if dbg else "Internal").ap()

    if dbg:
        dbgb_d = nc.dram_tensor("dbgb", [128, 8192], BF16, kind="ExternalOutput").ap()
        dbgf_d = nc.dram_tensor("dbgf", [128, 4096], F32, kind="ExternalOutput").ap()
    pg = Prog(nc)
    O = pg.op
    bufs = {}

    def B(name):
        if name not in bufs:
            bufs[name] = Buf(name)
        return bufs[name]

    final_stores = []
    TWO_PI = 2.0 * math.pi

    with contextlib.ExitStack() as top:
        def T(st, name, shape, dt):
            return st.enter_context(nc.sbuf_tensor("s_" + name, list(shape), dt))

        def PS(st, name, shape, dt):
            return st.enter_context(nc.psum_tensor("p_" + name, list(shape), dt))

        identf = T(top, "identf", [128, 128], F32)
        identb = T(top, "identb", [128, 128], BF16)
        O("pool", lambda e: e.memset(identf[:], 1.0), writes=[B("identf")])
        O("pool", lambda e: e.affine_select(out=identf[:], in_=identf[:], pattern=[[1, 128]], compare_op=ALU.is_equal,
                                            fill=0.0, base=0, channel_multiplier=-1), reads=[B("identf")], writes=[B("identf")])
        O("dve", lambda e: e.tensor_copy(out=identb[:], in_=identf[:]), reads=[B("identf")], writes=[B("identb")])

        pool_dmas = []

        def load(eng, dst, src, bname, key=None, after=()):
            ex = tuple(after)
            o = O(eng, lambda e: e.dma_start(out=dst, in_=src), writes=[B(bname)], dma_key=key or bname, extra_deps=ex)
            if eng == "pool":
                pool_dmas.append(o)
            return o

        def rmsnorm_T(st_name, xt, xbuf, gsb, uT_dst_fn, ubuf, ps_list, psbufs, scr, pfx, gbuf="gsb"):
            sq, ss, rs, xn = scr["sq"], scr["ss"], scr["rs"], scr["xn"]
            O("act", lambda e: e.activation(out=sq, in_=xt, func=AF.Square, accum_out=ss), reads=[xbuf], writes=[B(pfx + "sq"), B(pfx + "ss")])
            O("act", lambda e: e.activation(out=rs, in_=ss, func=AF.Sqrt, scale=1.0 / D, bias=scr["eps"]), reads=[B(pfx + "ss"), B("epsb")], writes=[B(pfx + "rs")])
            O("dve", lambda e: e.reciprocal(out=rs, in_=rs), reads=[B(pfx + "rs")], writes=[B(pfx + "rs")])
            O("dve", lambda e: e.tensor_scalar(out=xn, in0=xt, scalar1=rs, scalar2=None, op0=ALU.mult), reads=[xbuf, B(pfx + "rs")], writes=[B(pfx + "xn")])
            for half in range(2):
                ps = ps_list[half]
                pb = psbufs[half]
                for k in range(4):
                    dc = half * 4 + k
                    O("pe", lambda e, dc=dc, k=k, ps=ps: e.transpose(out=ps[:, k * 128:(k + 1) * 128], in_=xn[:, dc * 128:(dc + 1) * 128], identity=identf[:]),
                      reads=[B(pfx + "xn"), B("identf")], writes=[pb])
                for k in range(4):
                    dc = half * 4 + k
                    eng = "act" if k % 2 == 0 else "dve"
                    if eng == "act":
                        O("act", lambda e, dc=dc, k=k, ps=ps: e.activation(out=uT_dst_fn(dc), in_=ps[:, k * 128:(k + 1) * 128], func=AF.Copy, scale=gsb[:, dc:dc + 1]),
                          reads=[pb, B(gbuf)], writes=[ubuf])
                    else:
                        O("dve", lambda e, dc=dc, k=k, ps=ps: e.tensor_scalar(out=uT_dst_fn(dc), in0=ps[:, k * 128:(k + 1) * 128], scalar1=gsb[:, dc:dc + 1], scalar2=None, op0=ALU.mult),
                          reads=[pb, B(gbuf)], writes=[ubuf])

        epsb = T(top, "epsb", [128, 1], F32)
        O("dve", lambda e: e.memset(epsb[:], EPS), writes=[B("epsb")])
        g1sb = T(top, "g1sb", [128, 8], F32)
        g3sb = T(top, "g3sb", [128, 8], F32)
        bglu = T(top, "bglu", [128, 4], F32)
        load("sp", g1sb[:], g1_d, "gsb", "c0")
        load("sp", g3sb[:], g3_d, "gsb3", "c1")
        load("sp", bglu[:], bglu_d, "bglu", "c2")

        sKV = top.enter_context(contextlib.ExitStack())
        KT = T(sKV, "KT", [128, 4, L], BF16)
        VA = T(sKV, "VA", [128, 32, 8, 66], BF16)
        KS = T(sKV, "KS", [128, 4, 16], F32)
        O("pool", lambda e: e.memset(VA[:, :, :, 64:65], 1.0), writes=[B("VAones")])
        O("pool", lambda e: e.memset(VA[:, :, :, 65:66], 0.0), writes=[B("VAzeros")])

        with contextlib.ExitStack() as sS:
            MI = T(sS, "MI", [128, 32, 128], BF16)
            MinR = T(sS, "MinR", [128, 32, 64], BF16)
            MinI = T(sS, "MinI", [128, 32, 64], BF16)
            MoRp = T(sS, "MoRp", [128, 16, 2, 128], BF16)
            MoIp = T(sS, "MoIp", [128, 16, 2, 128], BF16)
            RHO8 = T(sS, "RHO8", [128, 16], F32)
            TH8 = T(sS, "TH8", [128, 16], F32)

            with contextlib.ExitStack() as s0:
                LR = T(s0, "LR", [128, 16], F32)
                LI = T(s0, "LI", [128, 16], F32)
                DT = T(s0, "DT", [128, 16], F32)
                BR = T(s0, "BR", [128, 16, 16], F32)
                BI = T(s0, "BI", [128, 16, 16], F32)
                CR = T(s0, "CR", [128, 16, 16], F32)
                CI = T(s0, "CI", [128, 16, 16], F32)
                DS = T(s0, "DS", [128, 32], F32)
                NVi = T(s0, "NVi", [128, 17], I32)
                NV = T(s0, "NV", [128, 17], F32)
                LRDT = T(s0, "LRDT", [128, 16], F32)
                TH = T(s0, "TH", [128, 16], F32)
                ARG = T(s0, "ARG", [128, 16, 17], F32)
                ARK = T(s0, "ARK", [128, 16, 17], F32)
                ARKi = T(s0, "ARKi", [128, 16, 17], I32)
                MAG = T(s0, "MAG", [128, 16, 17], F32)
                PWR = T(s0, "PWR", [128, 16, 17], F32)
                PWI = T(s0, "PWI", [128, 16, 17], F32)
                zt1 = T(s0, "zt1", [128, 16], F32)
                zt2 = T(s0, "zt2", [128, 16], F32)
                zt3 = T(s0, "zt3", [128, 16], F32)
                cfr = T(s0, "cfr", [128, 16], F32)
                cfi = T(s0, "cfi", [128, 16], F32)
                BBR = T(s0, "BBR", [128, 16, 16], F32)
                BBI = T(s0, "BBI", [128, 16, 16], F32)
                tb = T(s0, "tb", [128, 16, 16], F32)
                HR = T(s0, "HR", [128, 16, 8, 16], F32)
                HI = T(s0, "HI", [128, 16, 8, 16], F32)
                GR = T(s0, "GR", [128, 16, 8, 16], F32)
                GI = T(s0, "GI", [128, 16, 8, 16], F32)
                MOR = T(s0, "MOR", [128, 16, 8, 16], F32)
                MOI = T(s0, "MOI", [128, 16, 8, 16], F32)
                tq = T(s0, "tq", [128, 16, 8, 16], F32)
                msk = T(s0, "msk", [128, 8, 16], F32)
                tmi = T(s0, "tmi", [128, 128], F32)
                ps_s0 = [PS(s0, "ps_s0_%d" % k, [128, 512], F32) for k in range(4)]

                load("sp", LR[:], LR_d, "LR", "c3")
                load("sp", LI[:], LI_d, "LI", "c4")
                load("sp", DT[:], LDT_d, "DT", "c5")
                load("sp", BR[:].rearrange("p a b -> p (a b)"), BR_d, "BR", "c6")
                load("sp", BI[:].rearrange("p a b -> p (a b)"), BI_d, "BI", "c7")
                load("sp", CR[:].rearrange("p a b -> p (a b)"), CR_d, "CR", "c8")
                load("sp", CI[:].rearrange("p a b -> p (a b)"), CI_d, "CI", "c9")
                load("sp", DS[:], DS_d, "DS", "c10")

                def V_(fn, r, w, eng="dve"):
                    return O(eng, fn, reads=[B(n) for n in r], writes=[B(n) for n in w])

                V_(lambda e: e.iota(NVi[:, 0:8], pattern=[[-1, 8]], base=-1, channel_multiplier=0), [], ["NVi"], "pool")
                V_(lambda e: e.iota(NVi[:, 8:17], pattern=[[1, 9]], base=0, channel_multiplier=0), [], ["NVi"], "pool")
                V_(lambda e: e.tensor_copy(out=NV[:], in_=NVi[:]), ["NVi"], ["NV"])
                V_(lambda e: e.activation(out=DT[:], in_=DT[:], func=AF.Exp), ["DT"], ["DT"], "act")
                V_(lambda e: e.tensor_tensor(out=LRDT[:], in0=LR[:], in1=DT[:], op=ALU.mult), ["LR", "DT"], ["LRDT"])
                V_(lambda e: e.tensor_tensor(out=TH[:], in0=LI[:], in1=DT[:], op=ALU.mult), ["LI", "DT"], ["TH"])
                bc3 = lambda a: a.unsqueeze(2).to_broadcast([128, 16, 17])
                nvb = NV[:].unsqueeze(1).to_broadcast([128, 16, 17])
                V_(lambda e: e.tensor_tensor(out=ARG[:], in0=bc3(LRDT[:]), in1=nvb, op=ALU.mult), ["LRDT", "NV"], ["ARG"])
                V_(lambda e: e.activation(out=MAG[:], in_=ARG[:], func=AF.Exp), ["ARG"], ["MAG"], "act")

                def sin_of(dst, src_fn, srcbufs, dstbuf, shift, shape3):
                    scrF, scrI = shape3
                    V_(lambda e: e.tensor_scalar(out=scrF, in0=src_fn(), scalar1=shift, scalar2=1.0 / TWO_PI, op0=ALU.add, op1=ALU.mult), srcbufs, ["scrF"])
                    V_(lambda e: e.tensor_copy(out=scrI, in_=scrF), ["scrF"], ["scrI"])
                    V_(lambda e: e.tensor_copy(out=scrF, in_=scrI), ["scrI"], ["scrF"])
                    V_(lambda e: e.tensor_scalar(out=scrF, in0=scrF, scalar1=-TWO_PI, scalar2=shift, op0=ALU.mult, op1=ALU.add), ["scrF"], ["scrF"])
                    V_(lambda e: e.tensor_tensor(out=scrF, in0=scrF, in1=src_fn(), op=ALU.add), ["scrF"] + srcbufs, ["scrF"])
                    V_(lambda e: e.tensor_scalar(out=scrF, in0=scrF, scalar1=3.14159, scalar2=-3.14159, op0=ALU.min, op1=ALU.max), ["scrF"], ["scrF"])
                    V_(lambda e: e.activation(out=dst, in_=scrF, func=AF.Sin), ["scrF"], [dstbuf], "act")

                V_(lambda e: e.tensor_tensor(out=ARG[:], in0=bc3(TH[:]), in1=nvb, op=ALU.mult), ["TH", "NV", "MAG"], ["ARG"])
                sin_of(PWI[:], lambda: ARG[:], ["ARG"], "PWI", 0.0, (ARK[:], ARKi[:]))
                sin_of(PWR[:], lambda: ARG[:], ["ARG"], "PWR", math.pi / 2, (ARK[:], ARKi[:]))
                V_(lambda e: e.tensor_tensor(out=PWR[:], in0=PWR[:], in1=MAG[:], op=ALU.mult), ["PWR", "MAG"], ["PWR"])
                V_(lambda e: e.tensor_tensor(out=PWI[:], in0=PWI[:], in1=MAG[:], op=ALU.mult), ["PWI", "MAG"], ["PWI"])
                V_(lambda e: e.tensor_copy(out=RHO8[:], in_=MAG[:, :, 16]), ["MAG"], ["RHO8"])
                V_(lambda e: e.tensor_scalar(out=TH8[:], in0=TH[:], scalar1=8.0, scalar2=None, op0=ALU.mult), ["TH"], ["TH8"])
                abr = PWR[:, :, 9]
                abi = PWI[:, :, 9]
                V_(lambda e: e.tensor_scalar(out=zt1[:], in0=abr, scalar1=-1.0, scalar2=None, op0=ALU.add), ["PWR"], ["zt1"])
                V_(lambda e: e.tensor_tensor(out=zt2[:], in0=LR[:], in1=LR[:], op=ALU.mult), ["LR"], ["zt2"])
                V_(lambda e: e.tensor_tensor(out=zt3[:], in0=LI[:], in1=LI[:], op=ALU.mult), ["LI"], ["zt3"])
                V_(lambda e: e.tensor_tensor(out=zt2[:], in0=zt2[:], in1=zt3[:], op=ALU.add), ["zt2", "zt3"], ["zt2"])
                V_(lambda e: e.reciprocal(out=zt2[:], in_=zt2[:]), ["zt2"], ["zt2"])
                V_(lambda e: e.tensor_tensor(out=cfr[:], in0=zt1[:], in1=LR[:], op=ALU.mult), ["zt1", "LR"], ["cfr"])
                V_(lambda e: e.tensor_tensor(out=zt3[:], in0=abi, in1=LI[:], op=ALU.mult), ["PWI", "LI"], ["zt3"])
                V_(lambda e: e.tensor_tensor(out=cfr[:], in0=cfr[:], in1=zt3[:], op=ALU.add), ["cfr", "zt3"], ["cfr"])
                V_(lambda e: e.tensor_tensor(out=cfr[:], in0=cfr[:], in1=zt2[:], op=ALU.mult), ["cfr", "zt2"], ["cfr"])
                V_(lambda e: e.tensor_tensor(out=cfi[:], in0=abi, in1=LR[:], op=ALU.mult), ["PWI", "LR"], ["cfi"])
                V_(lambda e: e.tensor_tensor(out=zt3[:], in0=zt1[:], in1=LI[:], op=ALU.mult), ["zt1", "LI"], ["zt3"])
                V_(lambda e: e.tensor_tensor(out=cfi[:], in0=cfi[:], in1=zt3[:], op=ALU.subtract), ["cfi", "zt3"], ["cfi"])
                V_(lambda e: e.tensor_tensor(out=cfi[:], in0=cfi[:], in1=zt2[:], op=ALU.mult), ["cfi", "zt2"], ["cfi"])
                bh = lambda a: a.unsqueeze(2).to_broadcast([128, 16, 16])
                V_(lambda e: e.tensor_tensor(out=BBR[:], in0=bh(cfr[:]), in1=BR[:], op=ALU.mult), ["cfr", "BR"], ["BBR"])
                V_(lambda e: e.tensor_tensor(out=tb[:], in0=bh(cfi[:]), in1=BI[:], op=ALU.mult), ["cfi", "BI"], ["tb"])
                V_(lambda e: e.tensor_tensor(out=BBR[:], in0=BBR[:], in1=tb[:], op=ALU.subtract), ["BBR", "tb"], ["BBR"])
                V_(lambda e: e.tensor_tensor(out=BBI[:], in0=bh(cfr[:]), in1=BI[:], op=ALU.mult), ["cfr", "BI"], ["BBI"])
                V_(lambda e: e.tensor_tensor(out=tb[:], in0=bh(cfi[:]), in1=BR[:], op=ALU.mult), ["cfi", "BR"], ["tb"])
                V_(lambda e: e.tensor_tensor(out=BBI[:], in0=BBI[:], in1=tb[:], op=ALU.add), ["BBI", "tb"], ["BBI"])
                pwb = lambda a, lo: a[:, :, lo:lo + 8].unsqueeze(3).to_broadcast([128, 16, 8, 16])
                b4 = lambda a: a.unsqueeze(2).to_broadcast([128, 16, 8, 16])
                V_(lambda e: e.tensor_tensor(out=HR[:], in0=pwb(PWR, 0), in1=b4(BBR[:]), op=ALU.mult), ["PWR", "BBR"], ["HR"])
                V_(lambda e: e.tensor_tensor(out=tq[:], in0=pwb(PWI, 0), in1=b4(BBI[:]), op=ALU.mult), ["PWI", "BBI"], ["tq"])
                V_(lambda e: e.tensor_tensor(out=HR[:], in0=HR[:], in1=tq[:], op=ALU.subtract), ["HR", "tq"], ["HR"])
                V_(lambda e: e.tensor_tensor(out=HI[:], in0=pwb(PWR, 0), in1=b4(BBI[:]), op=ALU.mult), ["PWR", "BBI"], ["HI"])
                V_(lambda e: e.tensor_tensor(out=tq[:], in0=pwb(PWI, 0), in1=b4(BBR[:]), op=ALU.mult), ["PWI", "BBR"], ["tq"])
                V_(lambda e: e.tensor_tensor(out=HI[:], in0=HI[:], in1=tq[:], op=ALU.add), ["HI", "tq"], ["HI"])
                f3 = lambda a: a.rearrange("p q i h -> p q (i h)")
                a8 = lambda a: a[:, :, 16:17].to_broadcast([128, 16, 128])
                V_(lambda e: e.tensor_tensor(out=f3(GR[:]), in0=a8(PWR), in1=f3(HR[:]), op=ALU.mult), ["PWR", "HR"], ["GR"])
                V_(lambda e: e.tensor_tensor(out=f3(tq[:]), in0=a8(PWI), in1=f3(HI[:]), op=ALU.mult), ["PWI", "HI"], ["tq"])
                V_(lambda e: e.tensor_tensor(out=GR[:], in0=GR[:], in1=tq[:], op=ALU.subtract), ["GR", "tq"], ["GR"])
                V_(lambda e: e.tensor_tensor(out=f3(GI[:]), in0=a8(PWR), in1=f3(HI[:]), op=ALU.mult), ["PWR", "HI"], ["GI"])
                V_(lambda e: e.tensor_tensor(out=f3(tq[:]), in0=a8(PWI), in1=f3(HR[:]), op=ALU.mult), ["PWI", "HR"], ["tq"])
                V_(lambda e: e.tensor_tensor(out=GI[:], in0=GI[:], in1=tq[:], op=ALU.add), ["GI", "tq"], ["GI"])
                V_(lambda e: e.tensor_tensor(out=MOR[:], in0=pwb(PWR, 9), in1=b4(CR[:]), op=ALU.mult), ["PWR", "CR"], ["MOR"])
                V_(lambda e: e.tensor_tensor(out=tq[:], in0=pwb(PWI, 9), in1=b4(CI[:]), op=ALU.mult), ["PWI", "CI"], ["tq"])
                V_(lambda e: e.tensor_tensor(out=MOR[:], in0=MOR[:], in1=tq[:], op=ALU.subtract), ["MOR", "tq"], ["MOR"])
                V_(lambda e: e.tensor_tensor(out=MOI[:], in0=pwb(PWI, 9), in1=b4(CR[:]), op=ALU.mult), ["PWI", "CR"], ["MOI"])
                V_(lambda e: e.tensor_tensor(out=tq[:], in0=pwb(PWR, 9), in1=b4(CI[:]), op=ALU.mult), ["PWR", "CI"], ["tq"])
                V_(lambda e: e.tensor_tensor(out=MOI[:], in0=MOI[:], in1=tq[:], op=ALU.add), ["MOI", "tq"], ["MOI"])
                V_(lambda e: e.tensor_scalar(out=MOI[:], in0=MOI[:], scalar1=-1.0, scalar2=None, op0=ALU.mult), ["MOI"], ["MOI"])
                V_(lambda e: e.memset(MoRp[:], 0.0), [], ["MoRp"], "pool")
                V_(lambda e: e.memset(MoIp[:], 0.0), [], ["MoIp"], "pool")
                for m in range(2):
                    rows = slice(64 * m, 64 * m + 64)
                    V_(lambda e, rows=rows, m=m: e.tensor_copy(out=MoRp[rows, :, m, :], in_=f3(MOR[:])[rows, :, :]), ["MOR", "MoRp"], ["MoRp"])
                    V_(lambda e, rows=rows, m=m: e.tensor_copy(out=MoIp[rows, :, m, :], in_=f3(MOI[:])[rows, :, :]), ["MOI", "MoIp"], ["MoIp"])
                V_(lambda e: e.memset(msk[:], 1.0), [], ["msk"], "pool")
                V_(lambda e: e.affine_select(out=msk[:], in_=msk[:], pattern=[[16, 8], [0, 16]], compare_op=ALU.is_ge,
                                             fill=0.0, base=15, channel_multiplier=-1), ["msk"], ["msk"], "pool")
                for g in range(32):
                    q, m = g // 2, g % 2
                    rows = slice(64 * m, 64 * m + 64)
                    ps = ps_s0[g % 4]
                    pb = B("ps_s0_%d" % (g % 4))
                    O("pe", lambda e, ps=ps, rows=rows, q=q: e.transpose(out=ps[:, 0:64], in_=f3(GR[:])[rows, q, :], identity=identf[rows, rows]),
                      reads=[B("GR"), B("identf")], writes=[pb])
                    O("pe", lambda e, ps=ps, rows=rows, q=q: e.transpose(out=ps[:, 64:128], in_=f3(GI[:])[rows, q, :], identity=identf[rows, rows]),
                      reads=[B("GI"), B("identf")], writes=[pb])
                    O("pe", lambda e, ps=ps, rows=rows, q=q: e.matmul(ps[:, 128:256], lhsT=f3(HR[:])[rows, q, :], rhs=f3(MOR[:])[rows, q, :], start=True, stop=False),
                      reads=[B("HR"), B("MOR")], writes=[pb])
                    O("pe", lambda e, ps=ps, rows=rows, q=q: e.matmul(ps[:, 128:256], lhsT=f3(HI[:])[rows, q, :], rhs=f3(MOI[:])[rows, q, :], start=False, stop=True),
                      reads=[B("HI"), B("MOI")], writes=[pb])
                    O("act", lambda e, ps=ps, g=g: e.activation(func=AF.Identity, out=MinR[:, g, :], in_=ps[:, 0:64]), reads=[pb], writes=[B("MinR")])
                    O("act", lambda e, ps=ps, g=g: e.activation(func=AF.Identity, out=MinI[:, g, :], in_=ps[:, 64:128]), reads=[pb], writes=[B("MinI")])
                    O("dve", lambda e, ps=ps: e.tensor_tensor(out=tmi[:], in0=ps[:, 128:256], in1=msk[:].rearrange("p j h -> p (j h)"), op=ALU.mult),
                      reads=[pb, B("msk")], writes=[B("tmi")])
                    O("dve", lambda e, g=g: e.scalar_tensor_tensor(out=MI[:, g, :], in0=identf[:], scalar=DS[:, g:g + 1], in1=tmi[:], op0=ALU.mult, op1=ALU.add),
                      reads=[B("identf"), B("DS"), B("tmi")], writes=[B("MI")])
                if STOP_AFTER == "S0":
                    d1 = O("sp", lambda e: e.dma_start(out=dbgb_d[:, 0:4096], in_=MI[:].rearrange("p g f -> p (g f)")), reads=[B("MI")], dma_key="dbg1")
                    d2 = O("sp", lambda e: e.dma_start(out=dbgb_d[:, 4096:6144], in_=MinR[:].rearrange("p g f -> p (g f)")), reads=[B("MinR")], dma_key="dbg2")
                    d3 = O("sp", lambda e: e.dma_start(out=dbgb_d[:, 6144:8192], in_=MoRp[:, 0:8, :, :].rearrange("p q m f -> p (q m f)")), reads=[B("MoRp")], dma_key="dbg3")
                    d4 = O("sp", lambda e: e.dma_start(out=dbgf_d[:, 0:272], in_=PWR[:].rearrange("p q n -> p (q n)")), reads=[B("PWR")], dma_key="dbg4")
                    d5 = O("sp", lambda e: e.dma_start(out=dbgf_d[:, 272:544], in_=PWI[:].rearrange("p q n -> p (q n)")), reads=[B("PWI")], dma_key="dbg5")
                    d6 = O("sp", lambda e: e.dma_start(out=dbgf_d[:, 544:2592], in_=HR[:].rearrange("p q i h -> p (q i h)")), reads=[B("HR")], dma_key="dbg6")
                    pg.emit(final_wait_ops=[d1, d2, d3, d4, d5, d6])
                    return nc
            pg.barrier()

            with contextlib.ExitStack() as s1:
                Zt = T(s1, "Zt", [128, 4, 32, 8, 16], BF16)
                wS = T(s1, "wS", [128, 8, 512], BF16)
                wK = T(s1, "wK", [128, 8, 512], BF16)
                wV = T(s1, "wV", [128, 8, 512], BF16)
                wG = T(s1, "wG", [128, 4, 512], BF16)
                for dc in range(8):
                    rows = slice(dc * 128, (dc + 1) * 128)
                    load("pool", wS[:, dc, :], w_in_d[rows, 0:512], "wS", "wS")
                    load("pool", wK[:, dc, :], w_in_d[rows, 1024:1536], "wK", "wK")
                    load("pool", wV[:, dc, :], w_in_d[rows, 1536:2048], "wV", "wV")
                for fc in range(4):
                    load("pool", wG[:, fc, :], w_glu_d[fc * 128:(fc + 1) * 128, :], "wG", "wG")

                if STOP_AFTER == "S1a0":
                    d1 = O("sp", lambda e: e.dma_start(out=dbgb_d[:, 0:4096], in_=wS[:].rearrange("p a b -> p (a b)")), reads=[B("wS")], dma_key="dbg1")
                    d2 = O("sp", lambda e: e.dma_start(out=dbgb_d[:, 4096:6144], in_=wG[:].rearrange("p a b -> p (a b)")), reads=[B("wG")], dma_key="dbg2")
                    pg.emit(final_wait_ops=[d1, d2])
                    return nc
                with contextlib.ExitStack() as s1a:
                    uT = T(s1a, "uT", [128, 8, 1024], BF16)
                    xt = [T(s1a, "xt%d" % k, [128, D], F32) for k in range(2)]
                    scr = {"sq": T(s1a, "sq", [128, D], BF16)[:], "ss": T(s1a, "ss", [128, 1], F32)[:], "rs": T(s1a, "rs", [128, 1], F32)[:],
                           "xn": T(s1a, "xn", [128, D], F32)[:], "eps": epsb[:]}
                    psT = [PS(s1a, "psT%d" % k, [128, 512], F32) for k in range(2)]
                    psM = [PS(s1a, "psM%d" % k, [128, 512], F32) for k in range(4)]
                    psTb = [B("psT0"), B("psT1")]
                    nmm = [0]

                    def next_psM():
                        k = nmm[0] % 4
                        nmm[0] += 1
                        return psM[k], B("psM%d" % k)

                    x_loads = {}

                    def issue_xload(tt):
                        slot = tt % 2
                        x_loads[tt] = load("sp", xt[slot][:], x_d[tt * 128:(tt + 1) * 128, :], "xt%d" % slot, "xt%d" % slot)

                    issue_xload(0)
                    for s in range(4):
                        for t8 in range(8):
                            tt = s * 8 + t8
                            if tt + 1 < 32:
                                issue_xload(tt + 1)
                            slot = tt % 2
                            rmsnorm_T("S", xt[slot][:], B("xt%d" % slot), g1sb,
                                      lambda dc, t8=t8: uT[:, dc, t8 * 128:(t8 + 1) * 128], B("uT"), psT, psTb, scr, "S")
                        if STOP_AFTER == "S1a1":
                            d1 = O("sp", lambda e: e.dma_start(out=dbgb_d[:, 0:8192], in_=uT[:].rearrange("p a b -> p (a b)")), reads=[B("uT"), B("Zt0"), B("KT"), B("VA"), B("KS")], dma_key="dbg1")
                            pg.emit(final_wait_ops=[d1])
                            return nc
                        for i in range(8):
                            ps, pb = next_psM()
                            for dc in range(8):
                                O("pe", lambda e, ps=ps, dc=dc, i=i: e.matmul(ps[:, :], lhsT=uT[:, dc, :].rearrange("p (c i) -> p i c", i=8)[:, i, :],
                                                                               rhs=wS[:, dc, :], start=(dc == 0), stop=(dc == 7)),
                                  reads=[B("uT"), B("wS")], writes=[pb])
                            eng = "act" if i % 2 == 0 else "dve"
                            if eng == "act":
                                O("act", lambda e, ps=ps, s=s, i=i: e.activation(func=AF.Identity, out=Zt[:, s, :, i, :], in_=ps[:, :].rearrange("p (g h) -> p g h", g=32)), reads=[pb], writes=[B("Zt%d" % q_) for q_ in range(16)])
                            else:
                                O("dve", lambda e, ps=ps, s=s, i=i: e.tensor_copy(out=Zt[:, s, :, i, :], in_=ps[:, :].rearrange("p (g h) -> p g h", g=32)), reads=[pb], writes=[B("Zt%d" % q_) for q_ in range(16)])
                        if STOP_AFTER == "S1a2":
                            d1 = O("sp", lambda e: e.dma_start(out=dbgb_d[:, 0:8192], in_=uT[:].rearrange("p a b -> p (a b)")), reads=[B("uT"), B("Zt0"), B("KT"), B("VA"), B("KS")], dma_key="dbg1")
                            pg.emit(final_wait_ops=[d1])
                            return nc
                        for hp in range(4):
                            for th in range(2):
                                ps, pb = next_psM()
                                for dc in range(8):
                                    O("pe", lambda e, ps=ps, dc=dc, hp=hp, th=th: e.matmul(ps[:, :], lhsT=wK[:, dc, hp * 128:(hp + 1) * 128],
                                                                                           rhs=uT[:, dc, th * 512:(th + 1) * 512], start=(dc == 0), stop=(dc == 7)),
                                      reads=[B("uT"), B("wK")], writes=[pb])
                                tok0 = s * 1024 + th * 512
                                for bb in range(2):
                                    blk = tok0 // 256 + bb
                                    O("act", lambda e, ps=ps, hp=hp, tok0=tok0, bb=bb, blk=blk: e.activation(
                                        out=KT[:, hp, tok0 + bb * 256:tok0 + (bb + 1) * 256], in_=ps[:, bb * 256:(bb + 1) * 256], func=AF.Copy,
                                        accum_out=KS[:, hp, blk:blk + 1]), reads=[pb], writes=[B("KT"), B("KS")])
                        if STOP_AFTER == "S1a3":
                            d1 = O("sp", lambda e: e.dma_start(out=dbgb_d[:, 0:8192], in_=uT[:].rearrange("p a b -> p (a b)")), reads=[B("uT"), B("Zt0"), B("KT"), B("VA"), B("KS")], dma_key="dbg1")
                            pg.emit(final_wait_ops=[d1])
                            return nc
                        for t8 in range(8):
                            tt = s * 8 + t8
                            ps, pb = next_psM()
                            for dc in range(8):
                                O("pe", lambda e, ps=ps, dc=dc, t8=t8: e.matmul(ps[:, :], lhsT=uT[:, dc, t8 * 128:(t8 + 1) * 128], rhs=wV[:, dc, :],
                                                                                start=(dc == 0), stop=(dc == 7)), reads=[B("uT"), B("wV")], writes=[pb])
                            if t8 % 2 == 0:
                                O("act", lambda e, ps=ps, tt=tt: e.activation(func=AF.Identity, out=VA[:, tt, :, 0:64], in_=ps[:, :].rearrange("p (h d) -> p h d", h=8)), reads=[pb], writes=[B("VA")])
                            else:
                                O("dve", lambda e, ps=ps, tt=tt: e.tensor_copy(out=VA[:, tt, :, 0:64], in_=ps[:, :].rearrange("p (h d) -> p h d", h=8)), reads=[pb], writes=[B("VA")])
                pg.barrier()
                if STOP_AFTER == "S1a":
                    d1 = O("sp", lambda e: e.dma_start(out=dbgb_d[:, 0:4096], in_=KT[:, 1, :]), reads=[B("KT")], dma_key="dbg1")
                    d2 = O("sp", lambda e: e.dma_start(out=dbgb_d[:, 4096:8192], in_=Zt[:, 1, :, :, :].rearrange("p g i h -> p (g i h)")), reads=[B("Zt0")], dma_key="dbg2")
                    d3 = O("sp", lambda e: e.dma_start(out=dbgf_d[:, 0:64], in_=KS[:].rearrange("p a b -> p (a b)")), reads=[B("KS")], dma_key="dbg3")
                    pg.emit(final_wait_ops=[d1, d2, d3])
                    return nc

                with contextlib.ExitStack() as s1b:
                    Ug = [T(s1b, "Ug%d" % m, [128, 512], BF16) for m in range(2)]
                    CI_i = T(s1b, "CIi", [128, 512], I32)
                    CIf = T(s1b, "CIf", [128, 512], F32)
                    ANG = T(s1b, "ANG", [128, 512], F32)
                    sF = T(s1b, "sF", [128, 512], F32)
                    sI = T(s1b, "sI", [128, 512], I32)
                    COSC = T(s1b, "COSC", [128, 512], F32)
                    SINC = T(s1b, "SINC", [128, 512], F32)
                    WR = T(s1b, "WR", [128, 512], F32)
                    WI = T(s1b, "WI", [128, 512], F32)
                    ta = T(s1b, "ta", [128, 512], F32)
                    tbb = T(s1b, "tbb", [128, 512], F32)
                    XPR = T(s1b, "XPR", [128, 516], BF16)
                    XPI = T(s1b, "XPI", [128, 516], BF16)
                    gs = T(s1b, "gs", [128, 512], F32)
                    gu = T(s1b, "gu", [128, 512], F32)
                    psU = PS(s1b, "psU", [128, 1024], BF16)
                    psXr = PS(s1b, "psXr", [128, 512], F32)
                    psXi = PS(s1b, "psXi", [128, 512], F32)
                    psY = [PS(s1b, "psY%d" % m, [128, 512], F32) for m in range(2)]

                    O("pool", lambda e: e.iota(CI_i[:], pattern=[[1, 512]], base=0, channel_multiplier=0), writes=[B("CIi")])
                    O("dve", lambda e: e.tensor_copy(out=CIf[:], in_=CI_i[:]), reads=[B("CIi")], writes=[B("CIf")])
                    O("dve", lambda e: e.memset(XPR[:, 0:1], 0.0), writes=[B("XPR")])
                    O("dve", lambda e: e.memset(XPI[:, 0:1], 0.0), writes=[B("XPI")])

                    def V2(fn, r, w, eng="dve"):
                        return O(eng, fn, reads=[B(n) for n in r], writes=[B(n) for n in w])

                    def sin2(dst, dstbuf, shift):
                        V2(lambda e: e.tensor_scalar(out=sF[:], in0=ANG[:], scalar1=shift, scalar2=1.0 / TWO_PI, op0=ALU.add, op1=ALU.mult), ["ANG"], ["sF"])
                        V2(lambda e: e.tensor_copy(out=sI[:], in_=sF[:]), ["sF"], ["sI"])
                        V2(lambda e: e.tensor_copy(out=sF[:], in_=sI[:]), ["sI"], ["sF"])
                        V2(lambda e: e.tensor_scalar(out=sF[:], in0=sF[:], scalar1=-TWO_PI, scalar2=shift, op0=ALU.mult, op1=ALU.add), ["sF"], ["sF"])
                        V2(lambda e: e.tensor_tensor(out=sF[:], in0=sF[:], in1=ANG[:], op=ALU.add), ["sF", "ANG"], ["sF"])
                        V2(lambda e: e.tensor_scalar(out=sF[:], in0=sF[:], scalar1=3.14159, scalar2=-3.14159, op0=ALU.min, op1=ALU.max), ["sF"], ["sF"])
                        V2(lambda e: e.activation(out=dst, in_=sF[:], func=AF.Sin), ["sF"], [dstbuf], "act")

                    for q in range(16):
                        zb = B("Zt%d" % q)
                        for m in range(2):
                            g = 2 * q + m
                            for s in range(4):
                                O("pe", lambda e, s=s, g=g, m=m: e.transpose(out=psU[:, m * 512 + s * 128: m * 512 + (s + 1) * 128],
                                                                             in_=Zt[:, s, g, :, :].rearrange("p i h -> p (i h)"), identity=identb[:]),
                                  reads=[zb, B("identb")], writes=[B("psU")])
                            if m == 0:
                                O("act", lambda e, m=m: e.activation(func=AF.Identity, out=Ug[m][:], in_=psU[:, m * 512:(m + 1) * 512]), reads=[B("psU")], writes=[B("Ug%d" % m)])
                            else:
                                O("dve", lambda e, m=m: e.tensor_copy(out=Ug[m][:], in_=psU[:, m * 512:(m + 1) * 512]), reads=[B("psU")], writes=[B("Ug%d" % m)])
                        for m in range(2):
                            g = 2 * q + m
                            O("pe", lambda e, m=m, g=g: e.matmul(psXr[64 * m:64 * m + 64, :], lhsT=MinR[:, g, :], rhs=Ug[m][:], start=True, stop=True),
                              reads=[B("MinR"), B("Ug%d" % m)], writes=[B("psXr")])
                            O("pe", lambda e, m=m, g=g: e.matmul(psXi[64 * m:64 * m + 64, :], lhsT=MinI[:, g, :], rhs=Ug[m][:], start=True, stop=True),
                              reads=[B("MinI"), B("Ug%d" % m)], writes=[B("psXi")])
                        V2(lambda e, q=q: e.tensor_scalar(out=ANG[:], in0=CIf[:], scalar1=TH8[:, q:q + 1], scalar2=None, op0=ALU.mult), ["CIf", "TH8"], ["ANG"])
                        sin2(SINC[:], "SINC", 0.0)
                        sin2(COSC[:], "COSC", math.pi / 2)
                        V2(lambda e: e.tensor_tensor(out=ta[:], in0=psXr[:], in1=COSC[:], op=ALU.mult), ["psXr", "COSC"], ["ta"])
                        V2(lambda e: e.tensor_tensor(out=tbb[:], in0=psXi[:], in1=SINC[:], op=ALU.mult), ["psXi", "SINC"], ["tbb"])
                        V2(lambda e: e.tensor_tensor(out=ta[:], in0=ta[:], in1=tbb[:], op=ALU.add), ["ta", "tbb"], ["ta"])
                        V2(lambda e, q=q: e.tensor_tensor_scan(out=WR[:], data0=RHO8[:, q:q + 1].to_broadcast([128, 512]), data1=ta[:], initial=0.0,
                                                                op0=ALU.mult, op1=ALU.add), ["ta", "RHO8"], ["WR"])
                        V2(lambda e: e.tensor_tensor(out=ta[:], in0=psXi[:], in1=COSC[:], op=ALU.mult), ["psXi", "COSC", "WR"], ["ta"])
                        V2(lambda e: e.tensor_tensor(out=tbb[:], in0=psXr[:], in1=SINC[:], op=ALU.mult), ["psXr", "SINC"], ["tbb"])
                        V2(lambda e: e.tensor_tensor(out=ta[:], in0=ta[:], in1=tbb[:], op=ALU.subtract), ["ta", "tbb"], ["ta"])
                        V2(lambda e, q=q: e.tensor_tensor_scan(out=WI[:], data0=RHO8[:, q:q + 1].to_broadcast([128, 512]), data1=ta[:], initial=0.0,
                                                                op0=ALU.mult, op1=ALU.add), ["ta", "RHO8"], ["WI"])
                        V2(lambda e: e.tensor_tensor(out=ta[:], in0=WR[:], in1=COSC[:], op=ALU.mult), ["WR", "COSC", "WI"], ["ta"])
                        V2(lambda e: e.tensor_tensor(out=tbb[:], in0=WI[:], in1=SINC[:], op=ALU.mult), ["WI", "SINC"], ["tbb"])
                        V2(lambda e: e.tensor_tensor(out=XPR[:, 1:513], in0=ta[:], in1=tbb[:], op=ALU.subtract), ["ta", "tbb"], ["XPR"])
                        V2(lambda e: e.tensor_tensor(out=ta[:], in0=WI[:], in1=COSC[:], op=ALU.mult), ["WI", "COSC", "XPR"], ["ta"])
                        V2(lambda e: e.tensor_tensor(out=tbb[:], in0=WR[:], in1=SINC[:], op=ALU.mult), ["WR", "SINC"], ["tbb"])
                        V2(lambda e: e.tensor_tensor(out=XPI[:, 1:513], in0=ta[:], in1=tbb[:], op=ALU.add), ["ta", "tbb"], ["XPI"])
                        for m in range(2):
                            g = 2 * q + m
                            pbY = B("psY%d" % m)
                            for s in range(4):
                                cs = slice(s * 128, (s + 1) * 128)
                                outp = psY[m][:, s * 128:(s + 1) * 128]
                                O("pe", lambda e, outp=outp, cs=cs, g=g, m=m: e.matmul(outp, lhsT=Ug[m][:, cs], rhs=MI[:, g, :], start=True, stop=False),
                                  reads=[B("Ug%d" % m), B("MI")], writes=[pbY])
                                O("pe", lambda e, outp=outp, cs=cs, q=q, m=m: e.matmul(outp, lhsT=XPR[:, cs], rhs=MoRp[:, q, m, :], start=False, stop=False),
                                  reads=[B("XPR"), B("MoRp")], writes=[pbY])
                                O("pe", lambda e, outp=outp, cs=cs, q=q, m=m: e.matmul(outp, lhsT=XPI[:, cs], rhs=MoIp[:, q, m, :], start=False, stop=True),
                                  reads=[B("XPI"), B("MoIp")], writes=[pbY])
                            yv = psY[m][:, :]
                            V2(lambda e, yv=yv: e.activation(out=gs[:], in_=yv, func=AF.Square), ["psY%d" % m], ["gs"], "act")
                            V2(lambda e: e.tensor_scalar(out=gs[:], in0=gs[:], scalar1=0.044715, scalar2=1.0, op0=ALU.mult, op1=ALU.add), ["gs"], ["gs"])
                            V2(lambda e, yv=yv: e.tensor_tensor(out=gu[:], in0=yv, in1=gs[:], op=ALU.mult), ["psY%d" % m, "gs"], ["gu"])
                            V2(lambda e: e.activation(out=gu[:], in_=gu[:], func=AF.Sigmoid, scale=1.5957691216), ["gu"], ["gu"], "act")
                            O("dve", lambda e, yv=yv, g=g: e.tensor_tensor(out=Zt[:, :, g, :, :],
                                                                            in0=yv.rearrange("p (s j h) -> p s j h", s=4, j=8),
                                                                            in1=gu[:].rearrange("p (s j h) -> p s j h", s=4, j=8), op=ALU.mult),
                              reads=[pbY, B("gu")], writes=[zb])
                pg.barrier()

                with contextlib.ExitStack() as s1c:
                    vT = [T(s1c, "vT%d" % k, [128, 4, 1024], BF16) for k in range(2)]
                    yaT = [T(s1c, "yaT%d" % k, [128, 4, 1024], BF16) for k in range(2)]
                    sg = T(s1c, "sg", [128, 512], F32)
                    tmpV = T(s1c, "tmpV", [128, 8, 512], BF16)
                    psV = [PS(s1c, "psV%d" % k, [128, 1024], BF16) for k in range(2)]
                    psG = [PS(s1c, "psG%d" % k, [128, 512], F32) for k in range(2)]
                    zall = [B("Zt%d" % q_) for q_ in range(16)]
                    cnt = 0
                    for s in range(4):
                        sl = s % 2
                        O("pool", lambda e, s=s: e.tensor_copy(out=tmpV[:].rearrange("p j (g h) -> p j g h", g=32), in_=Zt[:, s, :, :, :].rearrange("p g j h -> p j g h")),
                          reads=zall, writes=[B("tmpV")])
                        for fc in range(4):
                            pv = psV[fc % 2]
                            pvb = B("psV%d" % (fc % 2))
                            for j in range(8):
                                O("pe", lambda e, pv=pv, s=s, j=j, fc=fc: e.transpose(out=pv[:, j * 128:(j + 1) * 128], in_=tmpV[:, j, fc * 128:(fc + 1) * 128], identity=identb[:]),
                                  reads=[B("tmpV"), B("identb")], writes=[pvb])
                            dst = vT[sl][:, fc, :].rearrange("p (c j) -> p j c", j=8)
                            srcv = pv[:, :].rearrange("p (j c) -> p j c", j=8)
                            if fc % 2 == 0:
                                O("act", lambda e, dst=dst, srcv=srcv: e.activation(func=AF.Identity, out=dst, in_=srcv), reads=[pvb], writes=[B("vT%d" % sl)])
                            else:
                                O("dve", lambda e, dst=dst, srcv=srcv: e.tensor_copy(out=dst, in_=srcv), reads=[pvb], writes=[B("vT%d" % sl)])
                        for oc in range(4):
                            for th in range(2):
                                pgk = cnt % 2
                                cnt += 1
                                pG = psG[pgk]
                                pGb = B("psG%d" % pgk)
                                ts_ = slice(th * 512, (th + 1) * 512)
                                for fc in range(4):
                                    O("pe", lambda e, pG=pG, fc=fc, oc=oc, ts_=ts_, sl=sl: e.matmul(pG[:, :], lhsT=wG[:, fc, oc * 128:(oc + 1) * 128], rhs=vT[sl][:, fc, ts_],
                                                                                                   start=(fc == 0), stop=(fc == 3)), reads=[B("wG"), B("vT%d" % sl)], writes=[pGb])
                                O("act", lambda e, pG=pG, oc=oc: e.activation(out=sg[:], in_=pG[:, :], func=AF.Sigmoid, bias=bglu[:, oc:oc + 1]),
                                  reads=[pGb, B("bglu")], writes=[B("sg")])
                                O("dve", lambda e, oc=oc, ts_=ts_, sl=sl: e.tensor_tensor(out=yaT[sl][:, oc, ts_], in0=vT[sl][:, oc, ts_], in1=sg[:], op=ALU.mult),
                                  reads=[B("vT%d" % sl), B("sg")], writes=[B("yaT%d" % sl)])
                        for oc in range(4):
                            stp = O("sp", lambda e, oc=oc, s=s, sl=sl: e.dma_start(out=ya_d[oc * 128:(oc + 1) * 128, s * 1024:(s + 1) * 1024], in_=yaT[sl][:, oc, :]),
                                    reads=[B("yaT%d" % sl)], writes=[B("ya_d_%d_%d" % (s, oc))], dma_key="yast%d" % sl)
                    ya_last = [pg.ops["sp"][-1]]
                    ya_stores = [o for o in pg.ops["sp"] if o.is_dma and isinstance(o.dsem, str) and o.dsem.startswith("yast")]
            pg.barrier(dma_ops=ya_stores[-8:])

        if STOP_AFTER == "S":
            pg.emit(final_wait_ops=ya_stores[-8:])
            return nc

        with contextlib.ExitStack() as sA:
            wQ = T(sA, "wQ", [128, 8, 512], BF16)
            wGA = T(sA, "wGA", [128, 8, 1024], BF16)
            wGB = T(sA, "wGB", [128, 8, 1024], BF16)
            wA = T(sA, "wA", [128, 4, 1024], BF16)
            wB = T(sA, "wB", [128, 4, 1024], BF16)
            wO = T(sA, "wO", [128, 8, 1024], BF16)
            g2b = T(sA, "g2b", [128, D], F32)
            load("sp", g2b[:], g2_d.partition_broadcast(128), "g2b", "g2b")
            for dc in range(8):
                rows = slice(dc * 128, (dc + 1) * 128)
                load("pool", wQ[:, dc, :], w_in_d[rows, 512:1024], "wQ", "wQ")
            for dc in range(8 if not DBG_ONLYQ else 0):
                rows = slice(dc * 128, (dc + 1) * 128)
                load("pool", wGA[:, dc, :], w_in_d[rows, 2048:3072], "wGA", "wGA")
                load("pool", wGB[:, dc, :], w_in_d[rows, 3072:4096], "wGB", "wGB")
            for fc in range(4 if not DBG_ONLYQ else 0):
                rows = slice(fc * 128, (fc + 1) * 128)
                load("pool", wA[:, fc, :], w_a_d[rows, :], "wA", "wA")
                load("pool", wB[:, fc, :], w_b_d[rows, :], "wB", "wB")
            for dc in range(8 if not DBG_ONLYQ else 0):
                rows = slice(dc * 128, (dc + 1) * 128)
                load("pool", wO[:, dc, :], w_out_d[rows, :], "wO", "wO")

            xq = [T(sA, "xq%d" % k, [128, 2, D], F32) for k in range(2)]
            yaq = [T(sA, "yaq%d" % k, [128, 4, 256], BF16) for k in range(2)]
            uTq = T(sA, "uTq", [128, 8, 256], BF16)
            scrA = {"sq": T(sA, "sqA", [128, D], BF16)[:], "ss": T(sA, "ssA", [128, 1], F32)[:], "rs": T(sA, "rsA", [128, 1], F32)[:],
                    "xn": T(sA, "xnA", [128, D], F32)[:], "eps": epsb[:]}
            QT = T(sA, "QT", [128, 4, 256], BF16)
            QTf = T(sA, "QTf", [128, 4, 256], F32)
            gate = T(sA, "gate", [128, 2, 8, 16], F32)
            top8 = T(sA, "top8", [128, 2, 8, 8], F32)
            sel = T(sA, "sel", [128, 2, 8, 16], F32)
            trim = T(sA, "trim", [128, 2, 256], BF16)
            PT = [T(sA, "PT%d" % k, [128, 2, 256], BF16) for k in range(3)]
            acc = T(sA, "acc", [128, 2, 8, 65], F32)
            rden = T(sA, "rden", [128, 2, 8], F32)
            ybt = T(sA, "ybt", [128, 2, 512], BF16)
            ybT = T(sA, "ybT", [128, 4, 256], BF16)
            sga2 = [T(sA, "sga%d" % k, [128, 256], F32) for k in range(2)]
            sgb2 = [T(sA, "sgb%d" % k, [128, 256], F32) for k in range(2)]
            tma2 = [T(sA, "tma%d" % k, [128, 256], F32) for k in range(2)]
            mT = T(sA, "mT", [128, 8, 256], BF16)
            mg = scrA["xn"]
            ssM = T(sA, "ssM", [128, 2], F32)
            rsM = T(sA, "rsM", [128, 1], F32)
            psT = [PS(sA, "psTA%d" % k, [128, 512], F32) for k in range(2)]
            psS = [PS(sA, "psS%d" % k, [128, 512], F32) for k in range(2)]
            psO = [PS(sA, "psO%d" % k, [128, 512], F32) for k in range(2)]
            psW = [PS(sA, "psW%d" % k, [128, 512], F32) for k in range(2)]
            psTb = [B("psTA0"), B("psTA1")]

            O("pool", lambda e: e.memset(trim[:], 1.0), writes=[B("trim")])
            for kh in range(2):
                O("pool", lambda e, kh=kh: e.affine_select(out=trim[:, kh, :], in_=trim[:, kh, :], pattern=[[1, 256]], compare_op=ALU.is_ge,
                                                           fill=0.0, base=-128 * kh, channel_multiplier=-1), reads=[B("trim")], writes=[B("trim")])

            def issue_A_loads(i):
                sl = i % 2
                for qh in range(2):
                    tt = 2 * i + qh
                    load("sp", xq[sl][:, qh, :], x_d[tt * 128:(tt + 1) * 128, :], "xq%d" % sl, "xq%d" % sl)
                if not DBG_NOYA:
                    for fc in range(4):
                        load("sp", yaq[sl][:, fc, :], ya_d[fc * 128:(fc + 1) * 128, i * 256:(i + 1) * 256], "yaq%d" % sl, "yaq%d" % sl)

            if STOP_AFTER == "A0":
                pg.emit(final_wait_ops=[])
                return nc
            issue_A_loads(0)
            ptc = [0]
            h1_stores = []
            for i in range(DBG_NBLK):
                sl = i % 2
                if i + 1 < DBG_NBLK:
                    issue_A_loads(i + 1)
                for qh in range(2):
                    rmsnorm_T("A", xq[sl][:, qh, :], B("xq%d" % sl), g1sb,
                              lambda dc, qh=qh: uTq[:, dc, qh * 128:(qh + 1) * 128], B("uTq"), psT, psTb, scrA, "A")
                if STOP_AFTER == "A1":
                    pg.emit(final_wait_ops=[])
                    return nc
                for hp in range(4):
                    ps = psS[hp % 2]
                    pb = B("psS%d" % (hp % 2))
                    for dc in range(8):
                        O("pe", lambda e, ps=ps, dc=dc, hp=hp: e.matmul(ps[:, 0:256], lhsT=wQ[:, dc, hp * 128:(hp + 1) * 128], rhs=uTq[:, dc, :],
                                                                        start=(dc == 0), stop=(dc == 7)), reads=[B("wQ"), B("uTq")], writes=[pb])
                    O("dve", lambda e, ps=ps, hp=hp: e.tensor_copy(out=QTf[:, hp, :], in_=ps[:, 0:256]), reads=[pb], writes=[B("QTf")])
                    O("pool", lambda e, hp=hp: e.tensor_copy(out=QT[:, hp, :], in_=QTf[:, hp, :]), reads=[B("QTf")], writes=[B("QT")])
                if STOP_AFTER == "A2":
                    pg.emit(final_wait_ops=[])
                    return nc
                use_sel = i >= 4
                if use_sel:
                    for qh in range(2):
                        pgt = psO[qh]
                        pgb = B("psO%d" % qh)
                        for h in range(8):
                            hp, par = h // 2, h % 2
                            rows = slice(64 * par, 64 * par + 64)
                            O("pe", lambda e, pgt=pgt, h=h, hp=hp, rows=rows, qh=qh: e.matmul(pgt[:, h * 16:(h + 1) * 16], lhsT=QTf[rows, hp, qh * 128:(qh + 1) * 128],
                                                                                            rhs=KS[rows, hp, :], start=True, stop=True),
                              reads=[B("QTf"), B("KS")], writes=[pgb])
                        O("dve", lambda e, qh=qh: e.memset(gate[:, qh, :, :], NEG), writes=[B("gate")])
                        O("dve", lambda e, qh=qh, pgt=pgt, i=i: e.tensor_copy(out=gate[:, qh, :, 0:i], in_=pgt[:, 0:128].rearrange("p (h j) -> p h j", h=8)[:, :, 0:i]),
                          reads=[pgb], writes=[B("gate")])
                        for h in range(8):
                            O("dve", lambda e, qh=qh, h=h: e.max(out=top8[:, qh, h, :], in_=gate[:, qh, h, :]), reads=[B("gate")], writes=[B("top8")])
                        O("dve", lambda e, qh=qh: e.tensor_tensor(out=sel[:, qh, :, :], in0=gate[:, qh, :, :], in1=top8[:, qh, :, 2:3].to_broadcast([128, 8, 16]), op=ALU.is_ge),
                          reads=[B("gate"), B("top8")], writes=[B("sel")])
                items = [(h, j) for h in range(8) for j in range(i + 1)]
                slots = {}

                def emit_qk(n):
                    h, j = items[n]
                    hp, par = h // 2, h % 2
                    rows = slice(64 * par, 64 * par + 64)
                    k3 = ptc[0] % 3
                    k2 = ptc[0] % 2
                    ptc[0] += 1
                    slots[n] = (k2, k3)
                    pS = psS[k2]
                    pSb = B("psS%d" % k2)
                    for kh in range(2):
                        key0 = j * 256 + kh * 128
                        O("pe", lambda e, pS=pS, kh=kh, rows=rows, hp=hp, key0=key0: e.matmul(pS[:, kh * 256:(kh + 1) * 256], lhsT=KT[rows, hp, key0:key0 + 128],
                                                                                             rhs=QT[rows, hp, :], start=True, stop=True),
                          reads=[B("KT"), B("QT")], writes=[pSb])

                def emit_rest(n):
                    h, j = items[n]
                    k2, k3 = slots[n]
                    pS = psS[k2]
                    pSb = B("psS%d" % k2)
                    pt = PT[k3]
                    ptb = B("PT%d" % k3)
                    O("act", lambda e, pS=pS, pt=pt: e.activation(out=pt[:].rearrange("p a b -> p (a b)"), in_=pS[:, :], func=AF.Exp, scale=0.125),
                      reads=[pSb], writes=[ptb])
                    if j == i:
                        O("pool", lambda e, pt=pt: e.tensor_tensor(out=pt[:], in0=pt[:], in1=trim[:], op=ALU.mult), reads=[ptb, B("trim")], writes=[ptb])
                    pO = psO[k2]
                    pOb = B("psO%d" % k2)
                    for qh in range(2):
                        for kh in range(2):
                            if j == i and kh == 1 and qh == 0:
                                continue
                            first = (kh == 0)
                            last = (kh == 1) or (j == i and qh == 0)
                            O("pe", lambda e, pO=pO, qh=qh, kh=kh, pt=pt, j=j, h=h, first=first, last=last: e.matmul(
                                pO[:, qh * 128:qh * 128 + 66], lhsT=pt[:, kh, qh * 128:(qh + 1) * 128], rhs=VA[:, 2 * j + kh, h, :], start=first, stop=last),
                              reads=[ptb, B("VA"), B("VAones"), B("VAzeros")], writes=[pOb])
                    for qh in range(2):
                        src = pO[:, qh * 128:qh * 128 + 65]
                        dsta = acc[:, qh, h, :]
                        ab = B("acc_%d_%d" % (qh, h))
                        if j == 0:
                            if use_sel and j < i:
                                O("dve", lambda e, src=src, dsta=dsta, qh=qh, h=h, j=j: e.tensor_scalar(out=dsta, in0=src, scalar1=sel[:, qh, h, j:j + 1], scalar2=None, op0=ALU.mult),
                                  reads=[pOb, B("sel")], writes=[ab])
                            else:
                                O("dve", lambda e, src=src, dsta=dsta: e.tensor_copy(out=dsta, in_=src), reads=[pOb], writes=[ab])
                        else:
                            if use_sel and j < i:
                                O("dve", lambda e, src=src, dsta=dsta, qh=qh, h=h, j=j: e.scalar_tensor_tensor(out=dsta, in0=src, scalar=sel[:, qh, h, j:j + 1], in1=dsta,
                                                                                                                  op0=ALU.mult, op1=ALU.add),
                                  reads=[pOb, B("sel"), ab], writes=[ab])
                            else:
                                O("dve", lambda e, src=src, dsta=dsta: e.tensor_tensor(out=dsta, in0=src, in1=dsta, op=ALU.add), reads=[pOb, ab], writes=[ab])

                emit_qk(0)
                for n in range(len(items)):
                    if n + 1 < len(items):
                        emit_qk(n + 1)
                    emit_rest(n)
                if STOP_AFTER == "A3":
                    pg.emit(final_wait_ops=[])
                    return nc
                for qh in range(2):
                    accbufs = [B("acc_%d_%d" % (qh, h_)) for h_ in range(8)]
                    O("dve", lambda e, qh=qh: e.reciprocal(out=rden[:, qh, :], in_=acc[:, qh, :, 64]), reads=accbufs, writes=[B("rden")])
                    O("dve", lambda e, qh=qh: e.tensor_tensor(out=ybt[:, qh, :].rearrange("p (h d) -> p h d", h=8), in0=acc[:, qh, :, 0:64],
                                                              in1=rden[:, qh, :].unsqueeze(2).to_broadcast([128, 8, 64]), op=ALU.mult),
                      reads=accbufs + [B("rden")], writes=[B("ybt")])
                pvT = psT[0]
                for qh in range(2):
                    for fc in range(4):
                        O("pe", lambda e, qh=qh, fc=fc: e.transpose(out=pvT[:].bitcast(BF16)[:, (qh * 4 + fc) * 128:(qh * 4 + fc + 1) * 128],
                                                                     in_=ybt[:, qh, fc * 128:(fc + 1) * 128], identity=identb[:]),
                          reads=[B("ybt"), B("identb")], writes=[psTb[0]])
                for qh in range(2):
                    O("act", lambda e, qh=qh: e.activation(func=AF.Identity, out=ybT[:, :, qh * 128:(qh + 1) * 128], in_=pvT[:].bitcast(BF16)[:, qh * 512:(qh + 1) * 512].rearrange("p (f t) -> p f t", f=4)),
                      reads=[psTb[0]], writes=[B("ybT")])
                if STOP_AFTER == "A4":
                    pg.emit(final_wait_ops=[])
                    return nc
                for oc in range(8):
                    ocs = slice(oc * 128, (oc + 1) * 128)
                    pw = psW[oc % 2]
                    pwbuf = B("psW%d" % (oc % 2))
                    pq = psS[oc % 2]
                    pqb = B("psS%d" % (oc % 2))
                    for fc in range(4):
                        O("pe", lambda e, pw=pw, fc=fc, ocs=ocs, sl=sl: e.matmul(pw[:, 0:256], lhsT=wA[:, fc, ocs], rhs=yaq[sl][:, fc, :], start=(fc == 0), stop=(fc == 3)),
                          reads=[B("wA"), B("yaq%d" % sl)], writes=[pwbuf])
                    for fc in range(4):
                        O("pe", lambda e, pw=pw, fc=fc, ocs=ocs: e.matmul(pw[:, 256:512], lhsT=wB[:, fc, ocs], rhs=ybT[:, fc, :], start=(fc == 0), stop=(fc == 3)),
                          reads=[B("wB"), B("ybT")], writes=[pwbuf])
                    for dc in range(8):
                        O("pe", lambda e, pq=pq, dc=dc, ocs=ocs: e.matmul(pq[:, 0:256], lhsT=wGA[:, dc, ocs], rhs=uTq[:, dc, :], start=(dc == 0), stop=(dc == 7)),
                          reads=[B("wGA"), B("uTq")], writes=[pqb])
                    for dc in range(8):
                        O("pe", lambda e, pq=pq, dc=dc, ocs=ocs: e.matmul(pq[:, 256:512], lhsT=wGB[:, dc, ocs], rhs=uTq[:, dc, :], start=(dc == 0), stop=(dc == 7)),
                          reads=[B("wGB"), B("uTq")], writes=[pqb])
                    ob = oc % 2
                    sga, sgb, tma = sga2[ob], sgb2[ob], tma2[ob]
                    O("act", lambda e, pq=pq, sga=sga: e.activation(out=sga[:], in_=pq[:, 0:256], func=AF.Sigmoid), reads=[pqb], writes=[B("sga%d" % ob)])
                    O("act", lambda e, pq=pq, sgb=sgb: e.activation(out=sgb[:], in_=pq[:, 256:512], func=AF.Sigmoid), reads=[pqb], writes=[B("sgb%d" % ob)])
                    O("dve", lambda e, pw=pw, sga=sga, tma=tma: e.tensor_tensor(out=tma[:], in0=pw[:, 0:256], in1=sga[:], op=ALU.mult), reads=[pwbuf, B("sga%d" % ob)], writes=[B("tma%d" % ob)])
                    O("dve", lambda e, pw=pw, sgb=sgb: e.tensor_tensor(out=sgb[:], in0=pw[:, 256:512], in1=sgb[:], op=ALU.mult), reads=[pwbuf, B("sgb%d" % ob)], writes=[B("sgb%d" % ob)])
                    O("dve", lambda e, oc=oc, tma=tma, sgb=sgb: e.tensor_tensor(out=mT[:, oc, :], in0=tma[:], in1=sgb[:], op=ALU.add), reads=[B("tma%d" % ob), B("sgb%d" % ob)], writes=[B("mT")])
                if STOP_AFTER == "A5":
                    pg.emit(final_wait_ops=[])
                    return nc
                for qh in range(2):
                    tt = 2 * i + qh
                    for ch in range(2):
                        pw = psW[ch]
                        pwbuf = B("psW%d" % ch)
                        for dc in range(8):
                            O("pe", lambda e, pw=pw, dc=dc, ch=ch, qh=qh: e.matmul(pw[:, :], lhsT=mT[:, dc, qh * 128:(qh + 1) * 128], rhs=wO[:, dc, ch * 512:(ch + 1) * 512],
                                                                                   start=(dc == 0), stop=(dc == 7)), reads=[B("mT"), B("wO")], writes=[pwbuf])
                        O("act", lambda e, pw=pw, ch=ch: e.activation(out=scrA["sq"][:, ch * 512:(ch + 1) * 512], in_=pw[:, :], func=AF.Square, accum_out=ssM[:, ch:ch + 1]),
                          reads=[pwbuf], writes=[B("Asq"), B("ssM")])
                        O("dve", lambda e, pw=pw, ch=ch: e.tensor_tensor(out=mg[:, ch * 512:(ch + 1) * 512], in0=pw[:, :], in1=g2b[:, ch * 512:(ch + 1) * 512], op=ALU.mult),
                          reads=[pwbuf, B("g2b"), B("ssM")], writes=[B("Axn")])
                    O("dve", lambda e: e.tensor_tensor(out=rsM[:], in0=ssM[:, 0:1], in1=ssM[:, 1:2], op=ALU.add), reads=[B("ssM")], writes=[B("rsM")])
                    O("act", lambda e: e.activation(out=rsM[:], in_=rsM[:], func=AF.Sqrt, scale=1.0 / D, bias=epsb[:]), reads=[B("rsM"), B("epsb")], writes=[B("rsM")])
                    O("dve", lambda e: e.reciprocal(out=rsM[:], in_=rsM[:]), reads=[B("rsM")], writes=[B("rsM")])
                    hs = sl
                    O("dve", lambda e, qh=qh, sl=sl: e.scalar_tensor_tensor(out=xq[sl][:, qh, :], in0=mg, scalar=rsM[:, 0:1], in1=xq[sl][:, qh, :], op0=ALU.mult, op1=ALU.add),
                      reads=[B("Axn"), B("rsM"), B("xq%d" % sl)], writes=[B("xq%d" % sl)])
                    st_ = O("sp", lambda e, sl=sl, qh=qh, tt=tt: e.dma_start(out=out_d[tt * 128:(tt + 1) * 128, :], in_=xq[sl][:, qh, :]), reads=[B("xq%d" % sl)],
                            writes=[B("out%d" % tt)], dma_key="h1st%d" % hs)
                    h1_stores.append(st_)
        pg.barrier(dma_ops=h1_stores)
        sKV.close()
        if STOP_AFTER == "A":
            pg.emit(final_wait_ops=h1_stores)
            return nc

        with contextlib.ExitStack() as sB:
            w1 = T(sB, "w1", [128, 8, 4096], BF16)
            w2 = T(sB, "w2", [128, 32, D], BF16)
            g4b = T(sB, "g4b", [128, D], F32)
            load("sp", g4b[:], g4_d.partition_broadcast(128), "g4b", "g4b")
            grp_last = []

            def w1_group(cc):
                aft = [grp_last[-2]] if len(grp_last) >= 2 else []
                for dc in range(8):
                    rows = slice(dc * 128, (dc + 1) * 128)
                    o_ = load("pool", w1[:, dc, cc * 1024:(cc + 1) * 1024], w_ff1_d[rows, cc * 1024:(cc + 1) * 1024], "w1_%d" % cc, "w1_%d" % cc,
                              after=aft if dc == 0 else ())
                grp_last.append(o_)

            def w2_group(fg):
                aft = [grp_last[-2]] if len(grp_last) >= 2 else []
                for fc in range(fg * 8, fg * 8 + 8):
                    rows = slice(fc * 128, (fc + 1) * 128)
                    o_ = load("pool", w2[:, fc, :], w_ff2_d[rows, :], "w2_%d" % fg, "w2_%d" % fg, after=aft if fc == fg * 8 else ())
                grp_last.append(o_)

            w1_group(0)
            w2_group(0)
            w1_group(1)
            w1_group(2)
            w1_group(3)
            w2_group(1)
            w2_group(2)
            w2_group(3)
            hx = [T(sB, "hx%d" % k, [128, 2, D], F32) for k in range(2)]
            fT = T(sB, "fT", [128, 8, 256], BF16)
            hT = T(sB, "hT", [128, 32, 256], BF16)
            rl2 = [T(sB, "rl2_%d" % k, [128, 512], BF16) for k in range(2)]
            scrB = {"sq": T(sB, "sqB", [128, D], BF16)[:], "ss": T(sB, "ssB", [128, 1], F32)[:], "rs": T(sB, "rsB", [128, 1], F32)[:],
                    "xn": T(sB, "xnB", [128, D], F32)[:], "eps": epsb[:]}
            mgB = scrB["xn"]
            ssF = T(sB, "ssF", [128, 2], F32)
            rsF = T(sB, "rsF", [128, 1], F32)
            psT = [PS(sB, "psTB%d" % k, [128, 512], F32) for k in range(2)]
            psH = [PS(sB, "psH%d" % k, [128, 512], F32) for k in range(3)]
            psW = [PS(sB, "psWB%d" % k, [128, 512], F32) for k in range(2)]
            psTb = [B("psTB0"), B("psTB1")]

            def issue_B_loads(gi):
                sl = gi % 2
                for t2 in range(2):
                    tt = gi * 2 + t2
                    load("sp", hx[sl][:, t2, :], out_d[tt * 128:(tt + 1) * 128, :], "hx%d" % sl, "hx%d" % sl)

            if STOP_AFTER == "B0":
                pg.emit(final_wait_ops=[])
                return nc
            issue_B_loads(0)
            hc = 0
            fTs = [fT, T(sB, "fT1", [128, 8, 256], BF16)]
            xnB = [T(sB, "xnB%d" % k, [128, D], F32) for k in range(4)]
            ssB4 = T(sB, "ssB4", [128, 4], F32)
            rsB4 = T(sB, "rsB4", [128, 4], F32)

            def normB_part1(g):
                sl_ = g % 2
                for t2 in range(2):
                    k = (g % 2) * 2 + t2
                    xt_ = hx[sl_][:, t2, :]
                    xb_ = B("hx%d" % sl_)
                    O("act", lambda e, xt_=xt_, k=k: e.activation(out=scrB["sq"], in_=xt_, func=AF.Square, accum_out=ssB4[:, k:k + 1]), reads=[xb_], writes=[B("Bsq"), B("ssB4_%d" % k)])
                    O("act", lambda e, k=k: e.activation(out=rsB4[:, k:k + 1], in_=ssB4[:, k:k + 1], func=AF.Sqrt, scale=1.0 / D, bias=epsb[:]), reads=[B("ssB4_%d" % k), B("epsb")], writes=[B("rsB4_%d" % k)])
                    O("dve", lambda e, k=k: e.reciprocal(out=rsB4[:, k:k + 1], in_=rsB4[:, k:k + 1]), reads=[B("rsB4_%d" % k)], writes=[B("rsB4_%d" % k)])
                    O("dve", lambda e, xt_=xt_, k=k: e.tensor_scalar(out=xnB[k][:], in0=xt_, scalar1=rsB4[:, k:k + 1], scalar2=None, op0=ALU.mult), reads=[xb_, B("rsB4_%d" % k)], writes=[B("xnB%d" % k)])

            def normB_part2(g):
                fTg = fTs[g % 2]
                fb = B("fT%d" % (g % 2))
                for t2 in range(2):
                    k = (g % 2) * 2 + t2
                    for half in range(2):
                        ps = psT[half]
                        pb = psTb[half]
                        for kk in range(4):
                            dc = half * 4 + kk
                            O("pe", lambda e, dc=dc, kk=kk, ps=ps, k=k: e.transpose(out=ps[:, kk * 128:(kk + 1) * 128], in_=xnB[k][:, dc * 128:(dc + 1) * 128], identity=identf[:]),
                              reads=[B("xnB%d" % k), B("identf")], writes=[pb])
                        for kk in range(4):
                            dc = half * 4 + kk
                            dst = fTg[:, dc, t2 * 128:(t2 + 1) * 128]
                            if kk % 2 == 0:
                                O("act", lambda e, dc=dc, kk=kk, ps=ps, dst=dst: e.activation(out=dst, in_=ps[:, kk * 128:(kk + 1) * 128], func=AF.Copy, scale=g3sb[:, dc:dc + 1]),
                                  reads=[pb, B("gsb3")], writes=[fb])
                            else:
                                O("dve", lambda e, dc=dc, kk=kk, ps=ps, dst=dst: e.tensor_scalar(out=dst, in0=ps[:, kk * 128:(kk + 1) * 128], scalar1=g3sb[:, dc:dc + 1], scalar2=None, op0=ALU.mult),
                                  reads=[pb, B("gsb3")], writes=[fb])

            normB_part1(0)
            normB_part2(0)
            for gi in range(16):
                sl = gi % 2
                fTg = fTs[gi % 2]
                fTb = B("fT%d" % (gi % 2))
                if gi + 1 < 16:
                    issue_B_loads(gi + 1)
                    normB_part1(gi + 1)
                for fc2 in range(16):
                    ph = psH[hc % 3]
                    phb = B("psH%d" % (hc % 3))
                    rlk = hc % 2
                    hc += 1
                    for u in range(2):
                        fc = fc2 * 2 + u
                        for dc in range(8):
                            O("pe", lambda e, ph=ph, dc=dc, fc=fc, u=u, fTg=fTg: e.matmul(ph[:, u * 256:(u + 1) * 256], lhsT=w1[:, dc, fc * 128:(fc + 1) * 128], rhs=fTg[:, dc, :],
                                                                                 start=(dc == 0), stop=(dc == 7)), reads=[B("w1_%d" % (fc // 8)), fTb], writes=[phb])
                    O("act", lambda e, ph=ph, rlk=rlk: e.activation(out=rl2[rlk][:], in_=ph[:, :], func=AF.Relu), reads=[phb], writes=[B("rl2_%d" % rlk)])
                    O("dve", lambda e, ph=ph, fc2=fc2, rlk=rlk: e.scalar_tensor_tensor(out=hT[:, fc2 * 2:fc2 * 2 + 2, :].rearrange("p a b -> p (a b)"), in0=ph[:, :], scalar=0.0,
                                                                                         in1=rl2[rlk][:], op0=ALU.max, op1=ALU.mult),
                      reads=[phb, B("rl2_%d" % rlk)], writes=[B("hT")])
                if STOP_AFTER == "B2":
                    pg.emit(final_wait_ops=[])
                    return nc
                if gi + 1 < 16:
                    normB_part2(gi + 1)
                for t2 in range(2):
                    tt = gi * 2 + t2
                    for ch in range(2):
                        pw = psW[ch]
                        pwbuf = B("psWB%d" % ch)
                        for fc in range(32):
                            O("pe", lambda e, pw=pw, fc=fc, ch=ch, t2=t2: e.matmul(pw[:, :], lhsT=hT[:, fc, t2 * 128:(t2 + 1) * 128], rhs=w2[:, fc, ch * 512:(ch + 1) * 512],
                                                                                   start=(fc == 0), stop=(fc == 31)), reads=[B("hT"), B("w2_%d" % (fc // 8))], writes=[pwbuf])
                        O("act", lambda e, pw=pw, ch=ch: e.activation(out=scrB["sq"][:, ch * 512:(ch + 1) * 512], in_=pw[:, :], func=AF.Square, accum_out=ssF[:, ch:ch + 1]),
                          reads=[pwbuf], writes=[B("Bsq"), B("ssF")])
                        O("dve", lambda e, pw=pw, ch=ch: e.tensor_tensor(out=mgB[:, ch * 512:(ch + 1) * 512], in0=pw[:, :], in1=g4b[:, ch * 512:(ch + 1) * 512], op=ALU.mult),
                          reads=[pwbuf, B("g4b"), B("ssF")], writes=[B("Bxn")])
                    O("dve", lambda e: e.tensor_tensor(out=rsF[:], in0=ssF[:, 0:1], in1=ssF[:, 1:2], op=ALU.add), reads=[B("ssF")], writes=[B("rsF")])
                    O("act", lambda e: e.activation(out=rsF[:], in_=rsF[:], func=AF.Sqrt, scale=1.0 / D, bias=epsb[:]), reads=[B("rsF"), B("epsb")], writes=[B("rsF")])
                    O("dve", lambda e: e.reciprocal(out=rsF[:], in_=rsF[:]), reads=[B("rsF")], writes=[B("rsF")])
                    O("dve", lambda e, t2=t2, sl=sl: e.scalar_tensor_tensor(out=hx[sl][:, t2, :], in0=mgB, scalar=rsF[:, 0:1], in1=hx[sl][:, t2, :], op0=ALU.mult, op1=ALU.add),
                      reads=[B("Bxn"), B("rsF"), B("hx%d" % sl)], writes=[B("hx%d" % sl)])
                    st_ = O("sp", lambda e, sl=sl, t2=t2, tt=tt: e.dma_start(out=out_d[tt * 128:(tt + 1) * 128, :], in_=hx[sl][:, t2, :]), reads=[B("hx%d" % sl)],
                            writes=[B("out%d" % tt)], dma_key="ost%d" % sl)
                    final_stores.append(st_)
        pg.emit(final_wait_ops=final_stores)
    return nc


def _prep_shared(inp):
    f = lambda a: np.ascontiguousarray(np.asarray(a, dtype=np.float32))
    lam_re = f(inp["lam_re"])[0]
    lam_im = f(inp["lam_im"])[0]
    log_dt = f(inp["log_dt"])[0]
    b_re = f(inp["b_re"])[0]
    b_im = f(inp["b_im"])[0]
    c_re = f(inp["c_re"])[0]
    c_im = f(inp["c_im"])[0]
    d_skip = f(inp["d_skip"])[0]

    def pq(a):
        return f(a.reshape(16, 2, 64).transpose(1, 2, 0).reshape(128, 16))

    def gv(a):
        return f(a.reshape(8, 128).T)

    sh = {
        "w_in": f(inp["w_in"])[0],
        "w_glu": f(inp["w_glu"])[0],
        "w_a": f(inp["w_branch_a"])[0],
        "w_b": f(inp["w_branch_b"])[0],
        "w_out": f(inp["w_out"])[0],
        "w_ff1": f(inp["w_ff1"])[0],
        "w_ff2": f(inp["w_ff2"])[0],
        "g1": gv(f(inp["g_pre_mix"])[0]),
        "g3": gv(f(inp["g_pre_ffn"])[0]),
        "g2": f(inp["g_post_mix"]).reshape(1, D),
        "g4": f(inp["g_post_ffn"]).reshape(1, D),
        "bglu": f(f(inp["b_glu"])[0].reshape(4, 128).T),
        "LR": pq(lam_re),
        "LI": pq(lam_im),
        "LDT": f(np.broadcast_to(log_dt.reshape(16, 2).T[:, None, :], (2, 64, 16)).reshape(128, 16)),
        "BR": f(b_re.reshape(16, 2, 64, 16).transpose(1, 2, 0, 3).reshape(128, 256)),
        "BI": f(b_im.reshape(16, 2, 64, 16).transpose(1, 2, 0, 3).reshape(128, 256)),
        "CR": f(c_re.reshape(16, 2, 16, 64).transpose(1, 3, 0, 2).reshape(128, 256)),
        "CI": f(c_im.reshape(16, 2, 16, 64).transpose(1, 3, 0, 2).reshape(128, 256)),
        "DS": f(np.tile(d_skip.reshape(32, 16).T, (8, 1))),
    }
    return sh


def kernel(**inputs):
    x = np.asarray(inputs["x"], dtype=np.float32)
    sh = _prep_shared(inputs)
    nc = build_nc()
    in_maps = []
    for b in range(8):
        m = dict(sh)
        m["x"] = np.ascontiguousarray(x[b])
        in_maps.append(m)
    res = run_bass_kernel_spmd(nc, in_maps, core_ids=list(range(8)))
    out = np.stack([np.asarray(res.results[b]["out"], dtype=np.float32) for b in range(8)], axis=0)
    return out
```
